# Optimizing a Trainium2 kernel written in Bass

```python
import math
import jax, jax.numpy as jnp
from jax import lax
import numpy as np

D_MODEL = 1024
BATCH = 8
SEQ = 4096
DEPTH = 1

HEAD_DIM = 64
FOX_HEADS = 8
NSA_HEADS = 8
NSA_KV_GROUPS = 2
NSA_HPG = NSA_HEADS // NSA_KV_GROUPS
FOX_WIDTH = FOX_HEADS * HEAD_DIM
NSA_WIDTH = NSA_HEADS * HEAD_DIM
NSA_KV_WIDTH = NSA_KV_GROUPS * HEAD_DIM
CMP_BLOCK = 32
CMP_STRIDE = 16
SEL_BLOCK = 64
SEL_TOPN = 16
WINDOW = 512
FOX_Q_BLOCK = 128
NSA_Q_BLOCK = 64
REL_BUCKETS = 32
REL_MAX_DIST = 128
N_EXPERTS = 32
TOP_K = 4
D_EXPERT = D_MODEL
SWIGLU_LIMIT = 7.0
SWIGLU_ALPHA = 1.702
MOE_BLOCK = 256
RMS_EPS = 1e-6
NEG = -1e30
BIG = 1e9
IN_SIZES = (FOX_WIDTH, FOX_WIDTH, FOX_WIDTH, FOX_HEADS, NSA_WIDTH,
            NSA_KV_WIDTH, NSA_KV_WIDTH, NSA_KV_WIDTH, NSA_KV_WIDTH, NSA_KV_WIDTH, NSA_KV_WIDTH,
            3 * NSA_HEADS, D_MODEL, D_MODEL)
IN_WIDTH = sum(IN_SIZES)

kernel_name = "hybrid_fox_nsa_moe_block"


def rms_norm(x, g):
    xf = x.astype(jnp.float32)
    y = xf * lax.rsqrt(jnp.mean(xf * xf, axis=-1, keepdims=True) + RMS_EPS)
    return (y * g.astype(jnp.float32)).astype(x.dtype)


def t5_bucket(dist):
    n = jnp.maximum(dist, 0)
    max_exact = REL_BUCKETS // 2
    nf = jnp.maximum(n, 1).astype(jnp.float32)
    large = max_exact + (jnp.log(nf / max_exact) / math.log(REL_MAX_DIST / max_exact)
                         * (REL_BUCKETS - max_exact)).astype(jnp.int32)
    large = jnp.minimum(large, REL_BUCKETS - 1)
    return jnp.where(n < max_exact, n, large)


def fox_attention(q, k, v, log_f):
    B, S, H, dh = q.shape
    scale = dh ** -0.5
    cum = jnp.cumsum(log_f, axis=1).transpose(0, 2, 1)
    qh = q.transpose(0, 2, 1, 3)
    kh = k.transpose(0, 2, 1, 3)
    vh = v.transpose(0, 2, 1, 3)
    kpos = jnp.arange(S)

    def block(b):
        q0 = b * FOX_Q_BLOCK
        qb = lax.dynamic_slice_in_dim(qh, q0, FOX_Q_BLOCK, axis=2)
        cb = lax.dynamic_slice_in_dim(cum, q0, FOX_Q_BLOCK, axis=2)
        tpos = q0 + jnp.arange(FOX_Q_BLOCK)
        logits = (jnp.einsum('bhqd,bhkd->bhqk', qb, kh).astype(jnp.float32) * scale
                  + (cb[..., :, None] - cum[..., None, :]))
        logits = jnp.where(kpos[None, :] <= tpos[:, None], logits, NEG)
        p = jax.nn.softmax(logits, axis=-1)
        return jnp.einsum('bhqk,bhkd->bhqd', p.astype(vh.dtype), vh)

    out = lax.map(block, jnp.arange(S // FOX_Q_BLOCK))
    return out.transpose(1, 0, 3, 2, 4).reshape(B, S, H * dh)


def nsa_attention(q, k_cmp, v_cmp, k_slc, v_slc, k_win, v_win, gates,
                  pe_k, pe_v, w_ck, w_cv, rel_bias):
    B, S, H, dh = q.shape
    G, hpg, Tq = NSA_KV_GROUPS, NSA_HPG, NSA_Q_BLOCK
    scale = dh ** -0.5
    n_cmp = (S - CMP_BLOCK) // CMP_STRIDE + 1
    cidx = jnp.arange(n_cmp)[:, None] * CMP_STRIDE + jnp.arange(CMP_BLOCK)[None, :]

    def compress(kv, pe, w):
        blk = kv[:, cidx] + pe[None, None, :, None, :]
        blk = blk.transpose(0, 3, 1, 2, 4).reshape(B, G, n_cmp, CMP_BLOCK * dh)
        return blk @ w

    kc = compress(k_cmp, pe_k, w_ck)
    vc = compress(v_cmp, pe_v, w_cv)
    cmp_start = jnp.arange(n_cmp) * CMP_STRIDE
    cmp_end = cmp_start + CMP_BLOCK - 1
    n_sel = S // SEL_BLOCK
    top_n = min(SEL_TOPN, n_sel)
    ks = k_slc.reshape(B, n_sel, SEL_BLOCK, G, dh).transpose(0, 3, 1, 2, 4)
    vs = v_slc.reshape(B, n_sel, SEL_BLOCK, G, dh).transpose(0, 3, 1, 2, 4)
    sel_start = jnp.arange(n_sel) * SEL_BLOCK
    overlap = ((cmp_start[:, None] < sel_start[None, :] + SEL_BLOCK)
               & (cmp_start[:, None] + CMP_BLOCK > sel_start[None, :])).astype(jnp.float32)
    kw = jnp.pad(k_win, ((0, 0), (WINDOW, 0), (0, 0), (0, 0))).transpose(0, 2, 1, 3)
    vw = jnp.pad(v_win, ((0, 0), (WINDOW, 0), (0, 0), (0, 0))).transpose(0, 2, 1, 3)
    wk = WINDOW + Tq
    rel_d = (jnp.arange(Tq)[:, None] + WINDOW) - jnp.arange(wk)[None, :]
    win_band = (rel_d >= 0) & (rel_d < WINDOW)
    win_bias = rel_bias[t5_bucket(rel_d)].transpose(2, 0, 1).reshape(G, hpg, Tq, wk)
    tbl = rel_bias.reshape(REL_BUCKETS, G, hpg)
    qg = q.reshape(B, S, G, hpg, dh).transpose(0, 2, 3, 1, 4)
    gts = gates.reshape(B, S, G, hpg, 3).transpose(0, 2, 3, 1, 4)
    gather_blocks = jax.vmap(jax.vmap(lambda kv, ix: kv[ix]))
    garr = jnp.arange(G)[None, :, None, None, None]
    jblk = jnp.arange(n_sel)

    def block(b):
        q0 = b * Tq
        t = q0 + jnp.arange(Tq)
        qb = lax.dynamic_slice_in_dim(qg, q0, Tq, axis=3)
        cvalid = cmp_end[None, :] <= t[:, None]
        cbias = tbl[t5_bucket(t[:, None] - cmp_end[None, :])].transpose(2, 3, 0, 1)
        lc = jnp.einsum('bghtd,bgcd->bghtc', qb, kc).astype(jnp.float32) * scale + cbias
        pc = jax.nn.softmax(jnp.where(cvalid, lc, NEG), axis=-1) * cvalid
        o_cmp = jnp.einsum('bghtc,bgcd->bghtd', pc.astype(vc.dtype), vc)
        imp = jnp.einsum('bghtc,cj->bgtj', pc, overlap)
        cur = t // SEL_BLOCK
        forced = (jblk[None, :] == 0) | (jblk[None, :] == cur[:, None]) | (jblk[None, :] == cur[:, None] - 1)
        svalid = jblk[None, :] * SEL_BLOCK <= t[:, None]
        score = jnp.where(forced, BIG, jnp.where(svalid, imp, -BIG))
        _, sel = lax.top_k(score, top_n)
        kb = gather_blocks(ks, sel)
        vb = gather_blocks(vs, sel).reshape(B, G, Tq, top_n * SEL_BLOCK, dh)
        pos = sel[..., None] * SEL_BLOCK + jnp.arange(SEL_BLOCK)
        dist = t[None, None, :, None, None] - pos
        sbias = jnp.moveaxis(tbl[t5_bucket(dist), garr], -1, 2).reshape(B, G, hpg, Tq, top_n * SEL_BLOCK)
        ls = (jnp.einsum('bghtd,bgtnkd->bghtnk', qb, kb).reshape(B, G, hpg, Tq, top_n * SEL_BLOCK)
              .astype(jnp.float32) * scale + sbias)
        smask = (dist >= 0).reshape(B, G, Tq, top_n * SEL_BLOCK)[:, :, None]
        ps = jax.nn.softmax(jnp.where(smask, ls, NEG), axis=-1)
        o_slc = jnp.einsum('bghtm,bgtmd->bghtd', ps.astype(vb.dtype), vb)
        kwb = lax.dynamic_slice_in_dim(kw, q0, wk, axis=2)
        vwb = lax.dynamic_slice_in_dim(vw, q0, wk, axis=2)
        kpos = q0 - WINDOW + jnp.arange(wk)
        wmask = win_band & (kpos[None, :] >= 0)
        lw = jnp.einsum('bghtd,bgkd->bghtk', qb, kwb).astype(jnp.float32) * scale + win_bias
        pw = jax.nn.softmax(jnp.where(wmask, lw, NEG), axis=-1)
        o_win = jnp.einsum('bghtk,bgkd->bghtd', pw.astype(vwb.dtype), vwb)
        g = lax.dynamic_slice_in_dim(gts, q0, Tq, axis=3)
        return g[..., 0:1] * o_cmp + g[..., 1:2] * o_slc + g[..., 2:3] * o_win

    out = lax.map(block, jnp.arange(S // Tq))
    return out.transpose(1, 0, 4, 2, 3, 5).reshape(B, S, H * dh)


def moe_ffn(h, w_router, b_router, w_gate, b_gate, w_up, b_up, w_down, b_down):
    B, S, D = h.shape
    xt = h.reshape(-1, D)
    T = xt.shape[0]
    logits = (xt @ w_router).astype(jnp.float32) + b_router
    top_v, top_i = lax.top_k(logits, TOP_K)
    top_w = jax.nn.softmax(top_v, axis=-1)
    A = T * TOP_K
    flat_e = top_i.reshape(-1)
    order = jnp.argsort(flat_e)
    sorted_e = flat_e[order]
    token_of = order // TOP_K
    counts = jax.ops.segment_sum(jnp.ones_like(flat_e), flat_e, num_segments=N_EXPERTS)
    padded = ((counts + MOE_BLOCK - 1) // MOE_BLOCK) * MOE_BLOCK
    pad_end = jnp.cumsum(padded)
    pad_start = pad_end - padded
    grp_start = jnp.cumsum(counts) - counts
    dest = pad_start[sorted_e] + jnp.arange(A) - grp_start[sorted_e]
    n_blocks = -(-A // MOE_BLOCK) + N_EXPERTS
    P = n_blocks * MOE_BLOCK
    xs = jnp.zeros((P, D), h.dtype).at[dest].set(xt[token_of])
    blk_e = jnp.minimum(jnp.searchsorted(pad_end, jnp.arange(n_blocks) * MOE_BLOCK, side='right'),
                        N_EXPERTS - 1)

    def expert_block(args):
        xb, e = args
        gt = jnp.minimum(xb @ w_gate[e] + b_gate[e], SWIGLU_LIMIT)
        up = jnp.clip(xb @ w_up[e] + b_up[e], -SWIGLU_LIMIT, SWIGLU_LIMIT)
        glu = gt * jax.nn.sigmoid(SWIGLU_ALPHA * gt)
        return (glu * (up + 1.0)) @ w_down[e] + b_down[e]

    ys = lax.map(expert_block, (xs.reshape(n_blocks, MOE_BLOCK, D), blk_e)).reshape(P, D)
    w_sorted = top_w.reshape(-1)[order]
    contrib = ys[dest] * w_sorted[:, None].astype(ys.dtype)
    return jax.ops.segment_sum(contrib, token_of, num_segments=T).reshape(B, S, D)


def setup_inputs(seed: int = 0) -> dict:
    key = jax.random.key(seed)
    ks = jax.random.split(key, 32)
    f32 = jnp.float32
    L, D, E, F = DEPTH, D_MODEL, N_EXPERTS, D_EXPERT

    def nrm(k, shape, s):
        return jax.random.normal(k, shape, f32) * s

    return {
        "x": nrm(ks[0], (BATCH, SEQ, D), 1.0),
        "c": nrm(ks[1], (BATCH, D), 1.0),
        "w_ada": nrm(ks[2], (L, D, 6 * D), 0.5 * D ** -0.5),
        "b_ada": nrm(ks[3], (L, 6 * D), 0.02),
        "g_mix_pre": 1.0 + nrm(ks[4], (L, D), 0.05),
        "g_mix_post": 1.0 + nrm(ks[5], (L, D), 0.05),
        "w_in": nrm(ks[6], (L, D, IN_WIDTH), D ** -0.5),
        "b_forget": jax.random.uniform(ks[7], (L, FOX_HEADS), f32, 3.0, 6.0),
        "pe_k": nrm(ks[8], (L, CMP_BLOCK, HEAD_DIM), 0.5),
        "pe_v": nrm(ks[9], (L, CMP_BLOCK, HEAD_DIM), 0.5),
        "w_cmp_k": nrm(ks[10], (L, CMP_BLOCK * HEAD_DIM, HEAD_DIM), (CMP_BLOCK * HEAD_DIM) ** -0.5),
        "w_cmp_v": nrm(ks[11], (L, CMP_BLOCK * HEAD_DIM, HEAD_DIM), (CMP_BLOCK * HEAD_DIM) ** -0.5),
        "w_fox_proj": nrm(ks[12], (L, FOX_WIDTH, D), FOX_WIDTH ** -0.5),
        "w_nsa_proj": nrm(ks[13], (L, NSA_WIDTH, D), NSA_WIDTH ** -0.5),
        "w_mix_out": nrm(ks[14], (L, D, D), D ** -0.5),
        "rel_bias": nrm(ks[15], (REL_BUCKETS, NSA_HEADS), 0.5),
        "g_ffn_pre": 1.0 + nrm(ks[16], (L, D), 0.05),
        "g_ffn_post": 1.0 + nrm(ks[17], (L, D), 0.05),
        "w_router": nrm(ks[18], (L, D, E), D ** -0.5),
        "b_router": nrm(ks[19], (L, E), 0.01),
        "w_gate": nrm(ks[20], (L, E, D, F), D ** -0.5),
        "b_gate": nrm(ks[21], (L, E, F), 0.01),
        "w_up": nrm(ks[22], (L, E, D, F), D ** -0.5),
        "b_up": nrm(ks[23], (L, E, F), 0.01),
        "w_down": nrm(ks[24], (L, E, F, D), F ** -0.5),
        "b_down": nrm(ks[25], (L, E, D), 0.01),
    }


def reference(x, c, w_ada, b_ada, g_mix_pre, g_mix_post, w_in, b_forget, pe_k, pe_v,
              w_cmp_k, w_cmp_v, w_fox_proj, w_nsa_proj, w_mix_out, rel_bias,
              g_ffn_pre, g_ffn_post, w_router, b_router, w_gate, b_gate, w_up, b_up, w_down, b_down):
    B, S, D = x.shape
    offs = []
    acc = 0
    for s in IN_SIZES[:-1]:
        acc += s
        offs.append(acc)
    for l in range(DEPTH):
        ada = (jax.nn.silu(c) @ w_ada[l] + b_ada[l])[:, None, :]
        sh1, sc1, ga1, sh2, sc2, ga2 = jnp.split(ada, 6, axis=-1)
        h = rms_norm(x, g_mix_pre[l]) * (1.0 + sc1) + sh1
        proj = h @ w_in[l]
        (fq, fk, fv, ff, nq, kcm, vcm, ksl, vsl, kwn, vwn, ng, mg_fox, mg_nsa) = jnp.split(proj, offs, axis=-1)
        log_f = jax.nn.log_sigmoid(ff.astype(jnp.float32) + b_forget[l])
        hd = lambda t, n: t.reshape(B, S, n, HEAD_DIM)
        y_fox = fox_attention(hd(fq, FOX_HEADS), hd(fk, FOX_HEADS), hd(fv, FOX_HEADS), log_f) @ w_fox_proj[l]
        y_nsa = nsa_attention(hd(nq, NSA_HEADS), hd(kcm, NSA_KV_GROUPS), hd(vcm, NSA_KV_GROUPS),
                              hd(ksl, NSA_KV_GROUPS), hd(vsl, NSA_KV_GROUPS),
                              hd(kwn, NSA_KV_GROUPS), hd(vwn, NSA_KV_GROUPS),
                              jax.nn.sigmoid(ng).reshape(B, S, NSA_HEADS, 3),
                              pe_k[l], pe_v[l], w_cmp_k[l], w_cmp_v[l], rel_bias) @ w_nsa_proj[l]
        mixed = (jax.nn.sigmoid(mg_fox) * y_fox + jax.nn.sigmoid(mg_nsa) * y_nsa) @ w_mix_out[l]
        x = x + ga1 * rms_norm(mixed, g_mix_post[l])
        h = rms_norm(x, g_ffn_pre[l]) * (1.0 + sc2) + sh2
        y = moe_ffn(h, w_router[l], b_router[l], w_gate[l], b_gate[l], w_up[l], b_up[l], w_down[l], b_down[l])
        x = x + ga2 * rms_norm(y, g_ffn_post[l])
    return x
```

```python
import math
from contextlib import ExitStack
import numpy as np
import concourse.bass as bass
import concourse.mybir as mybir
from concourse.bass_utils import run_bass_kernel_spmd

F32 = mybir.dt.float32
BF16 = mybir.dt.bfloat16
AF = mybir.ActivationFunctionType
ALU = mybir.AluOpType
AX = mybir.AxisListType

PE, ACT, DVE, POOL, SP = "tensor", "scalar", "vector", "gpsimd", "sync"
ENGS = (PE, ACT, DVE, POOL, SP)
NEGM = -30000.0
BIG = 1e9


class Buf:
    __slots__ = ("t", "name", "lw", "rd", "sem", "ndma", "multi", "psum")

    def __init__(self, t, name, base=None):
        self.t = t
        self.name = name
        self.lw = dict(base) if base else {}
        self.rd = {}
        self.sem = None
        self.ndma = 0
        self.multi = False
        self.psum = False

    def __getitem__(self, idx):
        return self.t[idx]


class Prog:
    def __init__(self, nc):
        self.nc = nc
        self.es = ExitStack()
        self.scopes = []
        self.q = {e: [] for e in ENGS}
        self.esem = {}
        self.pesems = set()
        self.ecnt = {e: 0 for e in ENGS}
        self.known = {e: {} for e in ENGS}
        self.base = {}
        self.nsem = 0
        self.sem_eng = {}
        for e in (PE, ACT, DVE, POOL):
            self._newsem(e)
        self.nbuf = 0

    def _sem(self, name):
        self.nsem += 1
        return self.es.enter_context(self.nc.semaphore(f"{name}_{self.nsem}"))

    def _newsem(self, e):
        s = self._sem("s_" + e)
        self.esem[e] = s
        self.sem_eng[s] = e
        self.ecnt[e] = 0
        if e == PE:
            self.pesems.add(s)

    class _Scope:
        def __init__(self, P):
            self.P = P
            self.es = ExitStack()
            self.bufs = []

        def __enter__(self):
            self.P.scopes.append(self)
            return self

        def __exit__(self, *a):
            P = self.P
            P.scopes.pop()
            for b in self.bufs:
                for d in (b.lw, b.rd):
                    for s, v in d.items():
                        if P.base.get(s, 0) < v:
                            P.base[s] = v
            self.es.close()
            return False

    def scope(self):
        return Prog._Scope(self)

    def _reg(self, cm, name):
        sc = self.scopes[-1] if self.scopes else None
        t = (sc.es if sc else self.es).enter_context(cm)
        b = Buf(t, name, self.base)
        if sc:
            sc.bufs.append(b)
        return b

    def sbuf(self, shape, dt, name):
        self.nbuf += 1
        name = f"{name}_{self.nbuf}"
        return self._reg(self.nc.sbuf_tensor(name, list(shape), dt), name)

    def psum(self, shape, dt, name):
        self.nbuf += 1
        name = f"{name}_{self.nbuf}"
        esz = 2 if dt == BF16 else 4
        full = 2048 // esz
        b = self._reg(self.nc.psum_tensor(name, [128, full], dt), name)
        n = 1
        for d_ in shape[1:]:
            n *= d_
        assert n <= full
        v = b.t[0:shape[0], 0:n]
        if len(shape) == 3:
            v = v.rearrange("p (a b) -> p a b", b=shape[2])
        b.t = v
        b.psum = True
        return b

    def dram(self, name, shape, dt, kind="Internal"):
        t = self.nc.dram_tensor(name, list(shape), dt, kind=kind)
        b = Buf(t.ap(), name)
        b.multi = True
        return b

    def _dsem(self, b):
        if b.sem is None or b.ndma >= 1500:
            b.sem = self._sem("d")
            b.ndma = 0
        return b.sem

    def _deps(self, eng, reads, writes):
        waits = {}

        def add(d):
            for s, v in d.items():
                if waits.get(s, 0) < v:
                    waits[s] = v
        for b in reads:
            add(b.lw)
            if b.psum:
                add({s_: v_ for s_, v_ in b.rd.items() if self.sem_eng.get(s_) != eng})
        for b in writes:
            if not b.multi:
                add(b.lw)
            add(b.rd)
        kn = self.known[eng]
        out = []
        for s, v in waits.items():
            if eng == PE and s in self.pesems:
                continue
            if kn.get(s, 0) >= v:
                continue
            kn[s] = v
            out.append((s, v))
        return out

    def _commit(self, tok, reads, writes):
        s, v = tok
        for b in reads:
            if b.rd.get(s, 0) < v:
                b.rd[s] = v
        for b in writes:
            if b.multi:
                if b.lw.get(s, 0) < v:
                    b.lw[s] = v
            else:
                b.lw = {s: v}
                b.rd = {}

    def op(self, eng, fn, reads=(), writes=()):
        waits = self._deps(eng, reads, writes)
        if self.ecnt[eng] >= 30000:
            self._newsem(eng)
        self.ecnt[eng] += 1
        tok = (self.esem[eng], self.ecnt[eng])
        self.q[eng].append((waits, fn, tok[0], 1))
        self._commit(tok, reads, writes)

    def dma(self, eng, out_b, out_ap, in_b, in_ap, sem_buf=None, **kw):
        sb = sem_buf or out_b
        waits = self._deps(eng, [in_b], [out_b])
        sem = self._dsem(sb)
        sb.ndma += 1
        tok = (sem, 16 * sb.ndma)
        self.q[eng].append((waits, lambda e: e.dma_start(out=out_ap, in_=in_ap, **kw), sem, 16))
        self._commit(tok, [in_b], [out_b])

    def wait_all(self, eng, bufs):
        waits = self._deps(eng, [], bufs)
        self.q[eng].append((waits, None, None, 0))

    def mm(self, ob, o, lb, l, rb, r, start=True, stop=True, extra=()):
        self.op(PE, lambda e: e.matmul(o, lhsT=l, rhs=r, start=start, stop=stop, skip_group_check=True),
                [lb, rb] + list(extra), [ob])

    def tr(self, ob, o, ib, i, idb):
        self.op(PE, lambda e: e.transpose(out=o, in_=i, identity=idb[:]), [ib, idb], [ob])

    def act(self, ob, o, ib, i, func, bias=None, scale=None, extra=(), eng=ACT):
        kw = {}
        if bias is not None:
            kw["bias"] = bias
        if scale is not None:
            kw["scale"] = scale
        self.op(eng, lambda e: e.activation(out=o, in_=i, func=func, **kw), [ib] + list(extra), [ob])

    def ts(self, eng, ob, o, ib, i, s1, s2, op0, op1=None, extra=()):
        if op1 is None:
            self.op(eng, lambda e: e.tensor_scalar(out=o, in0=i, scalar1=s1, scalar2=None, op0=op0),
                    [ib] + list(extra), [ob])
        else:
            self.op(eng, lambda e: e.tensor_scalar(out=o, in0=i, scalar1=s1, scalar2=s2, op0=op0, op1=op1),
                    [ib] + list(extra), [ob])

    def tt(self, eng, ob, o, ab, a, bb, b, op):
        self.op(eng, lambda e: e.tensor_tensor(out=o, in0=a, in1=b, op=op), [ab, bb], [ob])

    def stt(self, ob, o, ab, a, sc, bb, b, op0, op1, extra=()):
        self.op(DVE, lambda e: e.scalar_tensor_tensor(out=o, in0=a, scalar=sc, in1=b, op0=op0, op1=op1),
                [ab, bb] + list(extra), [ob])

    def cp(self, eng, ob, o, ib, i):
        if eng == ACT:
            self.op(ACT, lambda e: e.copy(out=o, in_=i), [ib], [ob])
        else:
            self.op(eng, lambda e: e.tensor_copy(out=o, in_=i), [ib], [ob])

    def call(self, eng, name, reads, writes, **kw):
        self.op(eng, lambda e: getattr(e, name)(**kw), reads, writes)

    def memset(self, eng, ob, o, val):
        self.op(eng, lambda e: e.memset(o, val), [], [ob])

    def emit(self):
        nc = self.nc
        with nc.Block() as block:
            for e in ENGS:
                ops = self.q[e]

                def body(eh, ops=ops):
                    for waits, fn, sem, inc in ops:
                        for s, v in waits:
                            eh.wait_ge(s, v)
                        if fn is not None:
                            fn(eh).then_inc(sem, inc)
                getattr(block, e)(body)
        self.es.close()


OFF = dict(fq=0, fk=512, fv=1024, ff=1536, nq=1544, kcm=2056, vcm=2184, ksl=2312, vsl=2440, kwn=2568,
           vwn=2696, ng=2824, mgf=2848, mgn=3872)
NF, NTK, NSM, NG = 1920, 896, 32, 2048
RMS_EPS = 1e-6


def w_in_perm():
    r = lambda a, n: list(range(OFF[a], OFF[a] + n))
    return np.array(r("fq", 512) + r("fk", 512) + r("nq", 512) + r("kcm", 128) + r("ksl", 128) + r("kwn", 128)
                    + r("fv", 512) + r("vcm", 128) + r("vsl", 128) + r("vwn", 128)
                    + r("ff", 8) + r("ng", 24) + r("mgf", 1024) + r("mgn", 1024))


def build(S=4096, E=32, dbg=False, phases=5):
    NT, NCH, NSEL = S // 128, S // 512, S // 64
    NCMP = (S - 32) // 16 + 1
    NCT = (NCMP + 127) // 128
    TOPN = min(16, NSEL)
    D = 1024
    nc = bass.Bass("TRN2", target_bir_lowering=False)
    P = Prog(nc)

    def din(name, shape, dt=F32):
        return Buf(nc.dram_tensor(name, list(shape), dt, kind="ExternalInput").ap(), name)

    x = din("x", [S, D]); cT = din("cT", [128, 8]); w_ada = din("w_ada", [D, 6 * D]); b_ada = din("b_ada", [1, 6 * D])
    gpre1 = din("gpre1", [128, 8]); gpre2 = din("gpre2", [128, 8])
    gpost1 = din("gpost1", [128, D]); gpost2 = din("gpost2", [128, D])
    w_in = din("w_in", [D, 4896]); bfg = din("bfg", [128, NT * 8])
    pekT = din("pekT", [64, 32]); pevT = din("pevT", [64, 32])
    wck = din("wck", [2048, 64]); wcv = din("wcv", [2048, 64])
    wfp = din("wfp", [512, D]); wnp = din("wnp", [512, D]); wmo = din("wmo", [D, D])
    winG = din("winG", [8, 9, 128, 512]); cmpG = din("cmpG", [8, 5, 128, 512]); farc = din("farc", [128, 8])
    winM = din("winM", [9, 128, 512]); cmpM = din("cmpM", [5, 128, 512]); caus = din("caus", [4, 128, 512])
    ovl = din("ovl", [NCT * 128, NSEL + 1]); selA = din("selA", [128, NT * NSEL]); selV = din("selV", [128, NT * NSEL])
    Xp = din("Xp", [NSEL, NT * 128]); tri = din("tri", [128, 128])
    w_router = din("w_router", [D, 32]); b_router = din("b_router", [128, 32])
    w_gate = din("w_gate", [E, 8, 128, D]); w_up = din("w_up", [E, 8, 128, D]); w_down = din("w_down", [E, D, D])
    bgT = din("bgT", [128, E * 8]); buT = din("buT", [128, E * 8]); bdn = din("bdn", [1, E * D])
    okind = "ExternalOutput"
    out = Buf(nc.dram_tensor("out", [S, D], F32, kind=okind).ap(), "out")
    out.multi = True
    sk = okind if dbg else "Internal"
    featT = P.dram("featT", [NF, S], BF16, sk)
    vtok = P.dram("vtok", [S, NTK], BF16, sk)
    hTs = P.dram("hTs", [128, 8, S], BF16, sk)
    h2Ts = P.dram("h2Ts", [128, 8, S], BF16, sk)
    x1s = P.dram("x1s", [S, D], F32, sk)
    if dbg:
        dsm = P.dram("dsm", [128, NT * 32], F32, okind)
        dL = P.dram("dL", [128, NT * 8], F32, okind)
        dfb = P.dram("dfb", [128, 8 * NCH * NT], F32, okind)
        dcar = P.dram("dcar", [128, (NT + 1) * 8], F32, okind)
        dyn = P.dram("dyn", [128, NT * 512], BF16, okind)
        drw = P.dram("drw", [128, NT * 32], F32, okind)

    identb = P.sbuf([128, 128], BF16, "identb"); identf = P.sbuf([128, 128], F32, "identf")
    ones_r = P.sbuf([1, 128], F32, "ones_r"); ones_rb = P.sbuf([1, 128], BF16, "ones_rb")
    onesm = P.sbuf([128, 128], F32, "onesm")
    A1 = P.sbuf([128, 8], F32, "A1"); B1 = P.sbuf([128, 8], F32, "B1")
    A2 = P.sbuf([128, 8], F32, "A2"); B2 = P.sbuf([128, 8], F32, "B2")
    G1 = P.sbuf([128, D], F32, "G1"); G2 = P.sbuf([128, D], F32, "G2")
    small = P.sbuf([128, NT, 32], F32, "small")
    rw = P.sbuf([128, NT, 32], F32, "rw")
    zcol = P.sbuf([128, 1], F32, "zcol"); farcs = P.sbuf([128, 8], F32, "farcs")

    P.memset(POOL, identf, identf[:], 0.0)
    P.op(POOL, lambda e: e.affine_select(out=identf[:], in_=identf[:], pattern=[[-1, 128]], compare_op=ALU.not_equal,
                                          fill=1.0, base=0, channel_multiplier=1), [identf], [identf])
    P.cp(DVE, identb, identb[:], identf, identf[:])
    P.memset(DVE, ones_r, ones_r[:], 1.0); P.memset(DVE, ones_rb, ones_rb[:], 1.0)
    P.memset(POOL, onesm, onesm[:], 1.0); P.memset(DVE, zcol, zcol[:], 0.0)
    P.dma(SP, farcs, farcs[:], farc, farc[:, :])

    with P.scope():
        cs = P.sbuf([128, 8], F32, "cs"); scs = P.sbuf([128, 8], F32, "scs")
        arow = P.sbuf([1, 6 * D], F32, "arow"); brow = P.sbuf([1, 6 * D], F32, "brow")
        gp1 = P.sbuf([128, 8], F32, "gp1"); gp2 = P.sbuf([128, 8], F32, "gp2")
        wa = [P.sbuf([128, 8, 512], F32, f"wa{i}") for i in range(2)]
        psr = [P.psum([1, 512], F32, f"psr{i}") for i in range(2)]
        psT = P.psum([128, 48], F32, "psT"); psb = [P.psum([128, 512], F32, f"psb{i}") for i in range(2)]
        gpo = P.sbuf([128, D], F32, "gpo")
        P.dma(SP, cs, cs[:], cT, cT[:, :]); P.dma(SP, brow, brow[:], b_ada, b_ada[:, :])
        P.dma(SP, gp1, gp1[:], gpre1, gpre1[:, :]); P.dma(SP, gp2, gp2[:], gpre2, gpre2[:, :])
        P.act(scs, scs[:], cs, cs[:], AF.Silu)
        wav = w_ada.t.rearrange("(k p) n -> p k n", p=128)
        for blk in range(12):
            w_ = wa[blk % 2]; pr = psr[blk % 2]
            P.dma(SP, w_, w_[:], w_ada, wav[:, :, blk * 512:(blk + 1) * 512])
            for k in range(8):
                P.mm(pr, pr[0:1, :], scs, scs[:, k:k + 1], w_, w_[:, k, :], start=(k == 0), stop=(k == 7))
            P.tt(DVE, arow, arow[0:1, blk * 512:(blk + 1) * 512], pr, pr[0:1, :], brow, brow[0:1, blk * 512:(blk + 1) * 512], ALU.add)
        for j in list(range(0, 16)) + list(range(24, 40)):
            P.mm(psT, psT[:, j:j + 1], arow, arow[0:1, j * 128:(j + 1) * 128], ones_r, ones_r[0:1, 0:1])
        P.stt(A1, A1[:], psT, psT[:, 8:16], 1.0, gp1, gp1[:], ALU.add, ALU.mult)
        P.cp(DVE, B1, B1[:], psT, psT[:, 0:8])
        P.stt(A2, A2[:], psT, psT[:, 32:40], 1.0, gp2, gp2[:], ALU.add, ALU.mult)
        P.cp(DVE, B2, B2[:], psT, psT[:, 24:32])
        for (Gx, gsrc, c0) in ((G1, gpost1, 2048), (G2, gpost2, 5120)):
            P.dma(SP, gpo, gpo[:], gsrc, gsrc[:, :])
            for i in range(2):
                pb = psb[i]
                P.mm(pb, pb[:], ones_r, ones_r[0:1, :], arow, arow[0:1, c0 + i * 512:c0 + (i + 1) * 512])
                P.tt(DVE, Gx, Gx[:, i * 512:(i + 1) * 512], pb, pb[:], gpo, gpo[:, i * 512:(i + 1) * 512], ALU.mult)

    def rstd_of(src_b, src_ap, junk, ss, rstd):
        P.act(junk, junk[:], src_b, src_ap, AF.Square)
        P.call(DVE, "reduce_sum", [junk], [ss], out=ss[:], in_=junk[:], axis=AX.X)
        P.ts(DVE, ss, ss[:], ss, ss[:], 1.0 / D, RMS_EPS, ALU.mult, ALU.add)
        P.act(ss, ss[:], ss, ss[:], AF.Sqrt)
        P.call(DVE, "reciprocal", [ss], [rstd], out=rstd[:], in_=ss[:])

    bcscope = P.scope(); bcscope.__enter__()
    Lc = P.sbuf([128, NT, 8], F32, "Lc"); carry = P.sbuf([128, NT + 1, 8], F32, "carry")
    fbias = P.sbuf([128, 8, NCH, NT], F32, "fbias")
    with P.scope():
        NW = NF + NTK + NSM
        wB = P.sbuf([128, 8, NW], BF16, "wB")
        wBk = [Buf(wB.t, f"wBk{k}", P.base) for k in range(8)]
        stg = [P.sbuf([128, NW // 2], F32, f"stg{i}") for i in range(2)]
        wv = w_in.t.rearrange("(k p) n -> p k n", p=128)
        i = 0
        for k in range(8):
            for hf in range(2):
                st = stg[i % 2]
                P.dma(SP, st, st[:], w_in, wv[:, k, hf * (NW // 2):(hf + 1) * (NW // 2)])
                P.cp(POOL if i % 2 else DVE, wBk[k], wB[:, k, hf * (NW // 2):(hf + 1) * (NW // 2)], st, st[:])
                i += 1
        xin = [P.sbuf([128, D], F32, f"xin{i}") for i in range(2)]
        junk = P.sbuf([128, D], F32, "junk"); ss = P.sbuf([128, 1], F32, "ss"); rstd = P.sbuf([128, 1], F32, "rstd")
        xn = [P.sbuf([128, D], BF16, f"xn{i}") for i in range(2)]
        ptr = [P.psum([128, D], BF16, f"ptr{i}") for i in range(2)]
        hT = [P.sbuf([128, 8, 512], BF16, f"hT{i}") for i in range(2)]
        psf = [P.psum([128, 512], F32, f"psf{i}") for i in range(3)]
        fst = [P.sbuf([128, 512], BF16, f"fst{i}") for i in range(3)]
        vst = [P.sbuf([128, NTK], BF16, f"vst{i}") for i in range(2)]
        ci = 0

        def prep(c):
            h_ = hT[c % 2]
            for tt in range(4):
                n = 4 * c + tt
                xi = xin[n % 2]; xb = xn[n % 2]; pt = ptr[n % 2]
                P.dma(SP, xi, xi[:], x, x[n * 128:(n + 1) * 128, :])
                rstd_of(xi, xi[:], junk, ss, rstd)
                yield
                P.ts(DVE, xb, xb[:], xi, xi[:], rstd[:, 0:1], None, ALU.mult, extra=[rstd])
                for k in range(8):
                    P.tr(pt, pt[:, k * 128:(k + 1) * 128], xb, xb[:, k * 128:(k + 1) * 128], identb)
                yield
                for k in range(8):
                    P.act(h_, h_[:, k, tt * 128:(tt + 1) * 128], pt, pt[:, k * 128:(k + 1) * 128], AF.Identity,
                          bias=B1[:, k:k + 1], scale=A1[:, k:k + 1], extra=[A1, B1])
                yield
            P.dma(POOL, hTs, hTs[:, :, c * 512:(c + 1) * 512], h_, h_[:], sem_buf=h_)

        def mms(c):
            nonlocal ci
            h_ = hT[c % 2]
            for fb in range(NF // 128):
                ps = psf[ci % 3]; fs = fst[ci % 3]; ci += 1
                for k in range(8):
                    P.mm(ps, ps[:], wBk[k], wB[:, k, fb * 128:(fb + 1) * 128], h_, h_[:, k, :], start=(k == 0), stop=(k == 7))
                isq = fb < 4 or 8 <= fb < 12
                if fb % 2:
                    P.act(fs, fs[:], ps, ps[:], AF.Copy, scale=(0.125 if isq else 1.0))
                else:
                    P.ts(DVE, fs, fs[:], ps, ps[:], (0.125 if isq else 1.0), None, ALU.mult)
                P.dma(POOL, featT, featT[fb * 128:(fb + 1) * 128, c * 512:(c + 1) * 512], fs, fs[:], sem_buf=fs)
                yield
            for tt in range(4):
                n = 4 * c + tt
                vs = vst[n % 2]
                ps = psf[ci % 3]; ci += 1
                for k in range(8):
                    P.mm(ps, ps[:], h_, h_[:, k, tt * 128:(tt + 1) * 128], wBk[k], wB[:, k, NF:NF + 512], start=(k == 0), stop=(k == 7))
                P.cp(ACT, vs, vs[:, 0:512], ps, ps[:])
                yield
                ps = psf[ci % 3]; ci += 1
                for k in range(8):
                    P.mm(ps, ps[:, 0:416], h_, h_[:, k, tt * 128:(tt + 1) * 128], wBk[k], wB[:, k, NF + 512:NW], start=(k == 0), stop=(k == 7))
                P.cp(DVE, vs, vs[:, 512:896], ps, ps[:, 0:384])
                P.cp(DVE, small, small[:, n, :], ps, ps[:, 384:416])
                P.dma(POOL, vtok, vtok[n * 128:(n + 1) * 128, :], vs, vs[:], sem_buf=vs)
                yield

        for _ in prep(0):
            pass
        for c in range(NCH):
            gens = [mms(c)] + ([prep(c + 1)] if c + 1 < NCH else [])
            live = [True] * len(gens)
            while any(live):
                for gi_ in range(len(gens)):
                    if live[gi_]:
                        try:
                            next(gens[gi_])
                        except StopIteration:
                            live[gi_] = False

        bfs = P.sbuf([128, NT, 8], F32, "bfs"); sp = P.sbuf([128, NT, 8], F32, "sp")
        tris = P.sbuf([128, 128], F32, "tris"); tots = P.sbuf([128, NT, 8], F32, "tots")
        pcw = P.psum([128, NT * 8], F32, "pcw"); ptot = P.psum([128, NT * 8], F32, "ptot")
        P.dma(SP, bfs, bfs[:], bfg, bfg.t.rearrange("p (n h) -> p n h", h=8))
        P.dma(SP, tris, tris[:], tri, tri[:, :])
        P.tt(DVE, sp, sp[:], small, small[:, :, 0:8], bfs, bfs[:], ALU.add)
        P.act(sp, sp[:], sp, sp[:], AF.Exp, scale=-1.0)
        P.act(sp, sp[:], sp, sp[:], AF.Ln, bias=1.0)
        spf = sp.t.rearrange("p n h -> p (n h)")
        P.mm(pcw, pcw[:], tris, tris[:], sp, spf)
        P.mm(ptot, ptot[:], onesm, onesm[:], sp, spf)
        P.cp(DVE, tots, tots.t.rearrange("p n h -> p (n h)"), ptot, ptot[:])
        P.memset(DVE, carry, carry[:, 0, :], 0.0)
        for n in range(NT):
            P.tt(DVE, carry, carry[:, n + 1, :], carry, carry[:, n, :], tots, tots[:, n, :], ALU.add)
        P.tt(DVE, Lc, Lc.t.rearrange("p n h -> p (n h)"), pcw, pcw[:], carry, carry[:, 0:NT, :].rearrange("p n h -> p (n h)"), ALU.add)
        for h in range(8):
            for c in range(NCH):
                P.ts(DVE, fbias, fbias[:, h, c, :], Lc, Lc[:, :, h], carry[:, 4 * c + 2, h:h + 1], None, ALU.subtract, extra=[carry])
        P.act(small, small[:, :, 8:32], small, small[:, :, 8:32], AF.Sigmoid)
        if dbg:
            P.dma(SP, dsm, dsm[:, :], small, small.t.rearrange("p n h -> p (n h)"), sem_buf=small)
            P.dma(SP, dL, dL[:, :], Lc, Lc.t.rearrange("p n h -> p (n h)"), sem_buf=Lc)
            P.dma(SP, dfb, dfb[:, :], fbias, fbias.t.rearrange("p h c n -> p (h c n)"), sem_buf=fbias)
            P.dma(SP, dcar, dcar[:, :], carry, carry.t.rearrange("p n h -> p (n h)"), sem_buf=carry)

    fin = [featT, vtok, hTs]
    yfs = P.dram("yfs", [128, NT * 512], BF16, sk)

    if phases >= 3:
      with P.scope():
        yfox = P.sbuf([128, NT, 512], BF16, "yfox")
        causb = P.sbuf([128, 4, 512], BF16, "causb")
        cst = P.sbuf([128, 512], F32, "cst")
        for d in range(4):
            P.dma(SP, cst, cst[:], caus, caus[d, :, :])
            P.cp(DVE, causb, causb[:, d, :], cst, cst[:])
        kT = [P.sbuf([128, S], BF16, f"kT{i}") for i in range(2)]
        qA = [P.sbuf([128, S], BF16, f"qA{i}") for i in range(2)]
        qB = [P.sbuf([128, S], BF16, f"qB{i}") for i in range(2)]
        for i in range(2):
            P.memset(POOL, qA[i], qA[i][64:128, :], 0.0); P.memset(POOL, qB[i], qB[i][0:64, :], 0.0)
        V = [P.sbuf([128, NT, 65], BF16, f"V{i}") for i in range(2)]
        for i in range(2):
            P.memset(POOL, V[i], V[i][:, :, 64:65], 1.0)
        psS = [P.psum([128, 512], F32, f"psS{i}") for i in range(4)]
        psO = [P.psum([128, 4, 65], F32, f"psO{i}") for i in range(2)]
        pT = [P.sbuf([128, 512], BF16, f"pT{i}") for i in range(4)]
        rec = P.sbuf([128, 4], F32, "rec")
        si = 0; oi = 0
        pend = []

        def flush(keep=0):
            idx = len(pend)
            if keep:
                starts = [i_ for i_, (st_, _) in enumerate(pend) if st_]
                idx = starts[-keep] if len(starts) >= keep else 0
            for _, f_ in pend[:idx]:
                f_()
            del pend[:idx]

        def fox_pv(O, p_, v_, k, c, first):
            for tt in range(4):
                if k <= 4 * c + tt:
                    P.mm(O, O[:, tt, :], p_, p_[:, tt * 128:(tt + 1) * 128], v_, v_[:, k, :], start=first, stop=(k == 4 * c + tt))
                    first = False

        def fox_norm(O, c, h):
            P.ts(DVE, rec, rec[:], O, O[:, :, 64], 1e-30, None, ALU.add)
            P.call(DVE, "reciprocal", [rec], [rec], out=rec[:], in_=rec[:])
            for tt in range(4):
                P.ts(DVE, yfox, yfox[:, 4 * c + tt, h * 64:(h + 1) * 64], O, O[:, tt, 0:64], rec[:, tt:tt + 1], None, ALU.mult, extra=[rec])

        for h in range(8):
            jp = h // 2; ih = h % 2
            k_, v_ = kT[jp % 2], V[h % 2]
            q_ = (qA if ih == 0 else qB)[jp % 2]
            if ih == 0:
                P.dma(SP, k_, k_[:], featT, featT[512 + jp * 128:512 + (jp + 1) * 128, :])
            P.dma(SP, q_, q_[64 * ih:64 * ih + 64, :], featT, featT[h * 64:(h + 1) * 64, :])
            P.dma(SP, v_, v_[:, :, 0:64], vtok, vtok.t[:, h * 64:(h + 1) * 64].rearrange("(n p) d -> p n d", p=128))
            for c in range(NCH):
                O = psO[oi % 2]; oi += 1
                first = True
                for k in range(4 * c + 4):
                    st = psS[si % len(psS)]; p_ = pT[si % len(pT)]; si += 1
                    diag = k >= 4 * c
                    c0 = 128 * max(0, k - 4 * c)
                    P.mm(st, st[:, c0:512], k_, k_[:, k * 128:(k + 1) * 128], q_, q_[:, c * 512 + c0:(c + 1) * 512], start=True, stop=not diag)
                    if diag:
                        P.mm(st, st[:, c0:512], identb, identb[:], causb, causb[:, k - 4 * c, c0:512], start=False, stop=True)
                    flush(keep=2)
                    P.act(p_, p_[:, c0:512], st, st[:, c0:512], AF.Exp, bias=fbias[:, h, c, k:k + 1], extra=[fbias])
                    pend.append((True, lambda O=O, p_=p_, v_=v_, k=k, c=c, first=first: fox_pv(O, p_, v_, k, c, first)))
                    first = False
                pend.append((False, lambda O=O, c=c, h=h: fox_norm(O, c, h)))
        flush()
        P.dma(POOL, yfs, yfs[:, :], yfox, yfox.t.rearrange("p n f -> p (n f)"), sem_buf=yfox)
        fin.append(yfox)
    bcscope.__exit__(None, None, None)
    if phases >= 3:
        yscope = P.scope(); yscope.__enter__()
        ynsa = P.sbuf([128, NT, 512], BF16, "ynsa")

    if phases >= 3:
      with P.scope():
        kcT = P.sbuf([64, 2, NCT * 128], BF16, "kcT"); Vc = P.sbuf([128, 2, NCT, 65], BF16, "Vc")
        P.memset(POOL, kcT, kcT[:], 0.0); P.memset(POOL, Vc, Vc[:], 0.0)
        P.memset(POOL, Vc, Vc[:, :, :, 64:65], 1.0)
        with P.scope():
            wk_f = P.sbuf([64, 32, 64], F32, "wk_f"); wk_b = P.sbuf([64, 32, 64], BF16, "wk_b")
            wv_b = P.sbuf([64, 32, 64], BF16, "wv_b")
            pe_f = P.sbuf([64, 32], F32, "pe_f"); pek_b = P.sbuf([64, 32], BF16, "pek_b"); pev_b = P.sbuf([64, 32], BF16, "pev_b")
            kcm = P.sbuf([64, S], BF16, "kcm"); vcm = P.sbuf([64, S], BF16, "vcm")
            vtk = P.sbuf([128, NT, 64], BF16, "vtk")
            pc1 = P.psum([64, 256], F32, "pc1"); pc2 = P.psum([128, 64], F32, "pc2"); pc3 = P.psum([128, 64], F32, "pc3")
            pcT = P.psum([64, 128], BF16, "pcT")
            kcon = P.sbuf([64, 1], F32, "kcon"); vcon = P.sbuf([128, 64], F32, "vcon")
            P.dma(SP, wk_f, wk_f[:], wck, wck.t.rearrange("(l d) e -> d l e", d=64)); P.cp(DVE, wk_b, wk_b[:], wk_f, wk_f[:])
            P.dma(SP, wk_f, wk_f[:], wcv, wcv.t.rearrange("(l d) e -> d l e", d=64)); P.cp(DVE, wv_b, wv_b[:], wk_f, wk_f[:])
            P.dma(SP, pe_f, pe_f[:], pekT, pekT[:, :]); P.cp(DVE, pek_b, pek_b[:], pe_f, pe_f[:])
            P.dma(SP, pe_f, pe_f[:], pevT, pevT[:, :]); P.cp(DVE, pev_b, pev_b[:], pe_f, pe_f[:])
            for l in range(32):
                P.mm(pc1, pc1[:, 0:1], wk_b, wk_b[:, l, :], pek_b, pek_b[:, l:l + 1], start=(l == 0), stop=(l == 31))
            P.cp(DVE, kcon, kcon[:], pc1, pc1[:, 0:1])
            pev_rep = P.sbuf([64, 32, 128], BF16, "pev_rep")
            for l in range(32):
                P.ts(DVE, pev_rep, pev_rep[:, l, :], onesm, onesm[0:64, :], pev_b[:, l:l + 1], None, ALU.mult, extra=[pev_b])
            for l in range(32):
                P.mm(pc3, pc3[:], pev_rep, pev_rep[:, l, :], wv_b, wv_b[:, l, :], start=(l == 0), stop=(l == 31))
            P.cp(DVE, vcon, vcon[:], pc3, pc3[:])
            for g in range(2):
                P.dma(SP, kcm, kcm[:], featT, featT[1536 + g * 64:1536 + (g + 1) * 64, :])
                P.dma(SP, vtk, vtk[:], vtok, vtok.t[:, 512 + g * 64:512 + (g + 1) * 64].rearrange("(n p) d -> p n d", p=128))
                for n in range(NT):
                    P.tr(pcT, pcT[:], vtk, vtk[:, n, :], identb)
                    P.cp(ACT, vcm, vcm[:, n * 128:(n + 1) * 128], pcT, pcT[:])
                for ct in range(NCT):
                    nc_ = min(128, NCMP - ct * 128)
                    c0 = ct * 128
                    for l in range(32):
                        P.mm(pc1, pc1[:, 0:nc_], wk_b, wk_b[:, l, :], kcm, kcm[:, 16 * c0 + l:16 * c0 + l + 16 * (nc_ - 1) + 1:16], start=(l == 0), stop=(l == 31))
                    P.act(kcT, kcT[:, g, c0:c0 + nc_], pc1, pc1[:, 0:nc_], AF.Identity, bias=kcon[:, 0:1], extra=[kcon])
                    for l in range(32):
                        P.mm(pc2, pc2[0:nc_, :], vcm, vcm[:, 16 * c0 + l:16 * c0 + l + 16 * (nc_ - 1) + 1:16], wv_b, wv_b[:, l, :], start=(l == 0), stop=(l == 31))
                    P.tt(DVE, Vc, Vc[0:nc_, g, ct, 0:64], pc2, pc2[0:nc_, :], vcon, vcon[0:nc_, :], ALU.add)

        ovb = P.sbuf([128, NCT, NSEL + 1], BF16, "ovb")
        sA = P.sbuf([128, NT, NSEL], BF16, "sA"); sV = P.sbuf([128, NT, NSEL], BF16, "sV")
        Xb = P.sbuf([NSEL, NT, 128], BF16, "Xb")
        with P.scope():
            ovf = P.sbuf([128, NCT, NSEL + 1], F32, "ovf")
            P.dma(SP, ovf, ovf[:], ovl, ovl.t.rearrange("(c p) j -> p c j", p=128)); P.cp(DVE, ovb, ovb[:], ovf, ovf[:])
            sf = P.sbuf([128, NT * NSEL], F32, "sf")
            P.dma(SP, sf, sf[:], selA, selA[:, :]); P.cp(DVE, sA, sA.t.rearrange("p n j -> p (n j)"), sf, sf[:])
            P.dma(SP, sf, sf[:], selV, selV[:, :]); P.cp(DVE, sV, sV.t.rearrange("p n j -> p (n j)"), sf, sf[:])
            Xf = P.sbuf([NSEL, NT * 128], F32, "Xf")
            P.dma(SP, Xf, Xf[:], Xp, Xp[:, :]); P.cp(POOL, Xb, Xb.t.rearrange("j n p -> j (n p)"), Xf, Xf[:])
        ksl = P.sbuf([128, S], BF16, "ksl"); kwn = P.sbuf([128, S], BF16, "kwn")
        kc2 = P.sbuf([128, NCT * 128], BF16, "kc2")
        for g in range(2):
            P.dma(SP, kc2, kc2[64 * g:64 * g + 64, :], kcT, kcT[:, g, :])
        P.dma(SP, ksl, ksl[:], featT, featT[1664:1792, :])
        P.dma(SP, kwn, kwn[:], featT, featT[1792:1920, :])
        Vs = P.sbuf([128, NT, 65], BF16, "Vs"); Vw = P.sbuf([128, NT, 65], BF16, "Vw")
        P.memset(POOL, Vs, Vs[:, :, 64:65], 1.0); P.memset(POOL, Vw, Vw[:, :, 64:65], 1.0)
        wB_ = P.sbuf([128, 4, 6, 512], BF16, "winB"); cB_ = P.sbuf([128, 4, 5, 512], BF16, "cmpB")
        wS_ = P.sbuf([128, 3, 512], BF16, "winS")
        bst = [P.sbuf([128, 512], F32, f"bst{i}") for i in range(2)]
        mst = [P.sbuf([128, 512], F32, f"mst{i}") for i in range(2)]
        q4 = [P.sbuf([128, 4, 512], BF16, f"q4{i}") for i in range(2)]
        psS = [P.psum([128, 512], F32, f"nS{i}") for i in range(3)]
        psO = [P.psum([128, 4, 65], F32, f"nO{i}") for i in range(3)]
        psI = P.psum([128, 4, NSEL + 1], F32, "nI")
        psX = P.psum([NSEL, 512], BF16, "nX")
        pT = [P.sbuf([128, 512], BF16, f"npT{i}") for i in range(4)]
        rec = P.sbuf([128, 4], F32, "nrec"); fac = P.sbuf([128, 4], F32, "nfac")
        imp = P.sbuf([128, 4, NSEL], F32, "imp"); sc_ = P.sbuf([128, NSEL], F32, "score"); sc2 = P.sbuf([128, NSEL], F32, "score2")
        m8 = P.sbuf([128, 16], F32, "m8"); sbt = P.sbuf([128, NSEL], BF16, "sbt")
        selT = P.sbuf([NSEL, 512], BF16, "selT")
        yacc = P.sbuf([128, 4, 4, 64], F32, "yacc")
        si = 0; oi = 0; bi = 0

        def att_tile(k_b, k_ap, q_b, q_ap, extra_mms, bias_ap, bias_b, v_b, v_ap, O, ttmask, first, lastk):
            nonlocal si
            st = psS[si % len(psS)]; p_ = pT[si % len(pT)]; si += 1
            tts = [tt for tt in range(4) if ttmask(tt)]
            c0, c1 = 128 * tts[0], 128 * (tts[-1] + 1)
            P.mm(st, st[:, c0:c1], k_b, k_ap, q_b, q_ap[:, c0:c1], start=True, stop=not extra_mms)
            for i_, (lb, l, rb, r) in enumerate(extra_mms):
                P.mm(st, st[:, c0:c1], lb, l, rb, r[:, c0:c1], start=False, stop=(i_ == len(extra_mms) - 1))
            flush(keep=1)
            P.act(p_, p_[:, c0:c1], st, st[:, c0:c1], AF.Exp, bias=bias_ap, extra=[bias_b])

            def pv(first=first):
                for tt in range(4):
                    if ttmask(tt):
                        P.mm(O, O[:, tt, :], p_, p_[:, tt * 128:(tt + 1) * 128], v_b, v_ap, start=first, stop=lastk(tt))
                        first = False
            pend.append((True, pv))
            return False, p_

        pend = []

        def flush(keep=0):
            idx = len(pend)
            if keep:
                starts = [i_ for i_, (st_, _) in enumerate(pend) if st_]
                idx = starts[-keep] if len(starts) >= keep else 0
            for _, f_ in pend[:idx]:
                f_()
            del pend[:idx]

        def cmp_imp(p_, k, firstI, lastI):
            for tt in range(4):
                P.mm(psI, psI[:, tt, :], p_, p_[:, tt * 128:(tt + 1) * 128], ovb, ovb[:, k, :], start=firstI, stop=lastI)
                firstI = False

        def cmp_norm(O, c, h, hh):
            P.ts(DVE, rec, rec[:], O, O[:, :, 64], 1e-30, None, ALU.add)
            P.call(DVE, "reciprocal", [rec], [rec], out=rec[:], in_=rec[:])
            for tt in range(4):
                n = 4 * c + tt
                P.tt(DVE, fac, fac[:, tt:tt + 1], rec, rec[:, tt:tt + 1], small, small[:, n, 8 + 3 * h:9 + 3 * h], ALU.mult)
                P.ts(DVE, yacc, yacc[:, tt, hh, :], O, O[:, tt, 0:64], fac[:, tt:tt + 1], None, ALU.mult, extra=[fac])
                if hh == 0:
                    P.ts(DVE, imp, imp[:, tt, :], psI, psI[:, tt, 0:NSEL], rec[:, tt:tt + 1], None, ALU.mult, extra=[rec])
                else:
                    P.stt(imp, imp[:, tt, :], psI, psI[:, tt, 0:NSEL], rec[:, tt:tt + 1], imp, imp[:, tt, :], ALU.mult, ALU.add, extra=[rec])

        def sw_norm(O, Ow, c, h, hh):
            for (Ox, gi) in ((O, 1), (Ow, 2)):
                P.ts(DVE, rec, rec[:], Ox, Ox[:, :, 64], 1e-30, None, ALU.add)
                P.call(DVE, "reciprocal", [rec], [rec], out=rec[:], in_=rec[:])
                for tt in range(4):
                    n = 4 * c + tt
                    P.tt(DVE, fac, fac[:, tt:tt + 1], rec, rec[:, tt:tt + 1], small, small[:, n, 8 + 3 * h + gi:9 + 3 * h + gi], ALU.mult)
                    P.stt(yacc, yacc[:, tt, hh, :], Ox, Ox[:, tt, 0:64], fac[:, tt:tt + 1], yacc, yacc[:, tt, hh, :], ALU.mult, ALU.add, extra=[fac])
            for tt in range(4):
                P.cp(POOL, ynsa, ynsa[:, 4 * c + tt, h * 64:(h + 1) * 64], yacc, yacc[:, tt, hh, :])

        for v in range(3):
            m1 = mst[v % 2]
            P.dma(SP, m1, m1[:], winM, winM[v, :, :]); P.cp(POOL, wS_, wS_[:, v, :], m1, m1[:])
        for g in range(2):
            for i in range(2):
                P.memset(POOL, q4[i], q4[i][:], 0.0)
            P.dma(SP, Vs, Vs[:, :, 0:64], vtok, vtok.t[:, 640 + g * 64:640 + (g + 1) * 64].rearrange("(n p) d -> p n d", p=128))
            P.dma(SP, Vw, Vw[:, :, 0:64], vtok, vtok.t[:, 768 + g * 64:768 + (g + 1) * 64].rearrange("(n p) d -> p n d", p=128))
            for hh in range(4):
                h = 4 * g + hh
                for (G_, M_, B_, vs_) in ((winG, winM, wB_, (3, 4, 5, 6, 7, 8)), (cmpG, cmpM, cB_, (0, 1, 2, 3, 4))):
                    for vi, v in enumerate(vs_):
                        b1 = bst[bi % 2]; m1 = mst[bi % 2]; bi += 1
                        P.dma(SP, b1, b1[:], G_, G_[h, v, :, :]); P.dma(SP, m1, m1[:], M_, M_[v, :, :])
                        P.tt(POOL, B_, B_[:, hh, vi, :], b1, b1[:], m1, m1[:], ALU.add)
            for c in range(NCH):
                q_ = q4[c % 2]
                P.dma(SP, q_, q_[64 * g:64 * g + 64, :, :], featT, featT.t[1024 + g * 256:1024 + (g + 1) * 256, c * 512:(c + 1) * 512].rearrange("(h d) t -> d h t", d=64))
                for hh in range(4):
                    h = 4 * g + hh
                    O = psO[oi % len(psO)]; oi += 1
                    first = True; firstI = True
                    kts = [k for k in range(NCT) if c - 4 * k >= 0]
                    for k in kts:
                        r = c - 4 * k
                        if r <= 4:
                            ex = [(identb, identb[:], cB_, cB_[:, hh, r, :])]; bap = zcol[:, 0:1]; bb = zcol
                        else:
                            ex = []; bap = farcs[:, h:h + 1]; bb = farcs
                        first, p_ = att_tile(kc2, kc2[:, k * 128:(k + 1) * 128], q_, q_[:, hh, :], ex, bap, bb,
                                             Vc, Vc[:, g, k, :], O, lambda tt: True, first, lambda tt, k=k: k == kts[-1])
                        pend.append((False, lambda p_=p_, k=k, firstI=firstI, lastI=(k == kts[-1]): cmp_imp(p_, k, firstI, lastI)))
                        firstI = False
                    pend.append((False, lambda O=O, c=c, h=h, hh=hh: cmp_norm(O, c, h, hh)))
                flush()
                for tt in range(4):
                    n = 4 * c + tt
                    P.tt(DVE, sc_, sc_[:], imp, imp[:, tt, :], sV, sV[:, n, :], ALU.mult)
                    P.tt(DVE, sc_, sc_[:], sc_, sc_[:], sA, sA[:, n, :], ALU.add)
                    P.call(DVE, "max", [sc_], [m8], out=m8[:, 0:8], in_=sc_[:])
                    if TOPN > 8:
                        P.call(DVE, "match_replace", [sc_, m8], [sc2], out=sc2[:], in_to_replace=m8[:, 0:8], in_values=sc_[:], imm_value=-3e38)
                        P.call(DVE, "max", [sc2], [m8], out=m8[:, 8:16], in_=sc2[:])
                    P.ts(DVE, sc2, sc2[:], sc_, sc_[:], m8[:, TOPN - 1:TOPN], None, ALU.is_ge, extra=[m8])
                    P.ts(DVE, sbt, sbt[:], sc2, sc2[:], -1.0, -NEGM, ALU.add, ALU.mult)
                    P.tr(psX, psX[:, tt * 128:(tt + 1) * 128], sbt, sbt[:], identb)
                P.cp(ACT, selT, selT[:], psX, psX[:])
                for hh in range(4):
                    h = 4 * g + hh
                    O = psO[oi % len(psO)]; oi += 1
                    first = True
                    for k in range(4 * c + 4):
                        d = k - 4 * c
                        ex = [(Xb, Xb[:, k, :], selT, selT[:])]
                        if d >= -1:
                            ex.append((identb, identb[:], wB_, wB_[:, hh, 5 if d == -1 else d + 1, :])); bap = zcol[:, 0:1]; bb = zcol
                        else:
                            bap = farcs[:, h:h + 1]; bb = farcs
                        first, _ = att_tile(ksl, ksl[:, k * 128:(k + 1) * 128], q_, q_[:, hh, :], ex, bap, bb,
                                            Vs, Vs[:, k, :], O, lambda tt, k=k: k <= 4 * c + tt, first, lambda tt, k=k: k == 4 * c + tt)
                    Ow = psO[oi % len(psO)]; oi += 1
                    first = True
                    k0 = max(0, 4 * c - 4)
                    for k in range(k0, 4 * c + 4):
                        d = k - 4 * c
                        if d <= -2:
                            ex = [(identb, identb[:], wS_, wS_[:, d + 4, :])]; bap = farcs[:, h:h + 1]; bb = farcs
                        else:
                            ex = [(identb, identb[:], wB_, wB_[:, hh, d + 1, :])]; bap = zcol[:, 0:1]; bb = zcol
                        first, _ = att_tile(kwn, kwn[:, k * 128:(k + 1) * 128], q_, q_[:, hh, :], ex, bap, bb,
                                            Vw, Vw[:, k, :], Ow, lambda tt, k=k: (k <= 4 * c + tt) and (k >= 4 * c + tt - 4), first,
                                            lambda tt, k=k: k == 4 * c + tt)
                    pend.append((False, lambda O=O, Ow=Ow, c=c, h=h, hh=hh: sw_norm(O, Ow, c, h, hh)))
                flush()
        if dbg:
            P.dma(SP, dyn, dyn[:, :], ynsa, ynsa.t.rearrange("p n f -> p (n f)"), sem_buf=ynsa)
            fin += [ynsa]

    if phases >= 4:
      with P.scope():
        wfb = P.sbuf([128, 4, D], BF16, "wfb"); wnb = P.sbuf([128, 4, D], BF16, "wnb"); wmb = P.sbuf([128, 8, D], BF16, "wmb")
        wgb = P.sbuf([128, 8, NG], BF16, "wgb"); wrt = P.sbuf([128, 8, 32], F32, "wrt"); brt = P.sbuf([128, 32], F32, "brt")
        with P.scope():
            stg = [P.sbuf([128, 1024], F32, f"dstg{i}") for i in range(2)]
            i = 0
            for (wd_, src, nk) in ((wfb, wfp, 4), (wnb, wnp, 4), (wmb, wmo, 8)):
                sv = src.t.rearrange("(k p) n -> p k n", p=128)
                for k in range(nk):
                    st = stg[i % 2]
                    P.dma(SP, st, st[:, 0:D], src, sv[:, k, :]); P.cp(POOL if i % 2 else DVE, wd_, wd_[:, k, :], st, st[:, 0:D]); i += 1
            wv = w_in.t.rearrange("(k p) n -> p k n", p=128)
            for k in range(8):
                for hf in range(2):
                    st = stg[i % 2]
                    P.dma(SP, st, st[:], w_in, wv[:, k, 2848 + hf * 1024:2848 + (hf + 1) * 1024])
                    P.cp(POOL if i % 2 else DVE, wgb, wgb[:, k, hf * 1024:(hf + 1) * 1024], st, st[:]); i += 1
        P.dma(SP, wrt, wrt[:], w_router, w_router.t.rearrange("(k p) n -> p k n", p=128))
        P.dma(SP, brt, brt[:], b_router, b_router[:, :])

        def mk(c_):
            d = {}
            d["hl"] = P.sbuf([128, 8, 128], BF16, f"hTl{c_}"); d["yf"] = P.sbuf([128, 512], BF16, f"yfl{c_}")
            d["xi"] = P.sbuf([128, D], F32, f"dx{c_}"); d["yT"] = P.sbuf([128, 8, 128], BF16, f"yT{c_}")
            d["ptb"] = P.psum([128, D], BF16, f"dptb{c_}"); d["pa"] = P.psum([128, 512], F32, f"dpa{c_}")
            d["pb"] = P.psum([128, 512], F32, f"dpb{c_}"); d["pq"] = P.psum([128, 512], F32, f"dpq{c_}")
            d["s0"] = P.sbuf([128, 512], F32, f"ds0{c_}"); d["s1"] = P.sbuf([128, 512], F32, f"ds1{c_}")
            d["tmp"] = P.sbuf([128, 512], F32, f"dtmp{c_}"); d["mtok"] = P.sbuf([128, D], BF16, f"mtok{c_}")
            d["mT"] = P.sbuf([128, 8, 128], BF16, f"mT{c_}")
            d["ss"] = P.sbuf([128, 1], F32, f"dss{c_}"); d["rstd"] = P.sbuf([128, 1], F32, f"drstd{c_}")
            d["x1"] = P.sbuf([128, D], F32, f"x1{c_}"); d["xn2"] = P.sbuf([128, D], F32, f"xn2{c_}")
            d["h2f"] = P.sbuf([128, 8, 128], F32, f"h2f{c_}"); d["hb"] = P.sbuf([128, 8, 128], BF16, f"h2b{c_}")
            d["lg"] = P.sbuf([128, 32], F32, f"lg{c_}"); d["m8"] = P.sbuf([128, 8], F32, f"dm8{c_}"); d["msk"] = P.sbuf([128, 32], F32, f"msk{c_}")
            d["ex"] = P.sbuf([128, 32], F32, f"ex{c_}"); d["nmx"] = P.sbuf([128, 1], F32, f"nmx{c_}"); d["sm"] = P.sbuf([128, 1], F32, f"sm{c_}")
            return d
        chains = [mk(0), mk(1)]

        def tile_d(n, d):
            hl, yf_, xi, yT, ptb, pa, pb, pq = d["hl"], d["yf"], d["xi"], d["yT"], d["ptb"], d["pa"], d["pb"], d["pq"]
            s0, s1, tmp, mtok, mT, ss, rstd, x1_, xn2, h2f, hb = (d[k_] for k_ in ("s0", "s1", "tmp", "mtok", "mT", "ss", "rstd", "x1", "xn2", "h2f", "hb"))
            lg, m8, msk, ex_, nmx, sm = (d[k_] for k_ in ("lg", "m8", "msk", "ex", "nmx", "sm"))
            P.dma(SP, hl, hl[:], hTs, hTs[:, :, n * 128:(n + 1) * 128])
            P.dma(SP, xi, xi[:], x, x[n * 128:(n + 1) * 128, :])
            P.dma(SP, yf_, yf_[:], yfs, yfs[:, n * 512:(n + 1) * 512])
            yield
            for j in range(4):
                P.tr(ptb, ptb[:, j * 128:(j + 1) * 128], yf_, yf_[:, j * 128:(j + 1) * 128], identb)
                P.tr(ptb, ptb[:, (4 + j) * 128:(5 + j) * 128], ynsa, ynsa[:, n, j * 128:(j + 1) * 128], identb)
            P.cp(ACT, yT, yT.t.rearrange("p k t -> p (k t)"), ptb, ptb[:])
            yield
            for hf in range(2):
                for j in range(4):
                    P.mm(pa, pa[:], yT, yT[:, j, :], wfb, wfb[:, j, hf * 512:(hf + 1) * 512], start=(j == 0), stop=(j == 3))
                for j in range(4):
                    P.mm(pb, pb[:], yT, yT[:, 4 + j, :], wnb, wnb[:, j, hf * 512:(hf + 1) * 512], start=(j == 0), stop=(j == 3))
                for k in range(8):
                    P.mm(pq, pq[:], hl, hl[:, k, :], wgb, wgb[:, k, hf * 512:(hf + 1) * 512], start=(k == 0), stop=(k == 7))
                P.act(s0, s0[:], pq, pq[:], AF.Sigmoid)
                yield
                for k in range(8):
                    P.mm(pq, pq[:], hl, hl[:, k, :], wgb, wgb[:, k, 1024 + hf * 512:1024 + (hf + 1) * 512], start=(k == 0), stop=(k == 7))
                P.act(s1, s1[:], pq, pq[:], AF.Sigmoid)
                P.tt(DVE, tmp, tmp[:], s0, s0[:], pa, pa[:], ALU.mult)
                yield
                P.tt(DVE, s1, s1[:], s1, s1[:], pb, pb[:], ALU.mult)
                P.tt(DVE, mtok, mtok[:, hf * 512:(hf + 1) * 512], s1, s1[:], tmp, tmp[:], ALU.add)
                yield
            for k in range(8):
                P.tr(ptb, ptb[:, k * 128:(k + 1) * 128], mtok, mtok[:, k * 128:(k + 1) * 128], identb)
            P.cp(ACT, mT, mT.t.rearrange("p k t -> p (k t)"), ptb, ptb[:])
            yield
            for hf, p_ in ((0, pa), (1, pb)):
                for k in range(8):
                    P.mm(p_, p_[:], mT, mT[:, k, :], wmb, wmb[:, k, hf * 512:(hf + 1) * 512], start=(k == 0), stop=(k == 7))
            yield
            for hf, p_ in ((0, pa), (1, pb)):
                P.act(xn2, xn2[:, hf * 512:(hf + 1) * 512], p_, p_[:], AF.Square)
            P.call(DVE, "reduce_sum", [xn2], [ss], out=ss[:], in_=xn2[:], axis=AX.X)
            P.ts(DVE, ss, ss[:], ss, ss[:], 1.0 / D, RMS_EPS, ALU.mult, ALU.add)
            yield
            P.act(ss, ss[:], ss, ss[:], AF.Sqrt)
            P.call(DVE, "reciprocal", [ss], [rstd], out=rstd[:], in_=ss[:])
            yield
            for hf, p_ in ((0, pa), (1, pb)):
                sl = slice(hf * 512, (hf + 1) * 512)
                P.stt(x1_, x1_[:, sl], p_, p_[:], rstd[:, 0:1], G1, G1[:, sl], ALU.mult, ALU.mult, extra=[rstd])
            P.tt(DVE, x1_, x1_[:], x1_, x1_[:], xi, xi[:], ALU.add)
            P.dma(POOL, x1s, x1s[n * 128:(n + 1) * 128, :], x1_, x1_[:], sem_buf=x1_)
            yield
            P.act(xn2, xn2[:], x1_, x1_[:], AF.Square)
            P.call(DVE, "reduce_sum", [xn2], [ss], out=ss[:], in_=xn2[:], axis=AX.X)
            P.ts(DVE, ss, ss[:], ss, ss[:], 1.0 / D, RMS_EPS, ALU.mult, ALU.add)
            yield
            P.act(ss, ss[:], ss, ss[:], AF.Sqrt)
            P.call(DVE, "reciprocal", [ss], [rstd], out=rstd[:], in_=ss[:])
            P.ts(DVE, xn2, xn2[:], x1_, x1_[:], rstd[:, 0:1], None, ALU.mult, extra=[rstd])
            yield
            for hf, p_ in ((0, pa), (1, pb)):
                for k in range(4):
                    kk = hf * 4 + k
                    P.tr(p_, p_[:, k * 128:(k + 1) * 128], xn2, xn2[:, kk * 128:(kk + 1) * 128], identf)
            yield
            for kk in range(8):
                p_ = pa if kk < 4 else pb
                P.act(h2f, h2f[:, kk, :], p_, p_[:, (kk % 4) * 128:(kk % 4 + 1) * 128], AF.Identity,
                      bias=B2[:, kk:kk + 1], scale=A2[:, kk:kk + 1], extra=[A2, B2])
            P.cp(DVE, hb, hb[:], h2f, h2f[:])
            P.dma(POOL, h2Ts, h2Ts[:, :, n * 128:(n + 1) * 128], hb, hb[:], sem_buf=hb)
            yield
            for k in range(8):
                P.mm(pq, pq[:, 0:32], h2f, h2f[:, k, :], wrt, wrt[:, k, :], start=(k == 0), stop=(k == 7))
            P.tt(DVE, lg, lg[:], pq, pq[:, 0:32], brt, brt[:], ALU.add)
            P.call(DVE, "max", [lg], [m8], out=m8[:], in_=lg[:])
            yield
            P.ts(DVE, msk, msk[:], lg, lg[:], m8[:, 3:4], None, ALU.is_ge, extra=[m8])
            P.ts(DVE, nmx, nmx[:], m8, m8[:, 0:1], -1.0, None, ALU.mult)
            P.act(ex_, ex_[:], lg, lg[:], AF.Exp, bias=nmx[:, 0:1], extra=[nmx])
            yield
            P.tt(DVE, ex_, ex_[:], ex_, ex_[:], msk, msk[:], ALU.mult)
            P.call(DVE, "reduce_sum", [ex_], [sm], out=sm[:], in_=ex_[:], axis=AX.X)
            P.call(DVE, "reciprocal", [sm], [sm], out=sm[:], in_=sm[:])
            P.ts(DVE, rw, rw[:, n, :], ex_, ex_[:], sm[:, 0:1], None, ALU.mult, extra=[sm])

        for n0 in range(0, NT, 2):
            gens = [tile_d(n0, chains[0]), tile_d(n0 + 1, chains[1])]
            live = [True, True]
            while any(live):
                for gi_ in range(2):
                    if live[gi_]:
                        try:
                            next(gens[gi_])
                        except StopIteration:
                            live[gi_] = False
        if dbg:
            P.dma(SP, drw, drw[:, :], rw, rw.t.rearrange("p n e -> p (n e)"), sem_buf=rw)
        fin += [x1s, h2Ts, rw]
    if phases >= 3:
        yscope.__exit__(None, None, None)

    if phases >= 5:
      with P.scope():
        TS = min(S, 1024); NTS = TS // 128; NCS = TS // 512
        acc = P.sbuf([128, NTS, D], F32, "acc")
        h2 = P.sbuf([128, 8, TS], BF16, "h2")
        wgt = [P.sbuf([128, 8, D], BF16, f"wE{i}") for i in range(2)]
        wgf = [[Buf(wgt[m].t, f"wE{m}_{f}", P.base) for f in range(8)] for m in range(2)]
        wdb = [P.sbuf([128, 8, D], BF16, f"wD{i}") for i in range(2)]
        wdk = [[Buf(wdb[b_].t, f"wD{b_}_{k}", P.base) for k in range(8)] for b_ in range(2)]
        stg = [P.sbuf([128, D], F32, f"estg{i}") for i in range(4)]
        hid = [P.sbuf([128, 8, 512], BF16, f"hid{i}") for i in range(2)]
        bgs = P.sbuf([128, E, 8], F32, "bgs"); bus = P.sbuf([128, E, 8], F32, "bus")
        bdall = P.sbuf([32, D], F32, "bdall"); rwT = P.sbuf([32, 128], F32, "rwT")
        P.dma(SP, bdall, bdall[0:E, :], bdn, bdn.t.rearrange("o (e d) -> (o e) d", d=D))
        P.dma(SP, bgs, bgs[:], bgT, bgT.t.rearrange("p (e k) -> p e k", k=8))
        P.dma(SP, bus, bus[:], buT, buT.t.rearrange("p (e k) -> p e k", k=8))
        bg17 = P.sbuf([128, E, 8], F32, "bg17")
        P.ts(DVE, bg17, bg17[:], bgs, bgs[:], 1.702, None, ALU.mult)
        SIGCAP = float(1.0 / (1.0 + math.exp(-1.702 * 7.0)))
        pG = [P.psum([128, 512], F32, f"eG{i}") for i in range(2)]
        pU = [P.psum([128, 512], F32, f"eU{i}") for i in range(2)]
        pY = [P.psum([128, 512], F32, f"eY{i}") for i in range(4)]
        gs = [P.sbuf([128, 512], F32, f"gs{i}") for i in range(2)]
        sg = [P.sbuf([128, 512], F32, f"esg{i}") for i in range(2)]
        us = [P.sbuf([128, 512], F32, f"us{i}") for i in range(2)]
        junk = P.sbuf([128, D], F32, "ejunk"); ss = P.sbuf([128, 1], F32, "ess"); rstd = P.sbuf([128, 1], F32, "erstd")
        x1l = [P.sbuf([128, D], F32, f"x1l{i}") for i in range(1)]
        ot = [P.sbuf([128, D], F32, f"ot{i}") for i in range(1)]
        wsrc = (w_gate, w_up, w_down)
        sti = 0; fi = 0; yi = 0

        def pf_gu(e, f):
            nonlocal sti
            for m in (0, 1):
                st = stg[sti % 4]
                stv = st.t.rearrange("p (k n) -> p k n", k=8)
                P.dma(SP, st, st[:], wsrc[m], wsrc[m].t[e, f, :, :])
                P.cp(ACT, wgf[m][f], wgt[m][:, :, f * 128:(f + 1) * 128], st, stv)
                sti += 1

        def pf_d(e, k, b_):
            nonlocal sti
            st = stg[sti % 4]
            P.dma(SP, st, st[:], w_down, w_down.t[e][k * 128:(k + 1) * 128, :])
            P.cp(ACT, wdk[b_][k], wdb[b_][:, k, :], st, st[:])
            sti += 1

        items = [(sp_, e) for sp_ in range(S // TS) for e in range(E)]
        pdown = []

        def emit_down(hd, wb, bd_b, c, sp_, e):
            nonlocal yi
            for tt in range(4):
                n = c * 4 + tt
                for hf in range(2):
                    Y_ = pY[yi % 4]; yi += 1
                    for f in range(8):
                        P.mm(Y_, Y_[:], hd, hd[:, f, tt * 128:(tt + 1) * 128], wdk[wb][f], wdb[wb][:, f, hf * 512:(hf + 1) * 512], start=(f == 0), stop=(f == 7))
                    sl = slice(hf * 512, (hf + 1) * 512)
                    wcol = rw[:, sp_ * NTS + n, e:e + 1]
                    P.stt(acc, acc[:, n, sl], Y_, Y_[:], wcol, acc, acc[:, n, sl], ALU.mult, ALU.add, extra=[rw])

        def flush_down():
            for f_ in pdown:
                f_()
            pdown.clear()
        for f in range(8):
            pf_gu(0, f)
        for k in range(8):
            pf_d(0, k, 0)
        for it, (sp_, e) in enumerate(items):
            nxt = items[it + 1][1] if it + 1 < len(items) else None
            wb = it % 2
            if True:
                if e == 0:
                    t0 = sp_ * TS
                    P.dma(SP, h2, h2[:], h2Ts, h2Ts[:, :, t0:t0 + TS])
                bd_b = None
                if e == 0:
                    for n in range(NTS):
                        Yt = pY[yi % 4]; yi += 1
                        P.tr(Yt, Yt[0:32, 0:128], rw, rw[:, sp_ * NTS + n, :], identf)
                        P.cp(ACT, rwT, rwT[:], Yt, Yt[0:32, 0:128])
                        for hf in range(2):
                            Yb = pY[yi % 4]; yi += 1
                            P.mm(Yb, Yb[:], rwT, rwT[0:E, :], bdall, bdall[0:E, hf * 512:(hf + 1) * 512])
                            P.cp(ACT, acc, acc[:, n, hf * 512:(hf + 1) * 512], Yb, Yb[:])
                for c in range(NCS):
                    hd = hid[(e * NCS + c) % 2]
                    for f in range(8):
                        G_ = pG[fi % 2]; U_ = pU[fi % 2]; g_ = gs[fi % 2]; s_ = sg[fi % 2]; u_ = us[fi % 2]; fi += 1
                        for k in range(8):
                            P.mm(G_, G_[:], wgf[0][f], wgt[0][:, k, f * 128:(f + 1) * 128], h2, h2[:, k, c * 512:(c + 1) * 512], start=(k == 0), stop=(k == 7))
                        for k in range(8):
                            P.mm(U_, U_[:], wgf[1][f], wgt[1][:, k, f * 128:(f + 1) * 128], h2, h2[:, k, c * 512:(c + 1) * 512], start=(k == 0), stop=(k == 7))
                        P.act(s_, s_[:], G_, G_[:], AF.Sigmoid, bias=bg17[:, e, f:f + 1], scale=1.702, extra=[bg17])
                        P.act(u_, u_[:], U_, U_[:], AF.Identity, bias=bus[:, e, f:f + 1], extra=[bus])
                        P.ts(DVE, g_, g_[:], G_, G_[:], bgs[:, e, f:f + 1], 7.0, ALU.add, ALU.min, extra=[bgs])
                        P.stt(g_, g_[:], s_, s_[:], SIGCAP, g_, g_[:], ALU.min, ALU.mult)
                        P.ts(DVE, u_, u_[:], u_, u_[:], -7.0, 7.0, ALU.max, ALU.min)
                        P.stt(hd, hd[:, f, :], u_, u_[:], 1.0, g_, g_[:], ALU.add, ALU.mult)
                        if f == 1:
                            flush_down()
                        if nxt is not None and c == 0 and f >= 2:
                            pf_d(nxt, f - 2, (it + 1) % 2)
                        if nxt is not None and c == NCS - 1:
                            pf_gu(nxt, f)
                    if nxt is not None and c == 0:
                        pf_d(nxt, 6, (it + 1) % 2); pf_d(nxt, 7, (it + 1) % 2)
                    pdown.append(lambda hd=hd, wb=wb, bd_b=bd_b, c=c, sp_=sp_, e=e: emit_down(hd, wb, bd_b, c, sp_, e))
            if e != E - 1:
                continue
            flush_down()
            for n in range(NTS):
                gn = sp_ * NTS + n
                xl = x1l[0]; o_ = ot[0]
                P.dma(SP, xl, xl[:], x1s, x1s[gn * 128:(gn + 1) * 128, :])
                rstd_of(acc, acc[:, n, :], junk, ss, rstd)
                P.stt(o_, o_[:], acc, acc[:, n, :], rstd[:, 0:1], G2, G2[:], ALU.mult, ALU.mult, extra=[rstd])
                P.tt(POOL, o_, o_[:], o_, o_[:], xl, xl[:], ALU.add)
                P.dma(POOL, out, out[gn * 128:(gn + 1) * 128, :], o_, o_[:], sem_buf=o_)
                fin.append(o_)
    P.wait_all(SP, fin)
    P.wait_all(POOL, fin)
    P.emit()
    return nc


def _bucket(n):
    n = np.maximum(n, 0)
    nf = np.maximum(n, 1).astype(np.float32)
    large = 16 + (np.log(nf / np.float32(16)) / np.float32(math.log(128 / 16)) * np.float32(16)).astype(np.int32)
    large = np.minimum(large, 31)
    return np.where(n < 16, n, large).astype(np.int64)


def host_consts(S):
    NT, NSEL = S // 128, S // 64
    NCMP = (S - 32) // 16 + 1
    NCT = (NCMP + 127) // 128
    p = np.arange(128)[:, None]; j = np.arange(512)[None, :]
    winM = np.zeros((9, 128, 512), np.float32); winI = np.zeros((9, 128, 512), np.int64)
    for idx in range(9):
        d = -1 if idx == 8 else idx - 4
        dist = j - (128 * d + p)
        ok = (dist >= 0) if idx == 8 else ((dist >= 0) & (dist < 512))
        winM[idx] = np.where(ok, 0.0, NEGM); winI[idx] = _bucket(dist)
    cmpM = np.zeros((5, 128, 512), np.float32); cmpI = np.zeros((5, 128, 512), np.int64)
    for r in range(5):
        nn = 512 * r + j - 16 * p - 31
        cmpM[r] = np.where(nn >= 0, 0.0, NEGM); cmpI[r] = _bucket(nn)
    caus = np.zeros((4, 128, 512), np.float32)
    for d in range(4):
        caus[d] = np.where(128 * d + p <= j, 0.0, NEGM)
    cs = np.arange(NCT * 128)[:, None] * 16; ss = np.arange(NSEL)[None, :] * 64
    ov = ((cs < ss + 64) & (cs + 32 > ss) & (np.arange(NCT * 128)[:, None] < NCMP)).astype(np.float32)
    ovl = np.concatenate([ov, np.ones((NCT * 128, 1), np.float32)], axis=1)
    t = np.arange(S)[:, None]; jb = np.arange(NSEL)[None, :]
    cur = t // 64
    forced = (jb == 0) | (jb == cur) | (jb == cur - 1)
    svalid = jb * 64 <= t
    A = np.where(forced, BIG, np.where(svalid, 0.0, -BIG)).astype(np.float32)
    Vm = (svalid & ~forced).astype(np.float32)
    tom = lambda a: np.ascontiguousarray(a.reshape(NT, 128, NSEL).transpose(1, 0, 2).reshape(128, NT * NSEL))
    Xp = np.zeros((NSEL, NT, 128), np.float32)
    for k in range(NT):
        Xp[2 * k, k, :64] = 1.0; Xp[2 * k + 1, k, 64:] = 1.0
    tri = (np.arange(128)[:, None] <= np.arange(128)[None, :]).astype(np.float32)
    return dict(winM=winM, cmpM=cmpM, caus=caus, ovl=ovl, selA=tom(A), selV=tom(Vm), Xp=Xp.reshape(NSEL, NT * 128),
                tri=tri), winI, cmpI


def _fblk(w):
    E_ = w.shape[0]
    return np.ascontiguousarray(w.reshape(E_, 8, 128, 8, 128).transpose(0, 3, 2, 1, 4).reshape(E_, 8, 128, 1024), dtype=np.float32)


def host_inputs(inp, b, S, E, consts, winI, cmpI):
    f = lambda a: np.ascontiguousarray(a, dtype=np.float32)
    NT = S // 128
    colT = lambda v: f(v.reshape(8, 128).T)
    rep = lambda v: f(np.broadcast_to(v[None, :], (128, v.shape[0])))
    rb = inp["rel_bias"]
    m = dict(consts)
    m.update(
        x=f(inp["x"][b, :S]), cT=colT(inp["c"][b]), w_ada=f(inp["w_ada"][0]), b_ada=f(inp["b_ada"][0][None, :]),
        gpre1=colT(inp["g_mix_pre"][0]), gpre2=colT(inp["g_ffn_pre"][0]),
        gpost1=rep(inp["g_mix_post"][0]), gpost2=rep(inp["g_ffn_post"][0]),
        w_in=f(inp["w_in"][0][:, w_in_perm()]), bfg=f(np.tile(rep(inp["b_forget"][0]), (1, NT))),
        pekT=f(inp["pe_k"][0].T), pevT=f(inp["pe_v"][0].T), wck=f(inp["w_cmp_k"][0]), wcv=f(inp["w_cmp_v"][0]),
        wfp=f(inp["w_fox_proj"][0]), wnp=f(inp["w_nsa_proj"][0]), wmo=f(inp["w_mix_out"][0]),
        winG=f(rb[winI].transpose(3, 0, 1, 2)), cmpG=f(rb[cmpI].transpose(3, 0, 1, 2)), farc=rep(rb[31]),
        w_router=f(inp["w_router"][0]), b_router=rep(inp["b_router"][0]),
        w_gate=_fblk(inp["w_gate"][0][:E]), w_up=_fblk(inp["w_up"][0][:E]), w_down=f(inp["w_down"][0][:E]),
        bgT=f(inp["b_gate"][0][:E].reshape(E, 8, 128).transpose(2, 0, 1).reshape(128, E * 8)),
        buT=f(inp["b_up"][0][:E].reshape(E, 8, 128).transpose(2, 0, 1).reshape(128, E * 8)),
        bdn=f(inp["b_down"][0][:E].reshape(1, E * 1024)),
    )
    return m


_NC_CACHE = {}


def kernel(**inputs):
    S, E, B = 4096, 32, 8
    if "nc" not in _NC_CACHE:
        _NC_CACHE["nc"] = build(S, E)
    nc = _NC_CACHE["nc"]
    inp = {k: np.asarray(v) for k, v in inputs.items()}
    consts, winI, cmpI = host_consts(S)
    shared = host_inputs(inp, 0, S, E, consts, winI, cmpI)
    in_maps = []
    for b in range(B):
        m = dict(shared)
        m["x"] = np.ascontiguousarray(inp["x"][b], dtype=np.float32)
        m["cT"] = np.ascontiguousarray(inp["c"][b].reshape(8, 128).T, dtype=np.float32)
        in_maps.append(m)
    res = run_bass_kernel_spmd(nc, in_maps, core_ids=list(range(B)))
    return np.stack([np.asarray(r["out"], dtype=np.float32) for r in res.results], axis=0)
```

```python
import math
from contextlib import ExitStack
import numpy as np
import concourse.bass as bass
import concourse.mybir as mybir
from concourse.bass_utils import run_bass_kernel_spmd

F32 = mybir.dt.float32
BF16 = mybir.dt.bfloat16
AF = mybir.ActivationFunctionType
ALU = mybir.AluOpType
AX = mybir.AxisListType

PE, ACT, DVE, POOL, SP = "tensor", "scalar", "vector", "gpsimd", "sync"
ENGS = (PE, ACT, DVE, POOL, SP)
NEGM = -30000.0
BIG = 1e9


class Buf:
    __slots__ = ("t", "name", "lw", "rd", "sem", "ndma", "multi", "psum")

    def __init__(self, t, name, base=None):
        self.t = t
        self.name = name
        self.lw = dict(base) if base else {}
        self.rd = {}
        self.sem = None
        self.ndma = 0
        self.multi = False
        self.psum = False

    def __getitem__(self, idx):
        return self.t[idx]


class Prog:
    def __init__(self, nc):
        self.nc = nc
        self.es = ExitStack()
        self.scopes = []
        self.q = {e: [] for e in ENGS}
        self.esem = {}
        self.pesems = set()
        self.ecnt = {e: 0 for e in ENGS}
        self.known = {e: {} for e in ENGS}
        self.base = {}
        self.nsem = 0
        self.sem_eng = {}
        for e in (PE, ACT, DVE, POOL):
            self._newsem(e)
        self.nbuf = 0

    def _sem(self, name):
        self.nsem += 1
        return self.es.enter_context(self.nc.semaphore(f"{name}_{self.nsem}"))

    def _newsem(self, e):
        s = self._sem("s_" + e)
        self.esem[e] = s
        self.sem_eng[s] = e
        self.ecnt[e] = 0
        if e == PE:
            self.pesems.add(s)

    class _Scope:
        def __init__(self, P):
            self.P = P
            self.es = ExitStack()
            self.bufs = []

        def __enter__(self):
            self.P.scopes.append(self)
            return self

        def __exit__(self, *a):
            P = self.P
            P.scopes.pop()
            for b in self.bufs:
                for d in (b.lw, b.rd):
                    for s, v in d.items():
                        if P.base.get(s, 0) < v:
                            P.base[s] = v
            self.es.close()
            return False

    def scope(self):
        return Prog._Scope(self)

    def _reg(self, cm, name):
        sc = self.scopes[-1] if self.scopes else None
        t = (sc.es if sc else self.es).enter_context(cm)
        b = Buf(t, name, self.base)
        if sc:
            sc.bufs.append(b)
        return b

    def sbuf(self, shape, dt, name):
        self.nbuf += 1
        name = f"{name}_{self.nbuf}"
        return self._reg(self.nc.sbuf_tensor(name, list(shape), dt), name)

    def psum(self, shape, dt, name):
        self.nbuf += 1
        name = f"{name}_{self.nbuf}"
        esz = 2 if dt == BF16 else 4
        full = 2048 // esz
        b = self._reg(self.nc.psum_tensor(name, [128, full], dt), name)
        n = 1
        for d_ in shape[1:]:
            n *= d_
        assert n <= full
        v = b.t[0:shape[0], 0:n]
        if len(shape) == 3:
            v = v.rearrange("p (a b) -> p a b", b=shape[2])
        b.t = v
        b.psum = True
        return b

    def dram(self, name, shape, dt, kind="Internal"):
        t = self.nc.dram_tensor(name, list(shape), dt, kind=kind)
        b = Buf(t.ap(), name)
        b.multi = True
        return b

    def _dsem(self, b):
        if b.sem is None or b.ndma >= 1500:
            b.sem = self._sem("d")
            b.ndma = 0
        return b.sem

    def _deps(self, eng, reads, writes):
        waits = {}

        def add(d):
            for s, v in d.items():
                if waits.get(s, 0) < v:
                    waits[s] = v
        for b in reads:
            add(b.lw)
            if b.psum:
                add({s_: v_ for s_, v_ in b.rd.items() if self.sem_eng.get(s_) != eng})
        for b in writes:
            if not b.multi:
                add(b.lw)
            add(b.rd)
        kn = self.known[eng]
        out = []
        for s, v in waits.items():
            if eng == PE and s in self.pesems:
                continue
            if kn.get(s, 0) >= v:
                continue
            kn[s] = v
            out.append((s, v))
        return out

    def _commit(self, tok, reads, writes):
        s, v = tok
        for b in reads:
            if b.rd.get(s, 0) < v:
                b.rd[s] = v
        for b in writes:
            if b.multi:
                if b.lw.get(s, 0) < v:
                    b.lw[s] = v
            else:
                b.lw = {s: v}
                b.rd = {}

    def op(self, eng, fn, reads=(), writes=()):
        waits = self._deps(eng, reads, writes)
        if self.ecnt[eng] >= 30000:
            self._newsem(eng)
        self.ecnt[eng] += 1
        tok = (self.esem[eng], self.ecnt[eng])
        self.q[eng].append((waits, fn, tok[0], 1))
        self._commit(tok, reads, writes)

    def dma(self, eng, out_b, out_ap, in_b, in_ap, sem_buf=None, **kw):
        sb = sem_buf or out_b
        waits = self._deps(eng, [in_b], [out_b])
        sem = self._dsem(sb)
        sb.ndma += 1
        tok = (sem, 16 * sb.ndma)
        self.q[eng].append((waits, lambda e: e.dma_start(out=out_ap, in_=in_ap, **kw), sem, 16))
        self._commit(tok, [in_b], [out_b])

    def wait_all(self, eng, bufs):
        waits = self._deps(eng, [], bufs)
        self.q[eng].append((waits, None, None, 0))

    def mm(self, ob, o, lb, l, rb, r, start=True, stop=True, extra=()):
        self.op(PE, lambda e: e.matmul(o, lhsT=l, rhs=r, start=start, stop=stop, skip_group_check=True),
                [lb, rb] + list(extra), [ob])

    def tr(self, ob, o, ib, i, idb):
        self.op(PE, lambda e: e.transpose(out=o, in_=i, identity=idb[:]), [ib, idb], [ob])

    def act(self, ob, o, ib, i, func, bias=None, scale=None, extra=(), eng=ACT):
        kw = {}
        if bias is not None:
            kw["bias"] = bias
        if scale is not None:
            kw["scale"] = scale
        self.op(eng, lambda e: e.activation(out=o, in_=i, func=func, **kw), [ib] + list(extra), [ob])

    def ts(self, eng, ob, o, ib, i, s1, s2, op0, op1=None, extra=()):
        if op1 is None:
            self.op(eng, lambda e: e.tensor_scalar(out=o, in0=i, scalar1=s1, scalar2=None, op0=op0),
                    [ib] + list(extra), [ob])
        else:
            self.op(eng, lambda e: e.tensor_scalar(out=o, in0=i, scalar1=s1, scalar2=s2, op0=op0, op1=op1),
                    [ib] + list(extra), [ob])

    def tt(self, eng, ob, o, ab, a, bb, b, op):
        self.op(eng, lambda e: e.tensor_tensor(out=o, in0=a, in1=b, op=op), [ab, bb], [ob])

    def stt(self, ob, o, ab, a, sc, bb, b, op0, op1, extra=()):
        self.op(DVE, lambda e: e.scalar_tensor_tensor(out=o, in0=a, scalar=sc, in1=b, op0=op0, op1=op1),
                [ab, bb] + list(extra), [ob])

    def cp(self, eng, ob, o, ib, i):
        if eng == ACT:
            self.op(ACT, lambda e: e.copy(out=o, in_=i), [ib], [ob])
        else:
            self.op(eng, lambda e: e.tensor_copy(out=o, in_=i), [ib], [ob])

    def call(self, eng, name, reads, writes, **kw):
        self.op(eng, lambda e: getattr(e, name)(**kw), reads, writes)

    def memset(self, eng, ob, o, val):
        self.op(eng, lambda e: e.memset(o, val), [], [ob])

    def emit(self):
        nc = self.nc
        with nc.Block() as block:
            for e in ENGS:
                ops = self.q[e]

                def body(eh, ops=ops):
                    for waits, fn, sem, inc in ops:
                        for s, v in waits:
                            eh.wait_ge(s, v)
                        if fn is not None:
                            fn(eh).then_inc(sem, inc)
                getattr(block, e)(body)
        self.es.close()


OFF = dict(fq=0, fk=512, fv=1024, ff=1536, nq=1544, kcm=2056, vcm=2184, ksl=2312, vsl=2440, kwn=2568,
           vwn=2696, ng=2824, mgf=2848, mgn=3872)
NF, NTK, NSM, NG = 1920, 896, 32, 2048
RMS_EPS = 1e-6


def w_in_perm():
    r = lambda a, n: list(range(OFF[a], OFF[a] + n))
    return np.array(r("fq", 512) + r("fk", 512) + r("nq", 512) + r("kcm", 128) + r("ksl", 128) + r("kwn", 128)
                    + r("fv", 512) + r("vcm", 128) + r("vsl", 128) + r("vwn", 128)
                    + r("ff", 8) + r("ng", 24) + r("mgf", 1024) + r("mgn", 1024))


def build(S=4096, E=32, dbg=False, phases=5):
    NT, NCH, NSEL = S // 128, S // 512, S // 64
    NCMP = (S - 32) // 16 + 1
    NCT = (NCMP + 127) // 128
    TOPN = min(16, NSEL)
    D = 1024
    nc = bass.Bass("TRN2", target_bir_lowering=False)
    P = Prog(nc)

    def din(name, shape, dt=F32):
        return Buf(nc.dram_tensor(name, list(shape), dt, kind="ExternalInput").ap(), name)

    x = din("x", [S, D]); cT = din("cT", [128, 8]); w_ada = din("w_ada", [D, 6 * D]); b_ada = din("b_ada", [1, 6 * D])
    gpre1 = din("gpre1", [128, 8]); gpre2 = din("gpre2", [128, 8])
    gpost1 = din("gpost1", [128, D]); gpost2 = din("gpost2", [128, D])
    w_in = din("w_in", [D, 4896]); bfg = din("bfg", [128, NT * 8])
    pekT = din("pekT", [64, 32]); pevT = din("pevT", [64, 32])
    wck = din("wck", [2048, 64]); wcv = din("wcv", [2048, 64])
    wfp = din("wfp", [512, D]); wnp = din("wnp", [512, D]); wmo = din("wmo", [D, D])
    winG = din("winG", [8, 9, 128, 512]); cmpG = din("cmpG", [8, 5, 128, 512]); farc = din("farc", [128, 8])
    winM = din("winM", [9, 128, 512]); cmpM = din("cmpM", [5, 128, 512]); caus = din("caus", [4, 128, 512])
    ovl = din("ovl", [NCT * 128, NSEL + 1]); selA = din("selA", [128, NT * NSEL]); selV = din("selV", [128, NT * NSEL])
    Xp = din("Xp", [NSEL, NT * 128]); tri = din("tri", [128, 128])
    w_router = din("w_router", [D, 32]); b_router = din("b_router", [128, 32])
    w_gate = din("w_gate", [E, 8, 128, D]); w_up = din("w_up", [E, 8, 128, D]); w_down = din("w_down", [E, D, D])
    bgT = din("bgT", [128, E * 8]); buT = din("buT", [128, E * 8]); bdn = din("bdn", [1, E * D])
    okind = "ExternalOutput"
    out = Buf(nc.dram_tensor("out", [S, D], F32, kind=okind).ap(), "out")
    out.multi = True
    sk = okind if dbg else "Internal"
    featT = P.dram("featT", [NF, S], BF16, sk)
    vtok = P.dram("vtok", [S, NTK], BF16, sk)
    hTs = P.dram("hTs", [128, 8, S], BF16, sk)
    h2Ts = P.dram("h2Ts", [128, 8, S], BF16, sk)
    x1s = P.dram("x1s", [S, D], F32, sk)
    if dbg:
        dsm = P.dram("dsm", [128, NT * 32], F32, okind)
        dL = P.dram("dL", [128, NT * 8], F32, okind)
        dfb = P.dram("dfb", [128, 8 * NCH * NT], F32, okind)
        dcar = P.dram("dcar", [128, (NT + 1) * 8], F32, okind)
        dyn = P.dram("dyn", [128, NT * 512], BF16, okind)
        drw = P.dram("drw", [128, NT * 32], F32, okind)

    identb = P.sbuf([128, 128], BF16, "identb"); identf = P.sbuf([128, 128], F32, "identf")
    ones_r = P.sbuf([1, 128], F32, "ones_r"); ones_rb = P.sbuf([1, 128], BF16, "ones_rb")
    onesm = P.sbuf([128, 128], F32, "onesm")
    A1 = P.sbuf([128, 8], F32, "A1"); B1 = P.sbuf([128, 8], F32, "B1")
    A2 = P.sbuf([128, 8], F32, "A2"); B2 = P.sbuf([128, 8], F32, "B2")
    G1 = P.sbuf([128, D], F32, "G1"); G2 = P.sbuf([128, D], F32, "G2")
    small = P.sbuf([128, NT, 32], F32, "small")
    rw = P.sbuf([128, NT, 32], F32, "rw")
    zcol = P.sbuf([128, 1], F32, "zcol"); farcs = P.sbuf([128, 8], F32, "farcs")

    P.memset(POOL, identf, identf[:], 0.0)
    P.op(POOL, lambda e: e.affine_select(out=identf[:], in_=identf[:], pattern=[[-1, 128]], compare_op=ALU.not_equal,
                                          fill=1.0, base=0, channel_multiplier=1), [identf], [identf])
    P.cp(DVE, identb, identb[:], identf, identf[:])
    P.memset(DVE, ones_r, ones_r[:], 1.0); P.memset(DVE, ones_rb, ones_rb[:], 1.0)
    P.memset(POOL, onesm, onesm[:], 1.0); P.memset(DVE, zcol, zcol[:], 0.0)
    P.dma(SP, farcs, farcs[:], farc, farc[:, :])

    with P.scope():
        cs = P.sbuf([128, 8], F32, "cs"); scs = P.sbuf([128, 8], F32, "scs")
        arow = P.sbuf([1, 6 * D], F32, "arow"); brow = P.sbuf([1, 6 * D], F32, "brow")
        gp1 = P.sbuf([128, 8], F32, "gp1"); gp2 = P.sbuf([128, 8], F32, "gp2")
        wa = [P.sbuf([128, 8, 512], F32, f"wa{i}") for i in range(2)]
        psr = [P.psum([1, 512], F32, f"psr{i}") for i in range(2)]
        psT = P.psum([128, 48], F32, "psT"); psb = [P.psum([128, 512], F32, f"psb{i}") for i in range(2)]
        gpo = P.sbuf([128, D], F32, "gpo")
        P.dma(SP, cs, cs[:], cT, cT[:, :]); P.dma(SP, brow, brow[:], b_ada, b_ada[:, :])
        P.dma(SP, gp1, gp1[:], gpre1, gpre1[:, :]); P.dma(SP, gp2, gp2[:], gpre2, gpre2[:, :])
        P.act(scs, scs[:], cs, cs[:], AF.Silu)
        wav = w_ada.t.rearrange("(k p) n -> p k n", p=128)
        for blk in range(12):
            w_ = wa[blk % 2]; pr = psr[blk % 2]
            P.dma(SP, w_, w_[:], w_ada, wav[:, :, blk * 512:(blk + 1) * 512])
            for k in range(8):
                P.mm(pr, pr[0:1, :], scs, scs[:, k:k + 1], w_, w_[:, k, :], start=(k == 0), stop=(k == 7))
            P.tt(DVE, arow, arow[0:1, blk * 512:(blk + 1) * 512], pr, pr[0:1, :], brow, brow[0:1, blk * 512:(blk + 1) * 512], ALU.add)
        for j in list(range(0, 16)) + list(range(24, 40)):
            P.mm(psT, psT[:, j:j + 1], arow, arow[0:1, j * 128:(j + 1) * 128], ones_r, ones_r[0:1, 0:1])
        P.stt(A1, A1[:], psT, psT[:, 8:16], 1.0, gp1, gp1[:], ALU.add, ALU.mult)
        P.cp(DVE, B1, B1[:], psT, psT[:, 0:8])
        P.stt(A2, A2[:], psT, psT[:, 32:40], 1.0, gp2, gp2[:], ALU.add, ALU.mult)
        P.cp(DVE, B2, B2[:], psT, psT[:, 24:32])
        for (Gx, gsrc, c0) in ((G1, gpost1, 2048), (G2, gpost2, 5120)):
            P.dma(SP, gpo, gpo[:], gsrc, gsrc[:, :])
            for i in range(2):
                pb = psb[i]
                P.mm(pb, pb[:], ones_r, ones_r[0:1, :], arow, arow[0:1, c0 + i * 512:c0 + (i + 1) * 512])
                P.tt(DVE, Gx, Gx[:, i * 512:(i + 1) * 512], pb, pb[:], gpo, gpo[:, i * 512:(i + 1) * 512], ALU.mult)

    def rstd_of(src_b, src_ap, junk, ss, rstd):
        P.act(junk, junk[:], src_b, src_ap, AF.Square)
        P.call(DVE, "reduce_sum", [junk], [ss], out=ss[:], in_=junk[:], axis=AX.X)
        P.ts(DVE, ss, ss[:], ss, ss[:], 1.0 / D, RMS_EPS, ALU.mult, ALU.add)
        P.act(ss, ss[:], ss, ss[:], AF.Sqrt)
        P.call(DVE, "reciprocal", [ss], [rstd], out=rstd[:], in_=ss[:])

    bcscope = P.scope(); bcscope.__enter__()
    Lc = P.sbuf([128, NT, 8], F32, "Lc"); carry = P.sbuf([128, NT + 1, 8], F32, "carry")
    fbias = P.sbuf([128, 8, NCH, NT], F32, "fbias")
    with P.scope():
        NW = NF + NTK + NSM
        wB = P.sbuf([128, 8, NW], BF16, "wB")
        wBk = [Buf(wB.t, f"wBk{k}", P.base) for k in range(8)]
        stg = [P.sbuf([128, NW // 2], F32, f"stg{i}") for i in range(2)]
        wv = w_in.t.rearrange("(k p) n -> p k n", p=128)
        i = 0
        for k in range(8):
            for hf in range(2):
                st = stg[i % 2]
                P.dma(SP, st, st[:], w_in, wv[:, k, hf * (NW // 2):(hf + 1) * (NW // 2)])
                P.cp(ACT if i % 2 else DVE, wBk[k], wB[:, k, hf * (NW // 2):(hf + 1) * (NW // 2)], st, st[:])
                i += 1
        xin = [P.sbuf([128, D], F32, f"xin{i}") for i in range(2)]
        junk = P.sbuf([128, D], F32, "junk"); ss = P.sbuf([128, 1], F32, "ss"); rstd = P.sbuf([128, 1], F32, "rstd")
        xn = [P.sbuf([128, D], BF16, f"xn{i}") for i in range(2)]
        ptr = [P.psum([128, D], BF16, f"ptr{i}") for i in range(2)]
        hT = [P.sbuf([128, 8, 512], BF16, f"hT{i}") for i in range(2)]
        psf = [P.psum([128, 512], F32, f"psf{i}") for i in range(3)]
        fst = [P.sbuf([128, 512], BF16, f"fst{i}") for i in range(3)]
        vst = [P.sbuf([128, NTK], BF16, f"vst{i}") for i in range(2)]
        ci = 0

        def prep(c):
            h_ = hT[c % 2]
            for tt in range(4):
                n = 4 * c + tt
                xi = xin[n % 2]; xb = xn[n % 2]; pt = ptr[n % 2]
                P.dma(SP, xi, xi[:], x, x[n * 128:(n + 1) * 128, :])
                rstd_of(xi, xi[:], junk, ss, rstd)
                yield
                P.ts(DVE, xb, xb[:], xi, xi[:], rstd[:, 0:1], None, ALU.mult, extra=[rstd])
                for k in range(8):
                    P.tr(pt, pt[:, k * 128:(k + 1) * 128], xb, xb[:, k * 128:(k + 1) * 128], identb)
                yield
                for k in range(8):
                    P.act(h_, h_[:, k, tt * 128:(tt + 1) * 128], pt, pt[:, k * 128:(k + 1) * 128], AF.Identity,
                          bias=B1[:, k:k + 1], scale=A1[:, k:k + 1], extra=[A1, B1])
                yield
            P.dma(POOL, hTs, hTs[:, :, c * 512:(c + 1) * 512], h_, h_[:], sem_buf=h_)

        def mms(c):
            nonlocal ci
            h_ = hT[c % 2]
            for fb in range(NF // 128):
                ps = psf[ci % 3]; fs = fst[ci % 3]; ci += 1
                for k in range(8):
                    P.mm(ps, ps[:], wBk[k], wB[:, k, fb * 128:(fb + 1) * 128], h_, h_[:, k, :], start=(k == 0), stop=(k == 7))
                isq = fb < 4 or 8 <= fb < 12
                if fb % 2:
                    P.act(fs, fs[:], ps, ps[:], AF.Copy, scale=(0.125 if isq else 1.0))
                else:
                    P.ts(DVE, fs, fs[:], ps, ps[:], (0.125 if isq else 1.0), None, ALU.mult)
                P.dma(POOL, featT, featT[fb * 128:(fb + 1) * 128, c * 512:(c + 1) * 512], fs, fs[:], sem_buf=fs)
                yield
            for tt in range(4):
                n = 4 * c + tt
                vs = vst[n % 2]
                ps = psf[ci % 3]; ci += 1
                for k in range(8):
                    P.mm(ps, ps[:], h_, h_[:, k, tt * 128:(tt + 1) * 128], wBk[k], wB[:, k, NF:NF + 512], start=(k == 0), stop=(k == 7))
                P.cp(ACT, vs, vs[:, 0:512], ps, ps[:])
                yield
                ps = psf[ci % 3]; ci += 1
                for k in range(8):
                    P.mm(ps, ps[:, 0:416], h_, h_[:, k, tt * 128:(tt + 1) * 128], wBk[k], wB[:, k, NF + 512:NW], start=(k == 0), stop=(k == 7))
                P.cp(DVE, vs, vs[:, 512:896], ps, ps[:, 0:384])
                P.cp(DVE, small, small[:, n, :], ps, ps[:, 384:416])
                P.dma(POOL, vtok, vtok[n * 128:(n + 1) * 128, :], vs, vs[:], sem_buf=vs)
                yield

        for _ in prep(0):
            pass
        for c in range(NCH):
            gens = [mms(c)] + ([prep(c + 1)] if c + 1 < NCH else [])
            live = [True] * len(gens)
            while any(live):
                for gi_ in range(len(gens)):
                    if live[gi_]:
                        try:
                            next(gens[gi_])
                        except StopIteration:
                            live[gi_] = False

        bfs = P.sbuf([128, NT, 8], F32, "bfs"); sp = P.sbuf([128, NT, 8], F32, "sp")
        tris = P.sbuf([128, 128], F32, "tris"); tots = P.sbuf([128, NT, 8], F32, "tots")
        pcw = P.psum([128, NT * 8], F32, "pcw"); ptot = P.psum([128, NT * 8], F32, "ptot")
        P.dma(SP, bfs, bfs[:], bfg, bfg.t.rearrange("p (n h) -> p n h", h=8))
        P.dma(SP, tris, tris[:], tri, tri[:, :])
        P.tt(DVE, sp, sp[:], small, small[:, :, 0:8], bfs, bfs[:], ALU.add)
        P.act(sp, sp[:], sp, sp[:], AF.Exp, scale=-1.0)
        P.act(sp, sp[:], sp, sp[:], AF.Ln, bias=1.0)
        spf = sp.t.rearrange("p n h -> p (n h)")
        P.mm(pcw, pcw[:], tris, tris[:], sp, spf)
        P.mm(ptot, ptot[:], onesm, onesm[:], sp, spf)
        P.cp(DVE, tots, tots.t.rearrange("p n h -> p (n h)"), ptot, ptot[:])
        P.memset(DVE, carry, carry[:, 0, :], 0.0)
        for n in range(NT):
            P.tt(DVE, carry, carry[:, n + 1, :], carry, carry[:, n, :], tots, tots[:, n, :], ALU.add)
        P.tt(DVE, Lc, Lc.t.rearrange("p n h -> p (n h)"), pcw, pcw[:], carry, carry[:, 0:NT, :].rearrange("p n h -> p (n h)"), ALU.add)
        for h in range(8):
            for c in range(NCH):
                P.ts(DVE, fbias, fbias[:, h, c, :], Lc, Lc[:, :, h], carry[:, 4 * c + 2, h:h + 1], None, ALU.subtract, extra=[carry])
        P.act(small, small[:, :, 8:32], small, small[:, :, 8:32], AF.Sigmoid)
        if dbg:
            P.dma(SP, dsm, dsm[:, :], small, small.t.rearrange("p n h -> p (n h)"), sem_buf=small)
            P.dma(SP, dL, dL[:, :], Lc, Lc.t.rearrange("p n h -> p (n h)"), sem_buf=Lc)
            P.dma(SP, dfb, dfb[:, :], fbias, fbias.t.rearrange("p h c n -> p (h c n)"), sem_buf=fbias)
            P.dma(SP, dcar, dcar[:, :], carry, carry.t.rearrange("p n h -> p (n h)"), sem_buf=carry)

    fin = [featT, vtok, hTs]
    yfs = P.dram("yfs", [128, NT * 512], BF16, sk)

    if phases >= 3:
      with P.scope():
        yfox = P.sbuf([128, NT, 512], BF16, "yfox")
        causb = P.sbuf([128, 4, 512], BF16, "causb")
        cst = P.sbuf([128, 512], F32, "cst")
        for d in range(4):
            P.dma(SP, cst, cst[:], caus, caus[d, :, :])
            P.cp(DVE, causb, causb[:, d, :], cst, cst[:])
        kT = [P.sbuf([128, S], BF16, f"kT{i}") for i in range(2)]
        qA = [P.sbuf([128, S], BF16, f"qA{i}") for i in range(2)]
        qB = [P.sbuf([128, S], BF16, f"qB{i}") for i in range(2)]
        for i in range(2):
            P.memset(POOL, qA[i], qA[i][64:128, :], 0.0); P.memset(POOL, qB[i], qB[i][0:64, :], 0.0)
        V = [P.sbuf([128, NT, 65], BF16, f"V{i}") for i in range(2)]
        for i in range(2):
            P.memset(POOL, V[i], V[i][:, :, 64:65], 1.0)
        psS = [P.psum([128, 512], F32, f"psS{i}") for i in range(4)]
        psO = [P.psum([128, 4, 65], F32, f"psO{i}") for i in range(2)]
        pT = [P.sbuf([128, 512], BF16, f"pT{i}") for i in range(4)]
        rec = P.sbuf([128, 4], F32, "rec")
        si = 0; oi = 0
        pend = []

        def flush(keep=0):
            idx = len(pend)
            if keep:
                starts = [i_ for i_, (st_, _) in enumerate(pend) if st_]
                idx = starts[-keep] if len(starts) >= keep else 0
            for _, f_ in pend[:idx]:
                f_()
            del pend[:idx]

        def fox_pv(O, p_, v_, k, c, first):
            for tt in range(4):
                if k <= 4 * c + tt:
                    P.mm(O, O[:, tt, :], p_, p_[:, tt * 128:(tt + 1) * 128], v_, v_[:, k, :], start=first, stop=(k == 4 * c + tt))
                    first = False

        def fox_norm(O, c, h):
            P.ts(DVE, rec, rec[:], O, O[:, :, 64], 1e-30, None, ALU.add)
            P.call(DVE, "reciprocal", [rec], [rec], out=rec[:], in_=rec[:])
            for tt in range(4):
                P.ts(DVE, yfox, yfox[:, 4 * c + tt, h * 64:(h + 1) * 64], O, O[:, tt, 0:64], rec[:, tt:tt + 1], None, ALU.mult, extra=[rec])

        for h in range(8):
            jp = h // 2; ih = h % 2
            k_, v_ = kT[jp % 2], V[h % 2]
            q_ = (qA if ih == 0 else qB)[jp % 2]
            if ih == 0:
                P.dma(SP, k_, k_[:], featT, featT[512 + jp * 128:512 + (jp + 1) * 128, :])
            P.dma(SP, q_, q_[64 * ih:64 * ih + 64, :], featT, featT[h * 64:(h + 1) * 64, :])
            P.dma(SP, v_, v_[:, :, 0:64], vtok, vtok.t[:, h * 64:(h + 1) * 64].rearrange("(n p) d -> p n d", p=128))
            for c in range(NCH):
                O = psO[oi % 2]; oi += 1
                first = True
                for k in range(4 * c + 4):
                    st = psS[si % len(psS)]; p_ = pT[si % len(pT)]; si += 1
                    diag = k >= 4 * c
                    c0 = 128 * max(0, k - 4 * c)
                    P.mm(st, st[:, c0:512], k_, k_[:, k * 128:(k + 1) * 128], q_, q_[:, c * 512 + c0:(c + 1) * 512], start=True, stop=not diag)
                    if diag:
                        P.mm(st, st[:, c0:512], identb, identb[:], causb, causb[:, k - 4 * c, c0:512], start=False, stop=True)
                    flush(keep=2)
                    P.act(p_, p_[:, c0:512], st, st[:, c0:512], AF.Exp, bias=fbias[:, h, c, k:k + 1], extra=[fbias])
                    pend.append((True, lambda O=O, p_=p_, v_=v_, k=k, c=c, first=first: fox_pv(O, p_, v_, k, c, first)))
                    first = False
                pend.append((False, lambda O=O, c=c, h=h: fox_norm(O, c, h)))
        flush()
        P.dma(POOL, yfs, yfs[:, :], yfox, yfox.t.rearrange("p n f -> p (n f)"), sem_buf=yfox)
        fin.append(yfox)
    bcscope.__exit__(None, None, None)
    if phases >= 3:
        yscope = P.scope(); yscope.__enter__()
        ynsa = P.sbuf([128, NT, 512], BF16, "ynsa")

    if phases >= 3:
      with P.scope():
        kcT = P.sbuf([64, 2, NCT * 128], BF16, "kcT"); Vc = P.sbuf([128, 2, NCT, 65], BF16, "Vc")
        P.memset(POOL, kcT, kcT[:], 0.0); P.memset(POOL, Vc, Vc[:], 0.0)
        P.memset(POOL, Vc, Vc[:, :, :, 64:65], 1.0)
        with P.scope():
            wk_f = P.sbuf([64, 32, 64], F32, "wk_f"); wk_b = P.sbuf([64, 32, 64], BF16, "wk_b")
            wv_b = P.sbuf([64, 32, 64], BF16, "wv_b")
            pe_f = P.sbuf([64, 32], F32, "pe_f"); pek_b = P.sbuf([64, 32], BF16, "pek_b"); pev_b = P.sbuf([64, 32], BF16, "pev_b")
            kcm = P.sbuf([64, S], BF16, "kcm"); vcm = P.sbuf([64, S], BF16, "vcm")
            vtk = P.sbuf([128, NT, 64], BF16, "vtk")
            pc1 = P.psum([64, 256], F32, "pc1"); pc2 = P.psum([128, 64], F32, "pc2"); pc3 = P.psum([128, 64], F32, "pc3")
            pcT = P.psum([64, 128], BF16, "pcT")
            kcon = P.sbuf([64, 1], F32, "kcon"); vcon = P.sbuf([128, 64], F32, "vcon")
            P.dma(SP, wk_f, wk_f[:], wck, wck.t.rearrange("(l d) e -> d l e", d=64)); P.cp(DVE, wk_b, wk_b[:], wk_f, wk_f[:])
            P.dma(SP, wk_f, wk_f[:], wcv, wcv.t.rearrange("(l d) e -> d l e", d=64)); P.cp(DVE, wv_b, wv_b[:], wk_f, wk_f[:])
            P.dma(SP, pe_f, pe_f[:], pekT, pekT[:, :]); P.cp(DVE, pek_b, pek_b[:], pe_f, pe_f[:])
            P.dma(SP, pe_f, pe_f[:], pevT, pevT[:, :]); P.cp(DVE, pev_b, pev_b[:], pe_f, pe_f[:])
            for l in range(32):
                P.mm(pc1, pc1[:, 0:1], wk_b, wk_b[:, l, :], pek_b, pek_b[:, l:l + 1], start=(l == 0), stop=(l == 31))
            P.cp(DVE, kcon, kcon[:], pc1, pc1[:, 0:1])
            pev_rep = P.sbuf([64, 32, 128], BF16, "pev_rep")
            for l in range(32):
                P.ts(DVE, pev_rep, pev_rep[:, l, :], onesm, onesm[0:64, :], pev_b[:, l:l + 1], None, ALU.mult, extra=[pev_b])
            for l in range(32):
                P.mm(pc3, pc3[:], pev_rep, pev_rep[:, l, :], wv_b, wv_b[:, l, :], start=(l == 0), stop=(l == 31))
            P.cp(DVE, vcon, vcon[:], pc3, pc3[:])
            for g in range(2):
                P.dma(SP, kcm, kcm[:], featT, featT[1536 + g * 64:1536 + (g + 1) * 64, :])
                P.dma(SP, vtk, vtk[:], vtok, vtok.t[:, 512 + g * 64:512 + (g + 1) * 64].rearrange("(n p) d -> p n d", p=128))
                for n in range(NT):
                    P.tr(pcT, pcT[:], vtk, vtk[:, n, :], identb)
                    P.cp(ACT, vcm, vcm[:, n * 128:(n + 1) * 128], pcT, pcT[:])
                for ct in range(NCT):
                    nc_ = min(128, NCMP - ct * 128)
                    c0 = ct * 128
                    for l in range(32):
                        P.mm(pc1, pc1[:, 0:nc_], wk_b, wk_b[:, l, :], kcm, kcm[:, 16 * c0 + l:16 * c0 + l + 16 * (nc_ - 1) + 1:16], start=(l == 0), stop=(l == 31))
                    P.act(kcT, kcT[:, g, c0:c0 + nc_], pc1, pc1[:, 0:nc_], AF.Identity, bias=kcon[:, 0:1], extra=[kcon])
                    for l in range(32):
                        P.mm(pc2, pc2[0:nc_, :], vcm, vcm[:, 16 * c0 + l:16 * c0 + l + 16 * (nc_ - 1) + 1:16], wv_b, wv_b[:, l, :], start=(l == 0), stop=(l == 31))
                    P.tt(DVE, Vc, Vc[0:nc_, g, ct, 0:64], pc2, pc2[0:nc_, :], vcon, vcon[0:nc_, :], ALU.add)

        ovb = P.sbuf([128, NCT, NSEL + 1], BF16, "ovb")
        sA = P.sbuf([128, NT, NSEL], BF16, "sA"); sV = P.sbuf([128, NT, NSEL], BF16, "sV")
        Xb = P.sbuf([NSEL, NT, 128], BF16, "Xb")
        with P.scope():
            ovf = P.sbuf([128, NCT, NSEL + 1], F32, "ovf")
            P.dma(SP, ovf, ovf[:], ovl, ovl.t.rearrange("(c p) j -> p c j", p=128)); P.cp(DVE, ovb, ovb[:], ovf, ovf[:])
            sf = P.sbuf([128, NT * NSEL], F32, "sf")
            P.dma(SP, sf, sf[:], selA, selA[:, :]); P.cp(DVE, sA, sA.t.rearrange("p n j -> p (n j)"), sf, sf[:])
            P.dma(SP, sf, sf[:], selV, selV[:, :]); P.cp(DVE, sV, sV.t.rearrange("p n j -> p (n j)"), sf, sf[:])
            Xf = P.sbuf([NSEL, NT * 128], F32, "Xf")
            P.dma(SP, Xf, Xf[:], Xp, Xp[:, :]); P.cp(POOL, Xb, Xb.t.rearrange("j n p -> j (n p)"), Xf, Xf[:])
        ksl = P.sbuf([128, S], BF16, "ksl"); kwn = P.sbuf([128, S], BF16, "kwn")
        kc2 = P.sbuf([128, NCT * 128], BF16, "kc2")
        for g in range(2):
            P.dma(SP, kc2, kc2[64 * g:64 * g + 64, :], kcT, kcT[:, g, :])
        P.dma(SP, ksl, ksl[:], featT, featT[1664:1792, :])
        P.dma(SP, kwn, kwn[:], featT, featT[1792:1920, :])
        Vs = P.sbuf([128, NT, 65], BF16, "Vs"); Vw = P.sbuf([128, NT, 65], BF16, "Vw")
        P.memset(POOL, Vs, Vs[:, :, 64:65], 1.0); P.memset(POOL, Vw, Vw[:, :, 64:65], 1.0)
        wB_ = P.sbuf([128, 4, 6, 512], BF16, "winB"); cB_ = P.sbuf([128, 4, 5, 512], BF16, "cmpB")
        wS_ = P.sbuf([128, 3, 512], BF16, "winS")
        bst = [P.sbuf([128, 512], F32, f"bst{i}") for i in range(2)]
        mst = [P.sbuf([128, 512], F32, f"mst{i}") for i in range(2)]
        q4 = [P.sbuf([128, 4, 512], BF16, f"q4{i}") for i in range(2)]
        psS = [P.psum([128, 512], F32, f"nS{i}") for i in range(3)]
        psO = [P.psum([128, 4, 65], F32, f"nO{i}") for i in range(3)]
        psI = P.psum([128, 4, NSEL + 1], F32, "nI")
        psX = P.psum([NSEL, 512], BF16, "nX")
        pT = [P.sbuf([128, 512], BF16, f"npT{i}") for i in range(4)]
        rec = P.sbuf([128, 4], F32, "nrec"); fac = P.sbuf([128, 4], F32, "nfac")
        imp = P.sbuf([128, 4, NSEL], F32, "imp"); sc_ = P.sbuf([128, NSEL], F32, "score"); sc2 = P.sbuf([128, NSEL], F32, "score2")
        m8 = P.sbuf([128, 16], F32, "m8"); sbt = P.sbuf([128, NSEL], BF16, "sbt")
        selT = P.sbuf([NSEL, 512], BF16, "selT")
        yacc = P.sbuf([128, 4, 4, 64], F32, "yacc")
        si = 0; oi = 0; bi = 0

        def att_tile(k_b, k_ap, q_b, q_ap, extra_mms, bias_ap, bias_b, v_b, v_ap, O, ttmask, first, lastk):
            nonlocal si
            st = psS[si % len(psS)]; p_ = pT[si % len(pT)]; si += 1
            tts = [tt for tt in range(4) if ttmask(tt)]
            c0, c1 = 128 * tts[0], 128 * (tts[-1] + 1)
            P.mm(st, st[:, c0:c1], k_b, k_ap, q_b, q_ap[:, c0:c1], start=True, stop=not extra_mms)
            for i_, (lb, l, rb, r) in enumerate(extra_mms):
                P.mm(st, st[:, c0:c1], lb, l, rb, r[:, c0:c1], start=False, stop=(i_ == len(extra_mms) - 1))
            flush(keep=1)
            P.act(p_, p_[:, c0:c1], st, st[:, c0:c1], AF.Exp, bias=bias_ap, extra=[bias_b])

            def pv(first=first):
                for tt in range(4):
                    if ttmask(tt):
                        P.mm(O, O[:, tt, :], p_, p_[:, tt * 128:(tt + 1) * 128], v_b, v_ap, start=first, stop=lastk(tt))
                        first = False
            pend.append((True, pv))
            return False, p_

        pend = []

        def flush(keep=0):
            idx = len(pend)
            if keep:
                starts = [i_ for i_, (st_, _) in enumerate(pend) if st_]
                idx = starts[-keep] if len(starts) >= keep else 0
            for _, f_ in pend[:idx]:
                f_()
            del pend[:idx]

        def cmp_imp(p_, k, firstI, lastI):
            for tt in range(4):
                P.mm(psI, psI[:, tt, :], p_, p_[:, tt * 128:(tt + 1) * 128], ovb, ovb[:, k, :], start=firstI, stop=lastI)
                firstI = False

        def cmp_norm(O, c, h, hh):
            P.ts(DVE, rec, rec[:], O, O[:, :, 64], 1e-30, None, ALU.add)
            P.call(DVE, "reciprocal", [rec], [rec], out=rec[:], in_=rec[:])
            for tt in range(4):
                n = 4 * c + tt
                P.tt(DVE, fac, fac[:, tt:tt + 1], rec, rec[:, tt:tt + 1], small, small[:, n, 8 + 3 * h:9 + 3 * h], ALU.mult)
                P.ts(DVE, yacc, yacc[:, tt, hh, :], O, O[:, tt, 0:64], fac[:, tt:tt + 1], None, ALU.mult, extra=[fac])
                if hh == 0:
                    P.ts(DVE, imp, imp[:, tt, :], psI, psI[:, tt, 0:NSEL], rec[:, tt:tt + 1], None, ALU.mult, extra=[rec])
                else:
                    P.stt(imp, imp[:, tt, :], psI, psI[:, tt, 0:NSEL], rec[:, tt:tt + 1], imp, imp[:, tt, :], ALU.mult, ALU.add, extra=[rec])

        def sw_norm(O, Ow, c, h, hh):
            for (Ox, gi) in ((O, 1), (Ow, 2)):
                P.ts(DVE, rec, rec[:], Ox, Ox[:, :, 64], 1e-30, None, ALU.add)
                P.call(DVE, "reciprocal", [rec], [rec], out=rec[:], in_=rec[:])
                for tt in range(4):
                    n = 4 * c + tt
                    P.tt(DVE, fac, fac[:, tt:tt + 1], rec, rec[:, tt:tt + 1], small, small[:, n, 8 + 3 * h + gi:9 + 3 * h + gi], ALU.mult)
                    P.stt(yacc, yacc[:, tt, hh, :], Ox, Ox[:, tt, 0:64], fac[:, tt:tt + 1], yacc, yacc[:, tt, hh, :], ALU.mult, ALU.add, extra=[fac])
            for tt in range(4):
                P.cp(POOL, ynsa, ynsa[:, 4 * c + tt, h * 64:(h + 1) * 64], yacc, yacc[:, tt, hh, :])

        for v in range(3):
            m1 = mst[v % 2]
            P.dma(SP, m1, m1[:], winM, winM[v, :, :]); P.cp(POOL, wS_, wS_[:, v, :], m1, m1[:])
        for g in range(2):
            for i in range(2):
                P.memset(POOL, q4[i], q4[i][:], 0.0)
            P.dma(SP, Vs, Vs[:, :, 0:64], vtok, vtok.t[:, 640 + g * 64:640 + (g + 1) * 64].rearrange("(n p) d -> p n d", p=128))
            P.dma(SP, Vw, Vw[:, :, 0:64], vtok, vtok.t[:, 768 + g * 64:768 + (g + 1) * 64].rearrange("(n p) d -> p n d", p=128))
            for hh in range(4):
                h = 4 * g + hh
                for (G_, M_, B_, vs_) in ((winG, winM, wB_, (3, 4, 5, 6, 7, 8)), (cmpG, cmpM, cB_, (0, 1, 2, 3, 4))):
                    for vi, v in enumerate(vs_):
                        b1 = bst[bi % 2]; m1 = mst[bi % 2]; bi += 1
                        P.dma(SP, b1, b1[:], G_, G_[h, v, :, :]); P.dma(SP, m1, m1[:], M_, M_[v, :, :])
                        P.tt(POOL, B_, B_[:, hh, vi, :], b1, b1[:], m1, m1[:], ALU.add)
            for c in range(NCH):
                q_ = q4[c % 2]
                P.dma(SP, q_, q_[64 * g:64 * g + 64, :, :], featT, featT.t[1024 + g * 256:1024 + (g + 1) * 256, c * 512:(c + 1) * 512].rearrange("(h d) t -> d h t", d=64))
                for hh in range(4):
                    h = 4 * g + hh
                    O = psO[oi % len(psO)]; oi += 1
                    first = True; firstI = True
                    kts = [k for k in range(NCT) if c - 4 * k >= 0]
                    for k in kts:
                        r = c - 4 * k
                        if r <= 4:
                            ex = [(identb, identb[:], cB_, cB_[:, hh, r, :])]; bap = zcol[:, 0:1]; bb = zcol
                        else:
                            ex = []; bap = farcs[:, h:h + 1]; bb = farcs
                        first, p_ = att_tile(kc2, kc2[:, k * 128:(k + 1) * 128], q_, q_[:, hh, :], ex, bap, bb,
                                             Vc, Vc[:, g, k, :], O, lambda tt: True, first, lambda tt, k=k: k == kts[-1])
                        pend.append((False, lambda p_=p_, k=k, firstI=firstI, lastI=(k == kts[-1]): cmp_imp(p_, k, firstI, lastI)))
                        firstI = False
                    pend.append((False, lambda O=O, c=c, h=h, hh=hh: cmp_norm(O, c, h, hh)))
                flush()
                for tt in range(4):
                    n = 4 * c + tt
                    P.tt(DVE, sc_, sc_[:], imp, imp[:, tt, :], sV, sV[:, n, :], ALU.mult)
                    P.tt(DVE, sc_, sc_[:], sc_, sc_[:], sA, sA[:, n, :], ALU.add)
                    P.call(DVE, "max", [sc_], [m8], out=m8[:, 0:8], in_=sc_[:])
                    if TOPN > 8:
                        P.call(DVE, "match_replace", [sc_, m8], [sc2], out=sc2[:], in_to_replace=m8[:, 0:8], in_values=sc_[:], imm_value=-3e38)
                        P.call(DVE, "max", [sc2], [m8], out=m8[:, 8:16], in_=sc2[:])
                    P.ts(DVE, sc2, sc2[:], sc_, sc_[:], m8[:, TOPN - 1:TOPN], None, ALU.is_ge, extra=[m8])
                    P.ts(DVE, sbt, sbt[:], sc2, sc2[:], -1.0, -NEGM, ALU.add, ALU.mult)
                    P.tr(psX, psX[:, tt * 128:(tt + 1) * 128], sbt, sbt[:], identb)
                P.cp(ACT, selT, selT[:], psX, psX[:])
                for hh in range(4):
                    h = 4 * g + hh
                    O = psO[oi % len(psO)]; oi += 1
                    first = True
                    for k in range(4 * c + 4):
                        d = k - 4 * c
                        ex = [(Xb, Xb[:, k, :], selT, selT[:])]
                        if d >= -1:
                            ex.append((identb, identb[:], wB_, wB_[:, hh, 5 if d == -1 else d + 1, :])); bap = zcol[:, 0:1]; bb = zcol
                        else:
                            bap = farcs[:, h:h + 1]; bb = farcs
                        first, _ = att_tile(ksl, ksl[:, k * 128:(k + 1) * 128], q_, q_[:, hh, :], ex, bap, bb,
                                            Vs, Vs[:, k, :], O, lambda tt, k=k: k <= 4 * c + tt, first, lambda tt, k=k: k == 4 * c + tt)
                    Ow = psO[oi % len(psO)]; oi += 1
                    first = True
                    k0 = max(0, 4 * c - 4)
                    for k in range(k0, 4 * c + 4):
                        d = k - 4 * c
                        if d <= -2:
                            ex = [(identb, identb[:], wS_, wS_[:, d + 4, :])]; bap = farcs[:, h:h + 1]; bb = farcs
                        else:
                            ex = [(identb, identb[:], wB_, wB_[:, hh, d + 1, :])]; bap = zcol[:, 0:1]; bb = zcol
                        first, _ = att_tile(kwn, kwn[:, k * 128:(k + 1) * 128], q_, q_[:, hh, :], ex, bap, bb,
                                            Vw, Vw[:, k, :], Ow, lambda tt, k=k: (k <= 4 * c + tt) and (k >= 4 * c + tt - 4), first,
                                            lambda tt, k=k: k == 4 * c + tt)
                    pend.append((False, lambda O=O, Ow=Ow, c=c, h=h, hh=hh: sw_norm(O, Ow, c, h, hh)))
                flush()
        if dbg:
            P.dma(SP, dyn, dyn[:, :], ynsa, ynsa.t.rearrange("p n f -> p (n f)"), sem_buf=ynsa)
            fin += [ynsa]

    if phases >= 4:
      with P.scope():
        wfb = P.sbuf([128, 4, D], BF16, "wfb"); wnb = P.sbuf([128, 4, D], BF16, "wnb"); wmb = P.sbuf([128, 8, D], BF16, "wmb")
        wgb = P.sbuf([128, 8, NG], BF16, "wgb"); wrt = P.sbuf([128, 8, 32], F32, "wrt"); brt = P.sbuf([128, 32], F32, "brt")
        with P.scope():
            stg = [P.sbuf([128, 1024], F32, f"dstg{i}") for i in range(2)]
            i = 0
            for (wd_, src, nk) in ((wfb, wfp, 4), (wnb, wnp, 4), (wmb, wmo, 8)):
                sv = src.t.rearrange("(k p) n -> p k n", p=128)
                for k in range(nk):
                    st = stg[i % 2]
                    P.dma(SP, st, st[:, 0:D], src, sv[:, k, :]); P.cp(ACT if i % 2 else DVE, wd_, wd_[:, k, :], st, st[:, 0:D]); i += 1
            wv = w_in.t.rearrange("(k p) n -> p k n", p=128)
            for k in range(8):
                for hf in range(2):
                    st = stg[i % 2]
                    P.dma(SP, st, st[:], w_in, wv[:, k, 2848 + hf * 1024:2848 + (hf + 1) * 1024])
                    P.cp(ACT if i % 2 else DVE, wgb, wgb[:, k, hf * 1024:(hf + 1) * 1024], st, st[:]); i += 1
        P.dma(SP, wrt, wrt[:], w_router, w_router.t.rearrange("(k p) n -> p k n", p=128))
        P.dma(SP, brt, brt[:], b_router, b_router[:, :])

        def mk(c_):
            d = {}
            d["hl"] = P.sbuf([128, 8, 128], BF16, f"hTl{c_}"); d["yf"] = P.sbuf([128, 512], BF16, f"yfl{c_}")
            d["xi"] = P.sbuf([128, D], F32, f"dx{c_}"); d["yT"] = P.sbuf([128, 8, 128], BF16, f"yT{c_}")
            d["ptb"] = P.psum([128, D], BF16, f"dptb{c_}"); d["pa"] = P.psum([128, 512], F32, f"dpa{c_}")
            d["pb"] = P.psum([128, 512], F32, f"dpb{c_}"); d["pq"] = P.psum([128, 512], F32, f"dpq{c_}")
            d["s0"] = P.sbuf([128, 512], F32, f"ds0{c_}"); d["s1"] = P.sbuf([128, 512], F32, f"ds1{c_}")
            d["tmp"] = P.sbuf([128, 512], F32, f"dtmp{c_}"); d["mtok"] = P.sbuf([128, D], BF16, f"mtok{c_}")
            d["mT"] = P.sbuf([128, 8, 128], BF16, f"mT{c_}")
            d["ss"] = P.sbuf([128, 1], F32, f"dss{c_}"); d["rstd"] = P.sbuf([128, 1], F32, f"drstd{c_}")
            d["x1"] = P.sbuf([128, D], F32, f"x1{c_}"); d["xn2"] = P.sbuf([128, D], F32, f"xn2{c_}")
            d["h2f"] = P.sbuf([128, 8, 128], F32, f"h2f{c_}"); d["hb"] = P.sbuf([128, 8, 128], BF16, f"h2b{c_}")
            d["lg"] = P.sbuf([128, 32], F32, f"lg{c_}"); d["m8"] = P.sbuf([128, 8], F32, f"dm8{c_}"); d["msk"] = P.sbuf([128, 32], F32, f"msk{c_}")
            d["ex"] = P.sbuf([128, 32], F32, f"ex{c_}"); d["nmx"] = P.sbuf([128, 1], F32, f"nmx{c_}"); d["sm"] = P.sbuf([128, 1], F32, f"sm{c_}")
            return d
        chains = [mk(0), mk(1)]

        def tile_d(n, d):
            hl, yf_, xi, yT, ptb, pa, pb, pq = d["hl"], d["yf"], d["xi"], d["yT"], d["ptb"], d["pa"], d["pb"], d["pq"]
            s0, s1, tmp, mtok, mT, ss, rstd, x1_, xn2, h2f, hb = (d[k_] for k_ in ("s0", "s1", "tmp", "mtok", "mT", "ss", "rstd", "x1", "xn2", "h2f", "hb"))
            lg, m8, msk, ex_, nmx, sm = (d[k_] for k_ in ("lg", "m8", "msk", "ex", "nmx", "sm"))
            P.dma(SP, hl, hl[:], hTs, hTs[:, :, n * 128:(n + 1) * 128])
            P.dma(SP, xi, xi[:], x, x[n * 128:(n + 1) * 128, :])
            P.dma(SP, yf_, yf_[:], yfs, yfs[:, n * 512:(n + 1) * 512])
            yield
            for j in range(4):
                P.tr(ptb, ptb[:, j * 128:(j + 1) * 128], yf_, yf_[:, j * 128:(j + 1) * 128], identb)
                P.tr(ptb, ptb[:, (4 + j) * 128:(5 + j) * 128], ynsa, ynsa[:, n, j * 128:(j + 1) * 128], identb)
            P.cp(ACT, yT, yT.t.rearrange("p k t -> p (k t)"), ptb, ptb[:])
            yield
            for hf in range(2):
                for j in range(4):
                    P.mm(pa, pa[:], yT, yT[:, j, :], wfb, wfb[:, j, hf * 512:(hf + 1) * 512], start=(j == 0), stop=(j == 3))
                for j in range(4):
                    P.mm(pb, pb[:], yT, yT[:, 4 + j, :], wnb, wnb[:, j, hf * 512:(hf + 1) * 512], start=(j == 0), stop=(j == 3))
                for k in range(8):
                    P.mm(pq, pq[:], hl, hl[:, k, :], wgb, wgb[:, k, hf * 512:(hf + 1) * 512], start=(k == 0), stop=(k == 7))
                P.act(s0, s0[:], pq, pq[:], AF.Sigmoid)
                yield
                for k in range(8):
                    P.mm(pq, pq[:], hl, hl[:, k, :], wgb, wgb[:, k, 1024 + hf * 512:1024 + (hf + 1) * 512], start=(k == 0), stop=(k == 7))
                P.act(s1, s1[:], pq, pq[:], AF.Sigmoid)
                P.tt(DVE, tmp, tmp[:], s0, s0[:], pa, pa[:], ALU.mult)
                yield
                P.tt(DVE, s1, s1[:], s1, s1[:], pb, pb[:], ALU.mult)
                P.tt(DVE, mtok, mtok[:, hf * 512:(hf + 1) * 512], s1, s1[:], tmp, tmp[:], ALU.add)
                yield
            for k in range(8):
                P.tr(ptb, ptb[:, k * 128:(k + 1) * 128], mtok, mtok[:, k * 128:(k + 1) * 128], identb)
            P.cp(ACT, mT, mT.t.rearrange("p k t -> p (k t)"), ptb, ptb[:])
            yield
            for hf, p_ in ((0, pa), (1, pb)):
                for k in range(8):
                    P.mm(p_, p_[:], mT, mT[:, k, :], wmb, wmb[:, k, hf * 512:(hf + 1) * 512], start=(k == 0), stop=(k == 7))
            yield
            for hf, p_ in ((0, pa), (1, pb)):
                P.act(xn2, xn2[:, hf * 512:(hf + 1) * 512], p_, p_[:], AF.Square)
            P.call(DVE, "reduce_sum", [xn2], [ss], out=ss[:], in_=xn2[:], axis=AX.X)
            P.ts(DVE, ss, ss[:], ss, ss[:], 1.0 / D, RMS_EPS, ALU.mult, ALU.add)
            yield
            P.act(ss, ss[:], ss, ss[:], AF.Sqrt)
            P.call(DVE, "reciprocal", [ss], [rstd], out=rstd[:], in_=ss[:])
            yield
            for hf, p_ in ((0, pa), (1, pb)):
                sl = slice(hf * 512, (hf + 1) * 512)
                P.stt(x1_, x1_[:, sl], p_, p_[:], rstd[:, 0:1], G1, G1[:, sl], ALU.mult, ALU.mult, extra=[rstd])
            P.tt(DVE, x1_, x1_[:], x1_, x1_[:], xi, xi[:], ALU.add)
            P.dma(POOL, x1s, x1s[n * 128:(n + 1) * 128, :], x1_, x1_[:], sem_buf=x1_)
            yield
            P.act(xn2, xn2[:], x1_, x1_[:], AF.Square)
            P.call(DVE, "reduce_sum", [xn2], [ss], out=ss[:], in_=xn2[:], axis=AX.X)
            P.ts(DVE, ss, ss[:], ss, ss[:], 1.0 / D, RMS_EPS, ALU.mult, ALU.add)
            yield
            P.act(ss, ss[:], ss, ss[:], AF.Sqrt)
            P.call(DVE, "reciprocal", [ss], [rstd], out=rstd[:], in_=ss[:])
            P.ts(DVE, xn2, xn2[:], x1_, x1_[:], rstd[:, 0:1], None, ALU.mult, extra=[rstd])
            yield
            for hf, p_ in ((0, pa), (1, pb)):
                for k in range(4):
                    kk = hf * 4 + k
                    P.tr(p_, p_[:, k * 128:(k + 1) * 128], xn2, xn2[:, kk * 128:(kk + 1) * 128], identf)
            yield
            for kk in range(8):
                p_ = pa if kk < 4 else pb
                P.act(h2f, h2f[:, kk, :], p_, p_[:, (kk % 4) * 128:(kk % 4 + 1) * 128], AF.Identity,
                      bias=B2[:, kk:kk + 1], scale=A2[:, kk:kk + 1], extra=[A2, B2])
            P.cp(DVE, hb, hb[:], h2f, h2f[:])
            P.dma(POOL, h2Ts, h2Ts[:, :, n * 128:(n + 1) * 128], hb, hb[:], sem_buf=hb)
            yield
            for k in range(8):
                P.mm(pq, pq[:, 0:32], h2f, h2f[:, k, :], wrt, wrt[:, k, :], start=(k == 0), stop=(k == 7))
            P.tt(DVE, lg, lg[:], pq, pq[:, 0:32], brt, brt[:], ALU.add)
            P.call(DVE, "max", [lg], [m8], out=m8[:], in_=lg[:])
            yield
            P.ts(DVE, msk, msk[:], lg, lg[:], m8[:, 3:4], None, ALU.is_ge, extra=[m8])
            P.ts(DVE, nmx, nmx[:], m8, m8[:, 0:1], -1.0, None, ALU.mult)
            P.act(ex_, ex_[:], lg, lg[:], AF.Exp, bias=nmx[:, 0:1], extra=[nmx])
            yield
            P.tt(DVE, ex_, ex_[:], ex_, ex_[:], msk, msk[:], ALU.mult)
            P.call(DVE, "reduce_sum", [ex_], [sm], out=sm[:], in_=ex_[:], axis=AX.X)
            P.call(DVE, "reciprocal", [sm], [sm], out=sm[:], in_=sm[:])
            P.ts(DVE, rw, rw[:, n, :], ex_, ex_[:], sm[:, 0:1], None, ALU.mult, extra=[sm])

        for n0 in range(0, NT, 2):
            gens = [tile_d(n0, chains[0]), tile_d(n0 + 1, chains[1])]
            live = [True, True]
            while any(live):
                for gi_ in range(2):
                    if live[gi_]:
                        try:
                            next(gens[gi_])
                        except StopIteration:
                            live[gi_] = False
        if dbg:
            P.dma(SP, drw, drw[:, :], rw, rw.t.rearrange("p n e -> p (n e)"), sem_buf=rw)
        fin += [x1s, h2Ts, rw]
    if phases >= 3:
        yscope.__exit__(None, None, None)

    if phases >= 5:
      with P.scope():
        TS = min(S, 1024); NTS = TS // 128; NCS = TS // 512
        acc = P.sbuf([128, NTS, D], F32, "acc")
        h2 = P.sbuf([128, 8, TS], BF16, "h2")
        wgt = [P.sbuf([128, 8, D], BF16, f"wE{i}") for i in range(2)]
        wgf = [[Buf(wgt[m].t, f"wE{m}_{f}", P.base) for f in range(8)] for m in range(2)]
        wdb = [P.sbuf([128, 8, D], BF16, f"wD{i}") for i in range(2)]
        wdk = [[Buf(wdb[b_].t, f"wD{b_}_{k}", P.base) for k in range(8)] for b_ in range(2)]
        stg = [P.sbuf([128, D], F32, f"estg{i}") for i in range(4)]
        hid = [P.sbuf([128, 8, 512], BF16, f"hid{i}") for i in range(2)]
        bgs = P.sbuf([128, E, 8], F32, "bgs"); bus = P.sbuf([128, E, 8], F32, "bus")
        bdall = P.sbuf([32, D], F32, "bdall"); rwT = P.sbuf([32, 128], F32, "rwT")
        P.dma(SP, bdall, bdall[0:E, :], bdn, bdn.t.rearrange("o (e d) -> (o e) d", d=D))
        P.dma(SP, bgs, bgs[:], bgT, bgT.t.rearrange("p (e k) -> p e k", k=8))
        P.dma(SP, bus, bus[:], buT, buT.t.rearrange("p (e k) -> p e k", k=8))
        bg17 = P.sbuf([128, E, 8], F32, "bg17")
        P.ts(DVE, bg17, bg17[:], bgs, bgs[:], 1.702, None, ALU.mult)
        SIGCAP = float(1.0 / (1.0 + math.exp(-1.702 * 7.0)))
        pG = [P.psum([128, 512], F32, f"eG{i}") for i in range(2)]
        pU = [P.psum([128, 512], F32, f"eU{i}") for i in range(2)]
        pY = [P.psum([128, 512], F32, f"eY{i}") for i in range(4)]
        gs = [P.sbuf([128, 512], F32, f"gs{i}") for i in range(2)]
        sg = [P.sbuf([128, 512], F32, f"esg{i}") for i in range(2)]
        us = [P.sbuf([128, 512], F32, f"us{i}") for i in range(2)]
        junk = P.sbuf([128, D], F32, "ejunk"); ss = P.sbuf([128, 1], F32, "ess"); rstd = P.sbuf([128, 1], F32, "erstd")
        x1l = [P.sbuf([128, D], F32, f"x1l{i}") for i in range(1)]
        ot = [P.sbuf([128, D], F32, f"ot{i}") for i in range(1)]
        wsrc = (w_gate, w_up, w_down)
        sti = 0; fi = 0; yi = 0

        def pf_gu(e, f):
            nonlocal sti
            for m in (0, 1):
                st = stg[sti % 4]
                stv = st.t.rearrange("p (k n) -> p k n", k=8)
                P.dma(SP, st, st[:], wsrc[m], wsrc[m].t[e, f, :, :])
                P.cp(ACT, wgf[m][f], wgt[m][:, :, f * 128:(f + 1) * 128], st, stv)
                sti += 1

        def pf_d(e, k, b_):
            nonlocal sti
            st = stg[sti % 4]
            P.dma(SP, st, st[:], w_down, w_down.t[e][k * 128:(k + 1) * 128, :])
            P.cp(ACT, wdk[b_][k], wdb[b_][:, k, :], st, st[:])
            sti += 1

        items = [(sp_, e) for sp_ in range(S // TS) for e in range(E)]
        pdown = []

        def emit_down(hd, wb, bd_b, c, sp_, e):
            nonlocal yi
            for tt in range(4):
                n = c * 4 + tt
                for hf in range(2):
                    Y_ = pY[yi % 4]; yi += 1
                    for f in range(8):
                        P.mm(Y_, Y_[:], hd, hd[:, f, tt * 128:(tt + 1) * 128], wdk[wb][f], wdb[wb][:, f, hf * 512:(hf + 1) * 512], start=(f == 0), stop=(f == 7))
                    sl = slice(hf * 512, (hf + 1) * 512)
                    wcol = rw[:, sp_ * NTS + n, e:e + 1]
                    P.stt(acc, acc[:, n, sl], Y_, Y_[:], wcol, acc, acc[:, n, sl], ALU.mult, ALU.add, extra=[rw])

        def flush_down():
            for f_ in pdown:
                f_()
            pdown.clear()
        for f in range(8):
            pf_gu(0, f)
        for k in range(8):
            pf_d(0, k, 0)
        for it, (sp_, e) in enumerate(items):
            nxt = items[it + 1][1] if it + 1 < len(items) else None
            wb = it % 2
            if True:
                if e == 0:
                    t0 = sp_ * TS
                    P.dma(SP, h2, h2[:], h2Ts, h2Ts[:, :, t0:t0 + TS])
                bd_b = None
                if e == 0:
                    for n in range(NTS):
                        Yt = pY[yi % 4]; yi += 1
                        P.tr(Yt, Yt[0:32, 0:128], rw, rw[:, sp_ * NTS + n, :], identf)
                        P.cp(ACT, rwT, rwT[:], Yt, Yt[0:32, 0:128])
                        for hf in range(2):
                            Yb = pY[yi % 4]; yi += 1
                            P.mm(Yb, Yb[:], rwT, rwT[0:E, :], bdall, bdall[0:E, hf * 512:(hf + 1) * 512])
                            P.cp(ACT, acc, acc[:, n, hf * 512:(hf + 1) * 512], Yb, Yb[:])
                for c in range(NCS):
                    hd = hid[(e * NCS + c) % 2]
                    for f in range(8):
                        G_ = pG[fi % 2]; U_ = pU[fi % 2]; g_ = gs[fi % 2]; s_ = sg[fi % 2]; u_ = us[fi % 2]; fi += 1
                        for k in range(8):
                            P.mm(G_, G_[:], wgf[0][f], wgt[0][:, k, f * 128:(f + 1) * 128], h2, h2[:, k, c * 512:(c + 1) * 512], start=(k == 0), stop=(k == 7))
                        for k in range(8):
                            P.mm(U_, U_[:], wgf[1][f], wgt[1][:, k, f * 128:(f + 1) * 128], h2, h2[:, k, c * 512:(c + 1) * 512], start=(k == 0), stop=(k == 7))
                        P.act(s_, s_[:], G_, G_[:], AF.Sigmoid, bias=bg17[:, e, f:f + 1], scale=1.702, extra=[bg17])
                        P.act(u_, u_[:], U_, U_[:], AF.Identity, bias=bus[:, e, f:f + 1], extra=[bus])
                        P.ts(DVE, g_, g_[:], G_, G_[:], bgs[:, e, f:f + 1], 7.0, ALU.add, ALU.min, extra=[bgs])
                        P.stt(g_, g_[:], s_, s_[:], SIGCAP, g_, g_[:], ALU.min, ALU.mult)
                        P.ts(DVE, u_, u_[:], u_, u_[:], -7.0, 7.0, ALU.max, ALU.min)
                        P.stt(hd, hd[:, f, :], u_, u_[:], 1.0, g_, g_[:], ALU.add, ALU.mult)
                        if f == 1:
                            flush_down()
                        if nxt is not None and c == 0 and f >= 2:
                            pf_d(nxt, f - 2, (it + 1) % 2)
                        if nxt is not None and c == NCS - 1:
                            pf_gu(nxt, f)
                    if nxt is not None and c == 0:
                        pf_d(nxt, 6, (it + 1) % 2); pf_d(nxt, 7, (it + 1) % 2)
                    pdown.append(lambda hd=hd, wb=wb, bd_b=bd_b, c=c, sp_=sp_, e=e: emit_down(hd, wb, bd_b, c, sp_, e))
            if e != E - 1:
                continue
            flush_down()
            for n in range(NTS):
                gn = sp_ * NTS + n
                xl = x1l[0]; o_ = ot[0]
                P.dma(SP, xl, xl[:], x1s, x1s[gn * 128:(gn + 1) * 128, :])
                rstd_of(acc, acc[:, n, :], junk, ss, rstd)
                P.stt(o_, o_[:], acc, acc[:, n, :], rstd[:, 0:1], G2, G2[:], ALU.mult, ALU.mult, extra=[rstd])
                P.tt(POOL, o_, o_[:], o_, o_[:], xl, xl[:], ALU.add)
                P.dma(POOL, out, out[gn * 128:(gn + 1) * 128, :], o_, o_[:], sem_buf=o_)
                fin.append(o_)
    P.wait_all(SP, fin)
    P.wait_all(POOL, fin)
    P.emit()
    return nc


def _bucket(n):
    n = np.maximum(n, 0)
    nf = np.maximum(n, 1).astype(np.float32)
    large = 16 + (np.log(nf / np.float32(16)) / np.float32(math.log(128 / 16)) * np.float32(16)).astype(np.int32)
    large = np.minimum(large, 31)
    return np.where(n < 16, n, large).astype(np.int64)


def host_consts(S):
    NT, NSEL = S // 128, S // 64
    NCMP = (S - 32) // 16 + 1
    NCT = (NCMP + 127) // 128
    p = np.arange(128)[:, None]; j = np.arange(512)[None, :]
    winM = np.zeros((9, 128, 512), np.float32); winI = np.zeros((9, 128, 512), np.int64)
    for idx in range(9):
        d = -1 if idx == 8 else idx - 4
        dist = j - (128 * d + p)
        ok = (dist >= 0) if idx == 8 else ((dist >= 0) & (dist < 512))
        winM[idx] = np.where(ok, 0.0, NEGM); winI[idx] = _bucket(dist)
    cmpM = np.zeros((5, 128, 512), np.float32); cmpI = np.zeros((5, 128, 512), np.int64)
    for r in range(5):
        nn = 512 * r + j - 16 * p - 31
        cmpM[r] = np.where(nn >= 0, 0.0, NEGM); cmpI[r] = _bucket(nn)
    caus = np.zeros((4, 128, 512), np.float32)
    for d in range(4):
        caus[d] = np.where(128 * d + p <= j, 0.0, NEGM)
    cs = np.arange(NCT * 128)[:, None] * 16; ss = np.arange(NSEL)[None, :] * 64
    ov = ((cs < ss + 64) & (cs + 32 > ss) & (np.arange(NCT * 128)[:, None] < NCMP)).astype(np.float32)
    ovl = np.concatenate([ov, np.ones((NCT * 128, 1), np.float32)], axis=1)
    t = np.arange(S)[:, None]; jb = np.arange(NSEL)[None, :]
    cur = t // 64
    forced = (jb == 0) | (jb == cur) | (jb == cur - 1)
    svalid = jb * 64 <= t
    A = np.where(forced, BIG, np.where(svalid, 0.0, -BIG)).astype(np.float32)
    Vm = (svalid & ~forced).astype(np.float32)
    tom = lambda a: np.ascontiguousarray(a.reshape(NT, 128, NSEL).transpose(1, 0, 2).reshape(128, NT * NSEL))
    Xp = np.zeros((NSEL, NT, 128), np.float32)
    for k in range(NT):
        Xp[2 * k, k, :64] = 1.0; Xp[2 * k + 1, k, 64:] = 1.0
    tri = (np.arange(128)[:, None] <= np.arange(128)[None, :]).astype(np.float32)
    return dict(winM=winM, cmpM=cmpM, caus=caus, ovl=ovl, selA=tom(A), selV=tom(Vm), Xp=Xp.reshape(NSEL, NT * 128),
                tri=tri), winI, cmpI


def _fblk(w):
    E_ = w.shape[0]
    return np.ascontiguousarray(w.reshape(E_, 8, 128, 8, 128).transpose(0, 3, 2, 1, 4).reshape(E_, 8, 128, 1024), dtype=np.float32)


def host_inputs(inp, b, S, E, consts, winI, cmpI):
    f = lambda a: np.ascontiguousarray(a, dtype=np.float32)
    NT = S // 128
    colT = lambda v: f(v.reshape(8, 128).T)
    rep = lambda v: f(np.broadcast_to(v[None, :], (128, v.shape[0])))
    rb = inp["rel_bias"]
    m = dict(consts)
    m.update(
        x=f(inp["x"][b, :S]), cT=colT(inp["c"][b]), w_ada=f(inp["w_ada"][0]), b_ada=f(inp["b_ada"][0][None, :]),
        gpre1=colT(inp["g_mix_pre"][0]), gpre2=colT(inp["g_ffn_pre"][0]),
        gpost1=rep(inp["g_mix_post"][0]), gpost2=rep(inp["g_ffn_post"][0]),
        w_in=f(inp["w_in"][0][:, w_in_perm()]), bfg=f(np.tile(rep(inp["b_forget"][0]), (1, NT))),
        pekT=f(inp["pe_k"][0].T), pevT=f(inp["pe_v"][0].T), wck=f(inp["w_cmp_k"][0]), wcv=f(inp["w_cmp_v"][0]),
        wfp=f(inp["w_fox_proj"][0]), wnp=f(inp["w_nsa_proj"][0]), wmo=f(inp["w_mix_out"][0]),
        winG=f(rb[winI].transpose(3, 0, 1, 2)), cmpG=f(rb[cmpI].transpose(3, 0, 1, 2)), farc=rep(rb[31]),
        w_router=f(inp["w_router"][0]), b_router=rep(inp["b_router"][0]),
        w_gate=_fblk(inp["w_gate"][0][:E]), w_up=_fblk(inp["w_up"][0][:E]), w_down=f(inp["w_down"][0][:E]),
        bgT=f(inp["b_gate"][0][:E].reshape(E, 8, 128).transpose(2, 0, 1).reshape(128, E * 8)),
        buT=f(inp["b_up"][0][:E].reshape(E, 8, 128).transpose(2, 0, 1).reshape(128, E * 8)),
        bdn=f(inp["b_down"][0][:E].reshape(1, E * 1024)),
    )
    return m


_NC_CACHE = {}


def kernel(**inputs):
    S, E, B = 4096, 32, 8
    if "nc" not in _NC_CACHE:
        _NC_CACHE["nc"] = build(S, E)
    nc = _NC_CACHE["nc"]
    inp = {k: np.asarray(v) for k, v in inputs.items()}
    consts, winI, cmpI = host_consts(S)
    shared = host_inputs(inp, 0, S, E, consts, winI, cmpI)
    in_maps = []
    for b in range(B):
        m = dict(shared)
        m["x"] = np.ascontiguousarray(inp["x"][b], dtype=np.float32)
        m["cT"] = np.ascontiguousarray(inp["c"][b].reshape(8, 128).T, dtype=np.float32)
        in_maps.append(m)
    res = run_bass_kernel_spmd(nc, in_maps, core_ids=list(range(B)))
    return np.stack([np.asarray(r["out"], dtype=np.float32) for r in res.results], axis=0)
```

```python
import math
from contextlib import ExitStack
import numpy as np
import concourse.bass as bass
import concourse.mybir as mybir
from concourse.bass_utils import run_bass_kernel_spmd

F32 = mybir.dt.float32
BF16 = mybir.dt.bfloat16
AF = mybir.ActivationFunctionType
ALU = mybir.AluOpType
AX = mybir.AxisListType

PE, ACT, DVE, POOL, SP = "tensor", "scalar", "vector", "gpsimd", "sync"
ENGS = (PE, ACT, DVE, POOL, SP)
NEGM = -30000.0
BIG = 1e9


class Buf:
    __slots__ = ("t", "name", "lw", "rd", "sem", "ndma", "multi", "psum")

    def __init__(self, t, name, base=None):
        self.t = t
        self.name = name
        self.lw = dict(base) if base else {}
        self.rd = {}
        self.sem = None
        self.ndma = 0
        self.multi = False
        self.psum = False

    def __getitem__(self, idx):
        return self.t[idx]


class Prog:
    def __init__(self, nc):
        self.nc = nc
        self.es = ExitStack()
        self.scopes = []
        self.q = {e: [] for e in ENGS}
        self.esem = {}
        self.pesems = set()
        self.ecnt = {e: 0 for e in ENGS}
        self.known = {e: {} for e in ENGS}
        self.base = {}
        self.nsem = 0
        self.sem_eng = {}
        for e in (PE, ACT, DVE, POOL):
            self._newsem(e)
        self.nbuf = 0

    def _sem(self, name):
        self.nsem += 1
        return self.es.enter_context(self.nc.semaphore(f"{name}_{self.nsem}"))

    def _newsem(self, e):
        s = self._sem("s_" + e)
        self.esem[e] = s
        self.sem_eng[s] = e
        self.ecnt[e] = 0
        if e == PE:
            self.pesems.add(s)

    class _Scope:
        def __init__(self, P):
            self.P = P
            self.es = ExitStack()
            self.bufs = []

        def __enter__(self):
            self.P.scopes.append(self)
            return self

        def __exit__(self, *a):
            P = self.P
            P.scopes.pop()
            for b in self.bufs:
                for d in (b.lw, b.rd):
                    for s, v in d.items():
                        if P.base.get(s, 0) < v:
                            P.base[s] = v
            self.es.close()
            return False

    def scope(self):
        return Prog._Scope(self)

    def _reg(self, cm, name):
        sc = self.scopes[-1] if self.scopes else None
        t = (sc.es if sc else self.es).enter_context(cm)
        b = Buf(t, name, self.base)
        if sc:
            sc.bufs.append(b)
        return b

    def sbuf(self, shape, dt, name):
        self.nbuf += 1
        name = f"{name}_{self.nbuf}"
        return self._reg(self.nc.sbuf_tensor(name, list(shape), dt), name)

    def psum(self, shape, dt, name):
        self.nbuf += 1
        name = f"{name}_{self.nbuf}"
        esz = 2 if dt == BF16 else 4
        full = 2048 // esz
        b = self._reg(self.nc.psum_tensor(name, [128, full], dt), name)
        n = 1
        for d_ in shape[1:]:
            n *= d_
        assert n <= full
        v = b.t[0:shape[0], 0:n]
        if len(shape) == 3:
            v = v.rearrange("p (a b) -> p a b", b=shape[2])
        b.t = v
        b.psum = True
        return b

    def dram(self, name, shape, dt, kind="Internal"):
        t = self.nc.dram_tensor(name, list(shape), dt, kind=kind)
        b = Buf(t.ap(), name)
        b.multi = True
        return b

    def _dsem(self, b):
        if b.sem is None or b.ndma >= 1500:
            b.sem = self._sem("d")
            b.ndma = 0
        return b.sem

    def _deps(self, eng, reads, writes):
        waits = {}

        def add(d):
            for s, v in d.items():
                if waits.get(s, 0) < v:
                    waits[s] = v
        for b in reads:
            add(b.lw)
            if b.psum:
                add({s_: v_ for s_, v_ in b.rd.items() if self.sem_eng.get(s_) != eng})
        for b in writes:
            if not b.multi:
                add(b.lw)
            add(b.rd)
        kn = self.known[eng]
        out = []
        for s, v in waits.items():
            if eng == PE and s in self.pesems:
                continue
            if kn.get(s, 0) >= v:
                continue
            kn[s] = v
            out.append((s, v))
        return out

    def _commit(self, tok, reads, writes):
        s, v = tok
        for b in reads:
            if b.rd.get(s, 0) < v:
                b.rd[s] = v
        for b in writes:
            if b.multi:
                if b.lw.get(s, 0) < v:
                    b.lw[s] = v
            else:
                b.lw = {s: v}
                b.rd = {}

    def op(self, eng, fn, reads=(), writes=()):
        waits = self._deps(eng, reads, writes)
        if self.ecnt[eng] >= 30000:
            self._newsem(eng)
        self.ecnt[eng] += 1
        tok = (self.esem[eng], self.ecnt[eng])
        self.q[eng].append((waits, fn, tok[0], 1))
        self._commit(tok, reads, writes)

    def dma(self, eng, out_b, out_ap, in_b, in_ap, sem_buf=None, **kw):
        sb = sem_buf or out_b
        waits = self._deps(eng, [in_b], [out_b])
        sem = self._dsem(sb)
        sb.ndma += 1
        tok = (sem, 16 * sb.ndma)
        self.q[eng].append((waits, lambda e: e.dma_start(out=out_ap, in_=in_ap, **kw), sem, 16))
        self._commit(tok, [in_b], [out_b])

    def wait_all(self, eng, bufs):
        waits = self._deps(eng, [], bufs)
        self.q[eng].append((waits, None, None, 0))

    def mm(self, ob, o, lb, l, rb, r, start=True, stop=True, extra=()):
        self.op(PE, lambda e: e.matmul(o, lhsT=l, rhs=r, start=start, stop=stop, skip_group_check=True),
                [lb, rb] + list(extra), [ob])

    def tr(self, ob, o, ib, i, idb):
        self.op(PE, lambda e: e.transpose(out=o, in_=i, identity=idb[:]), [ib, idb], [ob])

    def act(self, ob, o, ib, i, func, bias=None, scale=None, extra=(), eng=ACT):
        kw = {}
        if bias is not None:
            kw["bias"] = bias
        if scale is not None:
            kw["scale"] = scale
        self.op(eng, lambda e: e.activation(out=o, in_=i, func=func, **kw), [ib] + list(extra), [ob])

    def ts(self, eng, ob, o, ib, i, s1, s2, op0, op1=None, extra=()):
        if op1 is None:
            self.op(eng, lambda e: e.tensor_scalar(out=o, in0=i, scalar1=s1, scalar2=None, op0=op0),
                    [ib] + list(extra), [ob])
        else:
            self.op(eng, lambda e: e.tensor_scalar(out=o, in0=i, scalar1=s1, scalar2=s2, op0=op0, op1=op1),
                    [ib] + list(extra), [ob])

    def tt(self, eng, ob, o, ab, a, bb, b, op):
        self.op(eng, lambda e: e.tensor_tensor(out=o, in0=a, in1=b, op=op), [ab, bb], [ob])

    def stt(self, ob, o, ab, a, sc, bb, b, op0, op1, extra=()):
        self.op(DVE, lambda e: e.scalar_tensor_tensor(out=o, in0=a, scalar=sc, in1=b, op0=op0, op1=op1),
                [ab, bb] + list(extra), [ob])

    def cp(self, eng, ob, o, ib, i):
        if eng == ACT:
            self.op(ACT, lambda e: e.copy(out=o, in_=i), [ib], [ob])
        else:
            self.op(eng, lambda e: e.tensor_copy(out=o, in_=i), [ib], [ob])

    def call(self, eng, name, reads, writes, **kw):
        self.op(eng, lambda e: getattr(e, name)(**kw), reads, writes)

    def memset(self, eng, ob, o, val):
        self.op(eng, lambda e: e.memset(o, val), [], [ob])

    def emit(self):
        nc = self.nc
        with nc.Block() as block:
            for e in ENGS:
                ops = self.q[e]

                def body(eh, ops=ops):
                    for waits, fn, sem, inc in ops:
                        for s, v in waits:
                            eh.wait_ge(s, v)
                        if fn is not None:
                            fn(eh).then_inc(sem, inc)
                getattr(block, e)(body)
        self.es.close()


OFF = dict(fq=0, fk=512, fv=1024, ff=1536, nq=1544, kcm=2056, vcm=2184, ksl=2312, vsl=2440, kwn=2568,
           vwn=2696, ng=2824, mgf=2848, mgn=3872)
NF, NTK, NSM, NG = 1920, 896, 32, 2048
RMS_EPS = 1e-6


def w_in_perm():
    r = lambda a, n: list(range(OFF[a], OFF[a] + n))
    return np.array(r("fq", 512) + r("fk", 512) + r("nq", 512) + r("kcm", 128) + r("ksl", 128) + r("kwn", 128)
                    + r("fv", 512) + r("vcm", 128) + r("vsl", 128) + r("vwn", 128)
                    + r("ff", 8) + r("ng", 24) + r("mgf", 1024) + r("mgn", 1024))


def build(S=4096, E=32, dbg=False, phases=5):
    NT, NCH, NSEL = S // 128, S // 512, S // 64
    NCMP = (S - 32) // 16 + 1
    NCT = (NCMP + 127) // 128
    TOPN = min(16, NSEL)
    D = 1024
    nc = bass.Bass("TRN2", target_bir_lowering=False)
    P = Prog(nc)

    def din(name, shape, dt=F32):
        return Buf(nc.dram_tensor(name, list(shape), dt, kind="ExternalInput").ap(), name)

    x = din("x", [S, D]); cT = din("cT", [128, 8]); w_ada = din("w_ada", [D, 6 * D]); b_ada = din("b_ada", [1, 6 * D])
    gpre1 = din("gpre1", [128, 8]); gpre2 = din("gpre2", [128, 8])
    gpost1 = din("gpost1", [128, D]); gpost2 = din("gpost2", [128, D])
    w_in = din("w_in", [D, 4896]); bfg = din("bfg", [128, NT * 8])
    pekT = din("pekT", [64, 32]); pevT = din("pevT", [64, 32])
    wck = din("wck", [2048, 64]); wcv = din("wcv", [2048, 64])
    wfp = din("wfp", [512, D]); wnp = din("wnp", [512, D]); wmo = din("wmo", [D, D])
    winG = din("winG", [8, 9, 128, 512]); cmpG = din("cmpG", [8, 5, 128, 512]); farc = din("farc", [128, 8])
    winM = din("winM", [9, 128, 512]); cmpM = din("cmpM", [5, 128, 512]); caus = din("caus", [4, 128, 512])
    ovl = din("ovl", [NCT * 128, NSEL + 1]); selA = din("selA", [128, NT * NSEL]); selV = din("selV", [128, NT * NSEL])
    Xp = din("Xp", [NSEL, NT * 128]); tri = din("tri", [128, 128])
    w_router = din("w_router", [D, 32]); b_router = din("b_router", [128, 32])
    w_gate = din("w_gate", [E, 8, 128, D]); w_up = din("w_up", [E, 8, 128, D]); w_down = din("w_down", [E, D, D])
    bgT = din("bgT", [128, E * 8]); buT = din("buT", [128, E * 8]); bdn = din("bdn", [1, E * D])
    okind = "ExternalOutput"
    out = Buf(nc.dram_tensor("out", [S, D], F32, kind=okind).ap(), "out")
    out.multi = True
    sk = okind if dbg else "Internal"
    featT = P.dram("featT", [NF, S], BF16, sk)
    vtok = P.dram("vtok", [S, NTK], BF16, sk)
    hTs = P.dram("hTs", [128, 8, S], BF16, sk)
    h2Ts = P.dram("h2Ts", [128, 8, S], BF16, sk)
    x1s = P.dram("x1s", [S, D], F32, sk)
    if dbg:
        dsm = P.dram("dsm", [128, NT * 32], F32, okind)
        dL = P.dram("dL", [128, NT * 8], F32, okind)
        dfb = P.dram("dfb", [128, 8 * NCH * NT], F32, okind)
        dcar = P.dram("dcar", [128, (NT + 1) * 8], F32, okind)
        dyn = P.dram("dyn", [128, NT * 512], BF16, okind)
        drw = P.dram("drw", [128, NT * 32], F32, okind)

    identb = P.sbuf([128, 128], BF16, "identb"); identf = P.sbuf([128, 128], F32, "identf")
    ones_r = P.sbuf([1, 128], F32, "ones_r"); ones_rb = P.sbuf([1, 128], BF16, "ones_rb")
    onesm = P.sbuf([128, 128], F32, "onesm")
    A1 = P.sbuf([128, 8], F32, "A1"); B1 = P.sbuf([128, 8], F32, "B1")
    A2 = P.sbuf([128, 8], F32, "A2"); B2 = P.sbuf([128, 8], F32, "B2")
    G1 = P.sbuf([128, D], F32, "G1"); G2 = P.sbuf([128, D], F32, "G2")
    small = P.sbuf([128, NT, 32], F32, "small")
    rw = P.sbuf([128, NT, 32], F32, "rw")
    zcol = P.sbuf([128, 1], F32, "zcol"); farcs = P.sbuf([128, 8], F32, "farcs")

    P.memset(POOL, identf, identf[:], 0.0)
    P.op(POOL, lambda e: e.affine_select(out=identf[:], in_=identf[:], pattern=[[-1, 128]], compare_op=ALU.not_equal,
                                          fill=1.0, base=0, channel_multiplier=1), [identf], [identf])
    P.cp(DVE, identb, identb[:], identf, identf[:])
    P.memset(DVE, ones_r, ones_r[:], 1.0); P.memset(DVE, ones_rb, ones_rb[:], 1.0)
    P.memset(POOL, onesm, onesm[:], 1.0); P.memset(DVE, zcol, zcol[:], 0.0)
    P.dma(SP, farcs, farcs[:], farc, farc[:, :])

    with P.scope():
        cs = P.sbuf([128, 8], F32, "cs"); scs = P.sbuf([128, 8], F32, "scs")
        arow = P.sbuf([1, 6 * D], F32, "arow"); brow = P.sbuf([1, 6 * D], F32, "brow")
        gp1 = P.sbuf([128, 8], F32, "gp1"); gp2 = P.sbuf([128, 8], F32, "gp2")
        wa = [P.sbuf([128, 8, 512], F32, f"wa{i}") for i in range(2)]
        psr = [P.psum([1, 512], F32, f"psr{i}") for i in range(2)]
        psT = P.psum([128, 48], F32, "psT"); psb = [P.psum([128, 512], F32, f"psb{i}") for i in range(2)]
        gpo = P.sbuf([128, D], F32, "gpo")
        P.dma(SP, cs, cs[:], cT, cT[:, :]); P.dma(SP, brow, brow[:], b_ada, b_ada[:, :])
        P.dma(SP, gp1, gp1[:], gpre1, gpre1[:, :]); P.dma(SP, gp2, gp2[:], gpre2, gpre2[:, :])
        P.act(scs, scs[:], cs, cs[:], AF.Silu)
        wav = w_ada.t.rearrange("(k p) n -> p k n", p=128)
        for blk in range(12):
            w_ = wa[blk % 2]; pr = psr[blk % 2]
            P.dma(SP, w_, w_[:], w_ada, wav[:, :, blk * 512:(blk + 1) * 512])
            for k in range(8):
                P.mm(pr, pr[0:1, :], scs, scs[:, k:k + 1], w_, w_[:, k, :], start=(k == 0), stop=(k == 7))
            P.tt(DVE, arow, arow[0:1, blk * 512:(blk + 1) * 512], pr, pr[0:1, :], brow, brow[0:1, blk * 512:(blk + 1) * 512], ALU.add)
        for j in list(range(0, 16)) + list(range(24, 40)):
            P.mm(psT, psT[:, j:j + 1], arow, arow[0:1, j * 128:(j + 1) * 128], ones_r, ones_r[0:1, 0:1])
        P.stt(A1, A1[:], psT, psT[:, 8:16], 1.0, gp1, gp1[:], ALU.add, ALU.mult)
        P.cp(DVE, B1, B1[:], psT, psT[:, 0:8])
        P.stt(A2, A2[:], psT, psT[:, 32:40], 1.0, gp2, gp2[:], ALU.add, ALU.mult)
        P.cp(DVE, B2, B2[:], psT, psT[:, 24:32])
        for (Gx, gsrc, c0) in ((G1, gpost1, 2048), (G2, gpost2, 5120)):
            P.dma(SP, gpo, gpo[:], gsrc, gsrc[:, :])
            for i in range(2):
                pb = psb[i]
                P.mm(pb, pb[:], ones_r, ones_r[0:1, :], arow, arow[0:1, c0 + i * 512:c0 + (i + 1) * 512])
                P.tt(DVE, Gx, Gx[:, i * 512:(i + 1) * 512], pb, pb[:], gpo, gpo[:, i * 512:(i + 1) * 512], ALU.mult)

    def rstd_of(src_b, src_ap, junk, ss, rstd):
        P.act(junk, junk[:], src_b, src_ap, AF.Square)
        P.call(DVE, "reduce_sum", [junk], [ss], out=ss[:], in_=junk[:], axis=AX.X)
        P.ts(DVE, ss, ss[:], ss, ss[:], 1.0 / D, RMS_EPS, ALU.mult, ALU.add)
        P.act(ss, ss[:], ss, ss[:], AF.Sqrt)
        P.call(DVE, "reciprocal", [ss], [rstd], out=rstd[:], in_=ss[:])

    bcscope = P.scope(); bcscope.__enter__()
    Lc = P.sbuf([128, NT, 8], F32, "Lc"); carry = P.sbuf([128, NT + 1, 8], F32, "carry")
    fbias = P.sbuf([128, 8, NCH, NT], F32, "fbias")
    with P.scope():
        NW = NF + NTK + NSM
        wB = P.sbuf([128, 8, NW], BF16, "wB")
        wBk = [Buf(wB.t, f"wBk{k}", P.base) for k in range(8)]
        stg = [P.sbuf([128, NW // 2], F32, f"stg{i}") for i in range(2)]
        wv = w_in.t.rearrange("(k p) n -> p k n", p=128)
        i = 0
        for k in range(8):
            for hf in range(2):
                st = stg[i % 2]
                P.dma(SP, st, st[:], w_in, wv[:, k, hf * (NW // 2):(hf + 1) * (NW // 2)])
                P.cp(POOL if i % 2 else DVE, wBk[k], wB[:, k, hf * (NW // 2):(hf + 1) * (NW // 2)], st, st[:])
                i += 1
        xin = [P.sbuf([128, D], F32, f"xin{i}") for i in range(2)]
        junk = P.sbuf([128, D], F32, "junk"); ss = P.sbuf([128, 1], F32, "ss"); rstd = P.sbuf([128, 1], F32, "rstd")
        xn = [P.sbuf([128, D], BF16, f"xn{i}") for i in range(2)]
        ptr = [P.psum([128, D], BF16, f"ptr{i}") for i in range(2)]
        hT = [P.sbuf([128, 8, 512], BF16, f"hT{i}") for i in range(2)]
        psf = [P.psum([128, 512], F32, f"psf{i}") for i in range(3)]
        fst = [P.sbuf([128, 512], BF16, f"fst{i}") for i in range(3)]
        vst = [P.sbuf([128, NTK], BF16, f"vst{i}") for i in range(2)]
        ci = 0

        def prep(c):
            h_ = hT[c % 2]
            for tt in range(4):
                n = 4 * c + tt
                xi = xin[n % 2]; xb = xn[n % 2]; pt = ptr[n % 2]
                P.dma(SP, xi, xi[:], x, x[n * 128:(n + 1) * 128, :])
                rstd_of(xi, xi[:], junk, ss, rstd)
                yield
                P.ts(DVE, xb, xb[:], xi, xi[:], rstd[:, 0:1], None, ALU.mult, extra=[rstd])
                for k in range(8):
                    P.tr(pt, pt[:, k * 128:(k + 1) * 128], xb, xb[:, k * 128:(k + 1) * 128], identb)
                yield
                for k in range(8):
                    P.act(h_, h_[:, k, tt * 128:(tt + 1) * 128], pt, pt[:, k * 128:(k + 1) * 128], AF.Identity,
                          bias=B1[:, k:k + 1], scale=A1[:, k:k + 1], extra=[A1, B1])
                yield
            P.dma(POOL, hTs, hTs[:, :, c * 512:(c + 1) * 512], h_, h_[:], sem_buf=h_)

        def mms(c):
            nonlocal ci
            h_ = hT[c % 2]
            for fb in range(NF // 128):
                ps = psf[ci % 3]; fs = fst[ci % 3]; ci += 1
                for k in range(8):
                    P.mm(ps, ps[:], wBk[k], wB[:, k, fb * 128:(fb + 1) * 128], h_, h_[:, k, :], start=(k == 0), stop=(k == 7))
                isq = fb < 4 or 8 <= fb < 12
                if fb % 2:
                    P.act(fs, fs[:], ps, ps[:], AF.Copy, scale=(0.125 if isq else 1.0))
                else:
                    P.ts(DVE, fs, fs[:], ps, ps[:], (0.125 if isq else 1.0), None, ALU.mult)
                P.dma(POOL, featT, featT[fb * 128:(fb + 1) * 128, c * 512:(c + 1) * 512], fs, fs[:], sem_buf=fs)
                yield
            for tt in range(4):
                n = 4 * c + tt
                vs = vst[n % 2]
                ps = psf[ci % 3]; ci += 1
                for k in range(8):
                    P.mm(ps, ps[:], h_, h_[:, k, tt * 128:(tt + 1) * 128], wBk[k], wB[:, k, NF:NF + 512], start=(k == 0), stop=(k == 7))
                P.cp(ACT, vs, vs[:, 0:512], ps, ps[:])
                yield
                ps = psf[ci % 3]; ci += 1
                for k in range(8):
                    P.mm(ps, ps[:, 0:416], h_, h_[:, k, tt * 128:(tt + 1) * 128], wBk[k], wB[:, k, NF + 512:NW], start=(k == 0), stop=(k == 7))
                P.cp(DVE, vs, vs[:, 512:896], ps, ps[:, 0:384])
                P.cp(DVE, small, small[:, n, :], ps, ps[:, 384:416])
                P.dma(POOL, vtok, vtok[n * 128:(n + 1) * 128, :], vs, vs[:], sem_buf=vs)
                yield

        for _ in prep(0):
            pass
        for c in range(NCH):
            gens = [mms(c)] + ([prep(c + 1)] if c + 1 < NCH else [])
            live = [True] * len(gens)
            while any(live):
                for gi_ in range(len(gens)):
                    if live[gi_]:
                        try:
                            next(gens[gi_])
                        except StopIteration:
                            live[gi_] = False

        bfs = P.sbuf([128, NT, 8], F32, "bfs"); sp = P.sbuf([128, NT, 8], F32, "sp")
        tris = P.sbuf([128, 128], F32, "tris"); tots = P.sbuf([128, NT, 8], F32, "tots")
        pcw = P.psum([128, NT * 8], F32, "pcw"); ptot = P.psum([128, NT * 8], F32, "ptot")
        P.dma(SP, bfs, bfs[:], bfg, bfg.t.rearrange("p (n h) -> p n h", h=8))
        P.dma(SP, tris, tris[:], tri, tri[:, :])
        P.tt(DVE, sp, sp[:], small, small[:, :, 0:8], bfs, bfs[:], ALU.add)
        P.act(sp, sp[:], sp, sp[:], AF.Exp, scale=-1.0)
        P.act(sp, sp[:], sp, sp[:], AF.Ln, bias=1.0)
        spf = sp.t.rearrange("p n h -> p (n h)")
        P.mm(pcw, pcw[:], tris, tris[:], sp, spf)
        P.mm(ptot, ptot[:], onesm, onesm[:], sp, spf)
        P.cp(DVE, tots, tots.t.rearrange("p n h -> p (n h)"), ptot, ptot[:])
        P.memset(DVE, carry, carry[:, 0, :], 0.0)
        for n in range(NT):
            P.tt(DVE, carry, carry[:, n + 1, :], carry, carry[:, n, :], tots, tots[:, n, :], ALU.add)
        P.tt(DVE, Lc, Lc.t.rearrange("p n h -> p (n h)"), pcw, pcw[:], carry, carry[:, 0:NT, :].rearrange("p n h -> p (n h)"), ALU.add)
        for h in range(8):
            for c in range(NCH):
                P.ts(DVE, fbias, fbias[:, h, c, :], Lc, Lc[:, :, h], carry[:, 4 * c + 2, h:h + 1], None, ALU.subtract, extra=[carry])
        P.act(small, small[:, :, 8:32], small, small[:, :, 8:32], AF.Sigmoid)
        if dbg:
            P.dma(SP, dsm, dsm[:, :], small, small.t.rearrange("p n h -> p (n h)"), sem_buf=small)
            P.dma(SP, dL, dL[:, :], Lc, Lc.t.rearrange("p n h -> p (n h)"), sem_buf=Lc)
            P.dma(SP, dfb, dfb[:, :], fbias, fbias.t.rearrange("p h c n -> p (h c n)"), sem_buf=fbias)
            P.dma(SP, dcar, dcar[:, :], carry, carry.t.rearrange("p n h -> p (n h)"), sem_buf=carry)

    fin = [featT, vtok, hTs]
    yfs = P.dram("yfs", [128, NT * 512], BF16, sk)

    if phases >= 3:
      with P.scope():
        yfox = P.sbuf([128, NT, 512], BF16, "yfox")
        causb = P.sbuf([128, 4, 512], BF16, "causb")
        cst = P.sbuf([128, 512], F32, "cst")
        for d in range(4):
            P.dma(SP, cst, cst[:], caus, caus[d, :, :])
            P.cp(DVE, causb, causb[:, d, :], cst, cst[:])
        kT = [P.sbuf([128, S], BF16, f"kT{i}") for i in range(2)]
        qA = [P.sbuf([128, S], BF16, f"qA{i}") for i in range(2)]
        qB = [P.sbuf([128, S], BF16, f"qB{i}") for i in range(2)]
        for i in range(2):
            P.memset(POOL, qA[i], qA[i][64:128, :], 0.0); P.memset(POOL, qB[i], qB[i][0:64, :], 0.0)
        V = [P.sbuf([128, NT, 65], BF16, f"V{i}") for i in range(2)]
        for i in range(2):
            P.memset(POOL, V[i], V[i][:, :, 64:65], 1.0)
        psS = [P.psum([128, 512], F32, f"psS{i}") for i in range(4)]
        psO = [P.psum([128, 4, 65], F32, f"psO{i}") for i in range(2)]
        pT = [P.sbuf([128, 512], BF16, f"pT{i}") for i in range(4)]
        rec = P.sbuf([128, 4], F32, "rec")
        si = 0; oi = 0
        pend = []

        def flush(keep=0):
            idx = len(pend)
            if keep:
                starts = [i_ for i_, (st_, _) in enumerate(pend) if st_]
                idx = starts[-keep] if len(starts) >= keep else 0
            for _, f_ in pend[:idx]:
                f_()
            del pend[:idx]

        def fox_pv(O, p_, v_, k, c, first):
            for tt in range(4):
                if k <= 4 * c + tt:
                    P.mm(O, O[:, tt, :], p_, p_[:, tt * 128:(tt + 1) * 128], v_, v_[:, k, :], start=first, stop=(k == 4 * c + tt))
                    first = False

        def fox_norm(O, c, h):
            P.ts(DVE, rec, rec[:], O, O[:, :, 64], 1e-30, None, ALU.add)
            P.call(DVE, "reciprocal", [rec], [rec], out=rec[:], in_=rec[:])
            for tt in range(4):
                P.ts(DVE, yfox, yfox[:, 4 * c + tt, h * 64:(h + 1) * 64], O, O[:, tt, 0:64], rec[:, tt:tt + 1], None, ALU.mult, extra=[rec])

        for h in range(8):
            jp = h // 2; ih = h % 2
            k_, v_ = kT[jp % 2], V[h % 2]
            q_ = (qA if ih == 0 else qB)[jp % 2]
            if ih == 0:
                P.dma(SP, k_, k_[:], featT, featT[512 + jp * 128:512 + (jp + 1) * 128, :])
            P.dma(SP, q_, q_[64 * ih:64 * ih + 64, :], featT, featT[h * 64:(h + 1) * 64, :])
            P.dma(SP, v_, v_[:, :, 0:64], vtok, vtok.t[:, h * 64:(h + 1) * 64].rearrange("(n p) d -> p n d", p=128))
            for c in range(NCH):
                O = psO[oi % 2]; oi += 1
                first = True
                for k in range(4 * c + 4):
                    st = psS[si % len(psS)]; p_ = pT[si % len(pT)]; si += 1
                    diag = k >= 4 * c
                    c0 = 128 * max(0, k - 4 * c)
                    P.mm(st, st[:, c0:512], k_, k_[:, k * 128:(k + 1) * 128], q_, q_[:, c * 512 + c0:(c + 1) * 512], start=True, stop=not diag)
                    if diag:
                        P.mm(st, st[:, c0:512], identb, identb[:], causb, causb[:, k - 4 * c, c0:512], start=False, stop=True)
                    flush(keep=2)
                    P.act(p_, p_[:, c0:512], st, st[:, c0:512], AF.Exp, bias=fbias[:, h, c, k:k + 1], extra=[fbias])
                    pend.append((True, lambda O=O, p_=p_, v_=v_, k=k, c=c, first=first: fox_pv(O, p_, v_, k, c, first)))
                    first = False
                pend.append((False, lambda O=O, c=c, h=h: fox_norm(O, c, h)))
        flush()
        P.dma(POOL, yfs, yfs[:, :], yfox, yfox.t.rearrange("p n f -> p (n f)"), sem_buf=yfox)
        fin.append(yfox)
    bcscope.__exit__(None, None, None)
    if phases >= 3:
        yscope = P.scope(); yscope.__enter__()
        ynsa = P.sbuf([128, NT, 512], BF16, "ynsa")

    if phases >= 3:
      with P.scope():
        kcT = P.sbuf([64, 2, NCT * 128], BF16, "kcT"); Vc = P.sbuf([128, 2, NCT, 65], BF16, "Vc")
        P.memset(POOL, kcT, kcT[:], 0.0); P.memset(POOL, Vc, Vc[:], 0.0)
        P.memset(POOL, Vc, Vc[:, :, :, 64:65], 1.0)
        with P.scope():
            wk_f = P.sbuf([64, 32, 64], F32, "wk_f"); wk_b = P.sbuf([64, 32, 64], BF16, "wk_b")
            wv_b = P.sbuf([64, 32, 64], BF16, "wv_b")
            pe_f = P.sbuf([64, 32], F32, "pe_f"); pek_b = P.sbuf([64, 32], BF16, "pek_b"); pev_b = P.sbuf([64, 32], BF16, "pev_b")
            kcm = P.sbuf([64, S], BF16, "kcm"); vcm = P.sbuf([64, S], BF16, "vcm")
            vtk = P.sbuf([128, NT, 64], BF16, "vtk")
            pc1 = P.psum([64, 256], F32, "pc1"); pc2 = P.psum([128, 64], F32, "pc2"); pc3 = P.psum([128, 64], F32, "pc3")
            pcT = P.psum([64, 128], BF16, "pcT")
            kcon = P.sbuf([64, 1], F32, "kcon"); vcon = P.sbuf([128, 64], F32, "vcon")
            P.dma(SP, wk_f, wk_f[:], wck, wck.t.rearrange("(l d) e -> d l e", d=64)); P.cp(DVE, wk_b, wk_b[:], wk_f, wk_f[:])
            P.dma(SP, wk_f, wk_f[:], wcv, wcv.t.rearrange("(l d) e -> d l e", d=64)); P.cp(DVE, wv_b, wv_b[:], wk_f, wk_f[:])
            P.dma(SP, pe_f, pe_f[:], pekT, pekT[:, :]); P.cp(DVE, pek_b, pek_b[:], pe_f, pe_f[:])
            P.dma(SP, pe_f, pe_f[:], pevT, pevT[:, :]); P.cp(DVE, pev_b, pev_b[:], pe_f, pe_f[:])
            for l in range(32):
                P.mm(pc1, pc1[:, 0:1], wk_b, wk_b[:, l, :], pek_b, pek_b[:, l:l + 1], start=(l == 0), stop=(l == 31))
            P.cp(DVE, kcon, kcon[:], pc1, pc1[:, 0:1])
            pev_rep = P.sbuf([64, 32, 128], BF16, "pev_rep")
            for l in range(32):
                P.ts(DVE, pev_rep, pev_rep[:, l, :], onesm, onesm[0:64, :], pev_b[:, l:l + 1], None, ALU.mult, extra=[pev_b])
            for l in range(32):
                P.mm(pc3, pc3[:], pev_rep, pev_rep[:, l, :], wv_b, wv_b[:, l, :], start=(l == 0), stop=(l == 31))
            P.cp(DVE, vcon, vcon[:], pc3, pc3[:])
            for g in range(2):
                P.dma(SP, kcm, kcm[:], featT, featT[1536 + g * 64:1536 + (g + 1) * 64, :])
                P.dma(SP, vtk, vtk[:], vtok, vtok.t[:, 512 + g * 64:512 + (g + 1) * 64].rearrange("(n p) d -> p n d", p=128))
                for n in range(NT):
                    P.tr(pcT, pcT[:], vtk, vtk[:, n, :], identb)
                    P.cp(ACT, vcm, vcm[:, n * 128:(n + 1) * 128], pcT, pcT[:])
                for ct in range(NCT):
                    nc_ = min(128, NCMP - ct * 128)
                    c0 = ct * 128
                    for l in range(32):
                        P.mm(pc1, pc1[:, 0:nc_], wk_b, wk_b[:, l, :], kcm, kcm[:, 16 * c0 + l:16 * c0 + l + 16 * (nc_ - 1) + 1:16], start=(l == 0), stop=(l == 31))
                    P.act(kcT, kcT[:, g, c0:c0 + nc_], pc1, pc1[:, 0:nc_], AF.Identity, bias=kcon[:, 0:1], extra=[kcon])
                    for l in range(32):
                        P.mm(pc2, pc2[0:nc_, :], vcm, vcm[:, 16 * c0 + l:16 * c0 + l + 16 * (nc_ - 1) + 1:16], wv_b, wv_b[:, l, :], start=(l == 0), stop=(l == 31))
                    P.tt(DVE, Vc, Vc[0:nc_, g, ct, 0:64], pc2, pc2[0:nc_, :], vcon, vcon[0:nc_, :], ALU.add)

        ovb = P.sbuf([128, NCT, NSEL + 1], BF16, "ovb")
        sA = P.sbuf([128, NT, NSEL], BF16, "sA"); sV = P.sbuf([128, NT, NSEL], BF16, "sV")
        Xb = P.sbuf([NSEL, NT, 128], BF16, "Xb")
        with P.scope():
            ovf = P.sbuf([128, NCT, NSEL + 1], F32, "ovf")
            P.dma(SP, ovf, ovf[:], ovl, ovl.t.rearrange("(c p) j -> p c j", p=128)); P.cp(DVE, ovb, ovb[:], ovf, ovf[:])
            sf = P.sbuf([128, NT * NSEL], F32, "sf")
            P.dma(SP, sf, sf[:], selA, selA[:, :]); P.cp(DVE, sA, sA.t.rearrange("p n j -> p (n j)"), sf, sf[:])
            P.dma(SP, sf, sf[:], selV, selV[:, :]); P.cp(DVE, sV, sV.t.rearrange("p n j -> p (n j)"), sf, sf[:])
            Xf = P.sbuf([NSEL, NT * 128], F32, "Xf")
            P.dma(SP, Xf, Xf[:], Xp, Xp[:, :]); P.cp(POOL, Xb, Xb.t.rearrange("j n p -> j (n p)"), Xf, Xf[:])
        ksl = P.sbuf([128, S], BF16, "ksl"); kwn = P.sbuf([128, S], BF16, "kwn")
        kc2 = P.sbuf([128, NCT * 128], BF16, "kc2")
        for g in range(2):
            P.dma(SP, kc2, kc2[64 * g:64 * g + 64, :], kcT, kcT[:, g, :])
        P.dma(SP, ksl, ksl[:], featT, featT[1664:1792, :])
        P.dma(SP, kwn, kwn[:], featT, featT[1792:1920, :])
        Vs = P.sbuf([128, NT, 65], BF16, "Vs"); Vw = P.sbuf([128, NT, 65], BF16, "Vw")
        P.memset(POOL, Vs, Vs[:, :, 64:65], 1.0); P.memset(POOL, Vw, Vw[:, :, 64:65], 1.0)
        wB_ = P.sbuf([128, 4, 6, 512], BF16, "winB"); cB_ = P.sbuf([128, 4, 5, 512], BF16, "cmpB")
        wS_ = P.sbuf([128, 3, 512], BF16, "winS")
        bst = [P.sbuf([128, 512], F32, f"bst{i}") for i in range(2)]
        mst = [P.sbuf([128, 512], F32, f"mst{i}") for i in range(2)]
        q4 = [P.sbuf([128, 4, 512], BF16, f"q4{i}") for i in range(2)]
        psS = [P.psum([128, 512], F32, f"nS{i}") for i in range(3)]
        psO = [P.psum([128, 4, 65], F32, f"nO{i}") for i in range(3)]
        psI = P.psum([128, 4, NSEL + 1], F32, "nI")
        psX = P.psum([NSEL, 512], BF16, "nX")
        pT = [P.sbuf([128, 512], BF16, f"npT{i}") for i in range(4)]
        rec = P.sbuf([128, 4], F32, "nrec"); fac = P.sbuf([128, 4], F32, "nfac")
        imp = P.sbuf([128, 4, NSEL], F32, "imp"); sc_ = P.sbuf([128, NSEL], F32, "score"); sc2 = P.sbuf([128, NSEL], F32, "score2")
        m8 = P.sbuf([128, 16], F32, "m8"); sbt = P.sbuf([128, NSEL], BF16, "sbt")
        selT = P.sbuf([NSEL, 512], BF16, "selT")
        yacc = P.sbuf([128, 4, 4, 64], F32, "yacc")
        si = 0; oi = 0; bi = 0

        def att_tile(k_b, k_ap, q_b, q_ap, extra_mms, bias_ap, bias_b, v_b, v_ap, O, ttmask, first, lastk):
            nonlocal si
            st = psS[si % len(psS)]; p_ = pT[si % len(pT)]; si += 1
            tts = [tt for tt in range(4) if ttmask(tt)]
            c0, c1 = 128 * tts[0], 128 * (tts[-1] + 1)
            P.mm(st, st[:, c0:c1], k_b, k_ap, q_b, q_ap[:, c0:c1], start=True, stop=not extra_mms)
            for i_, (lb, l, rb, r) in enumerate(extra_mms):
                P.mm(st, st[:, c0:c1], lb, l, rb, r[:, c0:c1], start=False, stop=(i_ == len(extra_mms) - 1))
            flush(keep=1)
            P.act(p_, p_[:, c0:c1], st, st[:, c0:c1], AF.Exp, bias=bias_ap, extra=[bias_b])

            def pv(first=first):
                for tt in range(4):
                    if ttmask(tt):
                        P.mm(O, O[:, tt, :], p_, p_[:, tt * 128:(tt + 1) * 128], v_b, v_ap, start=first, stop=lastk(tt))
                        first = False
            pend.append((True, pv))
            return False, p_

        pend = []

        def flush(keep=0):
            idx = len(pend)
            if keep:
                starts = [i_ for i_, (st_, _) in enumerate(pend) if st_]
                idx = starts[-keep] if len(starts) >= keep else 0
            for _, f_ in pend[:idx]:
                f_()
            del pend[:idx]

        def cmp_imp(p_, k, firstI, lastI):
            for tt in range(4):
                P.mm(psI, psI[:, tt, :], p_, p_[:, tt * 128:(tt + 1) * 128], ovb, ovb[:, k, :], start=firstI, stop=lastI)
                firstI = False

        def cmp_norm(O, c, h, hh):
            P.ts(DVE, rec, rec[:], O, O[:, :, 64], 1e-30, None, ALU.add)
            P.call(DVE, "reciprocal", [rec], [rec], out=rec[:], in_=rec[:])
            for tt in range(4):
                n = 4 * c + tt
                P.tt(DVE, fac, fac[:, tt:tt + 1], rec, rec[:, tt:tt + 1], small, small[:, n, 8 + 3 * h:9 + 3 * h], ALU.mult)
                P.ts(DVE, yacc, yacc[:, tt, hh, :], O, O[:, tt, 0:64], fac[:, tt:tt + 1], None, ALU.mult, extra=[fac])
                if hh == 0:
                    P.ts(DVE, imp, imp[:, tt, :], psI, psI[:, tt, 0:NSEL], rec[:, tt:tt + 1], None, ALU.mult, extra=[rec])
                else:
                    P.stt(imp, imp[:, tt, :], psI, psI[:, tt, 0:NSEL], rec[:, tt:tt + 1], imp, imp[:, tt, :], ALU.mult, ALU.add, extra=[rec])

        def sw_norm(O, Ow, c, h, hh):
            for (Ox, gi) in ((O, 1), (Ow, 2)):
                P.ts(DVE, rec, rec[:], Ox, Ox[:, :, 64], 1e-30, None, ALU.add)
                P.call(DVE, "reciprocal", [rec], [rec], out=rec[:], in_=rec[:])
                for tt in range(4):
                    n = 4 * c + tt
                    P.tt(DVE, fac, fac[:, tt:tt + 1], rec, rec[:, tt:tt + 1], small, small[:, n, 8 + 3 * h + gi:9 + 3 * h + gi], ALU.mult)
                    P.stt(yacc, yacc[:, tt, hh, :], Ox, Ox[:, tt, 0:64], fac[:, tt:tt + 1], yacc, yacc[:, tt, hh, :], ALU.mult, ALU.add, extra=[fac])
            for tt in range(4):
                P.cp(POOL, ynsa, ynsa[:, 4 * c + tt, h * 64:(h + 1) * 64], yacc, yacc[:, tt, hh, :])

        for v in range(3):
            m1 = mst[v % 2]
            P.dma(SP, m1, m1[:], winM, winM[v, :, :]); P.cp(POOL, wS_, wS_[:, v, :], m1, m1[:])
        for g in range(2):
            for i in range(2):
                P.memset(POOL, q4[i], q4[i][:], 0.0)
            P.dma(SP, Vs, Vs[:, :, 0:64], vtok, vtok.t[:, 640 + g * 64:640 + (g + 1) * 64].rearrange("(n p) d -> p n d", p=128))
            P.dma(SP, Vw, Vw[:, :, 0:64], vtok, vtok.t[:, 768 + g * 64:768 + (g + 1) * 64].rearrange("(n p) d -> p n d", p=128))
            for hh in range(4):
                h = 4 * g + hh
                for (G_, M_, B_, vs_) in ((winG, winM, wB_, (3, 4, 5, 6, 7, 8)), (cmpG, cmpM, cB_, (0, 1, 2, 3, 4))):
                    for vi, v in enumerate(vs_):
                        b1 = bst[bi % 2]; m1 = mst[bi % 2]; bi += 1
                        P.dma(SP, b1, b1[:], G_, G_[h, v, :, :]); P.dma(SP, m1, m1[:], M_, M_[v, :, :])
                        P.tt(DVE, B_, B_[:, hh, vi, :], b1, b1[:], m1, m1[:], ALU.add)
            for c in range(NCH):
                q_ = q4[c % 2]
                P.dma(SP, q_, q_[64 * g:64 * g + 64, :, :], featT, featT.t[1024 + g * 256:1024 + (g + 1) * 256, c * 512:(c + 1) * 512].rearrange("(h d) t -> d h t", d=64))
                for hh in range(4):
                    h = 4 * g + hh
                    O = psO[oi % len(psO)]; oi += 1
                    first = True; firstI = True
                    kts = [k for k in range(NCT) if c - 4 * k >= 0]
                    for k in kts:
                        r = c - 4 * k
                        if r <= 4:
                            ex = [(identb, identb[:], cB_, cB_[:, hh, r, :])]; bap = zcol[:, 0:1]; bb = zcol
                        else:
                            ex = []; bap = farcs[:, h:h + 1]; bb = farcs
                        first, p_ = att_tile(kc2, kc2[:, k * 128:(k + 1) * 128], q_, q_[:, hh, :], ex, bap, bb,
                                             Vc, Vc[:, g, k, :], O, lambda tt: True, first, lambda tt, k=k: k == kts[-1])
                        pend.append((False, lambda p_=p_, k=k, firstI=firstI, lastI=(k == kts[-1]): cmp_imp(p_, k, firstI, lastI)))
                        firstI = False
                    pend.append((False, lambda O=O, c=c, h=h, hh=hh: cmp_norm(O, c, h, hh)))
                flush()
                for tt in range(4):
                    n = 4 * c + tt
                    P.tt(DVE, sc_, sc_[:], imp, imp[:, tt, :], sV, sV[:, n, :], ALU.mult)
                    P.tt(DVE, sc_, sc_[:], sc_, sc_[:], sA, sA[:, n, :], ALU.add)
                    P.call(DVE, "max", [sc_], [m8], out=m8[:, 0:8], in_=sc_[:])
                    if TOPN > 8:
                        P.call(DVE, "match_replace", [sc_, m8], [sc2], out=sc2[:], in_to_replace=m8[:, 0:8], in_values=sc_[:], imm_value=-3e38)
                        P.call(DVE, "max", [sc2], [m8], out=m8[:, 8:16], in_=sc2[:])
                    P.ts(DVE, sc2, sc2[:], sc_, sc_[:], m8[:, TOPN - 1:TOPN], None, ALU.is_ge, extra=[m8])
                    P.ts(DVE, sbt, sbt[:], sc2, sc2[:], -1.0, -NEGM, ALU.add, ALU.mult)
                    P.tr(psX, psX[:, tt * 128:(tt + 1) * 128], sbt, sbt[:], identb)
                P.cp(ACT, selT, selT[:], psX, psX[:])
                for hh in range(4):
                    h = 4 * g + hh
                    O = psO[oi % len(psO)]; oi += 1
                    first = True
                    for k in range(4 * c + 4):
                        d = k - 4 * c
                        ex = [(Xb, Xb[:, k, :], selT, selT[:])]
                        if d >= -1:
                            ex.append((identb, identb[:], wB_, wB_[:, hh, 5 if d == -1 else d + 1, :])); bap = zcol[:, 0:1]; bb = zcol
                        else:
                            bap = farcs[:, h:h + 1]; bb = farcs
                        first, _ = att_tile(ksl, ksl[:, k * 128:(k + 1) * 128], q_, q_[:, hh, :], ex, bap, bb,
                                            Vs, Vs[:, k, :], O, lambda tt, k=k: k <= 4 * c + tt, first, lambda tt, k=k: k == 4 * c + tt)
                    Ow = psO[oi % len(psO)]; oi += 1
                    first = True
                    k0 = max(0, 4 * c - 4)
                    for k in range(k0, 4 * c + 4):
                        d = k - 4 * c
                        if d <= -2:
                            ex = [(identb, identb[:], wS_, wS_[:, d + 4, :])]; bap = farcs[:, h:h + 1]; bb = farcs
                        else:
                            ex = [(identb, identb[:], wB_, wB_[:, hh, d + 1, :])]; bap = zcol[:, 0:1]; bb = zcol
                        first, _ = att_tile(kwn, kwn[:, k * 128:(k + 1) * 128], q_, q_[:, hh, :], ex, bap, bb,
                                            Vw, Vw[:, k, :], Ow, lambda tt, k=k: (k <= 4 * c + tt) and (k >= 4 * c + tt - 4), first,
                                            lambda tt, k=k: k == 4 * c + tt)
                    pend.append((False, lambda O=O, Ow=Ow, c=c, h=h, hh=hh: sw_norm(O, Ow, c, h, hh)))
                flush()
        if dbg:
            P.dma(SP, dyn, dyn[:, :], ynsa, ynsa.t.rearrange("p n f -> p (n f)"), sem_buf=ynsa)
            fin += [ynsa]

    if phases >= 4:
      with P.scope():
        wfb = P.sbuf([128, 4, D], BF16, "wfb"); wnb = P.sbuf([128, 4, D], BF16, "wnb"); wmb = P.sbuf([128, 8, D], BF16, "wmb")
        wgb = P.sbuf([128, 8, NG], BF16, "wgb"); wrt = P.sbuf([128, 8, 32], F32, "wrt"); brt = P.sbuf([128, 32], F32, "brt")
        with P.scope():
            stg = [P.sbuf([128, 1024], F32, f"dstg{i}") for i in range(2)]
            i = 0
            for (wd_, src, nk) in ((wfb, wfp, 4), (wnb, wnp, 4), (wmb, wmo, 8)):
                sv = src.t.rearrange("(k p) n -> p k n", p=128)
                for k in range(nk):
                    st = stg[i % 2]
                    P.dma(SP, st, st[:, 0:D], src, sv[:, k, :]); P.cp(POOL if i % 2 else DVE, wd_, wd_[:, k, :], st, st[:, 0:D]); i += 1
            wv = w_in.t.rearrange("(k p) n -> p k n", p=128)
            for k in range(8):
                for hf in range(2):
                    st = stg[i % 2]
                    P.dma(SP, st, st[:], w_in, wv[:, k, 2848 + hf * 1024:2848 + (hf + 1) * 1024])
                    P.cp(POOL if i % 2 else DVE, wgb, wgb[:, k, hf * 1024:(hf + 1) * 1024], st, st[:]); i += 1
        P.dma(SP, wrt, wrt[:], w_router, w_router.t.rearrange("(k p) n -> p k n", p=128))
        P.dma(SP, brt, brt[:], b_router, b_router[:, :])

        def mk(c_):
            d = {}
            d["hl"] = P.sbuf([128, 8, 128], BF16, f"hTl{c_}"); d["yf"] = P.sbuf([128, 512], BF16, f"yfl{c_}")
            d["xi"] = P.sbuf([128, D], F32, f"dx{c_}"); d["yT"] = P.sbuf([128, 8, 128], BF16, f"yT{c_}")
            d["ptb"] = P.psum([128, D], BF16, f"dptb{c_}"); d["pa"] = P.psum([128, 512], F32, f"dpa{c_}")
            d["pb"] = P.psum([128, 512], F32, f"dpb{c_}"); d["pq"] = P.psum([128, 512], F32, f"dpq{c_}")
            d["s0"] = P.sbuf([128, 512], F32, f"ds0{c_}"); d["s1"] = P.sbuf([128, 512], F32, f"ds1{c_}")
            d["tmp"] = P.sbuf([128, 512], F32, f"dtmp{c_}"); d["mtok"] = P.sbuf([128, D], BF16, f"mtok{c_}")
            d["mT"] = P.sbuf([128, 8, 128], BF16, f"mT{c_}")
            d["ss"] = P.sbuf([128, 1], F32, f"dss{c_}"); d["rstd"] = P.sbuf([128, 1], F32, f"drstd{c_}")
            d["x1"] = P.sbuf([128, D], F32, f"x1{c_}"); d["xn2"] = P.sbuf([128, D], F32, f"xn2{c_}")
            d["h2f"] = P.sbuf([128, 8, 128], F32, f"h2f{c_}"); d["hb"] = P.sbuf([128, 8, 128], BF16, f"h2b{c_}")
            d["lg"] = P.sbuf([128, 32], F32, f"lg{c_}"); d["m8"] = P.sbuf([128, 8], F32, f"dm8{c_}"); d["msk"] = P.sbuf([128, 32], F32, f"msk{c_}")
            d["ex"] = P.sbuf([128, 32], F32, f"ex{c_}"); d["nmx"] = P.sbuf([128, 1], F32, f"nmx{c_}"); d["sm"] = P.sbuf([128, 1], F32, f"sm{c_}")
            return d
        chains = [mk(0), mk(1)]

        def tile_d(n, d):
            hl, yf_, xi, yT, ptb, pa, pb, pq = d["hl"], d["yf"], d["xi"], d["yT"], d["ptb"], d["pa"], d["pb"], d["pq"]
            s0, s1, tmp, mtok, mT, ss, rstd, x1_, xn2, h2f, hb = (d[k_] for k_ in ("s0", "s1", "tmp", "mtok", "mT", "ss", "rstd", "x1", "xn2", "h2f", "hb"))
            lg, m8, msk, ex_, nmx, sm = (d[k_] for k_ in ("lg", "m8", "msk", "ex", "nmx", "sm"))
            P.dma(SP, hl, hl[:], hTs, hTs[:, :, n * 128:(n + 1) * 128])
            P.dma(SP, xi, xi[:], x, x[n * 128:(n + 1) * 128, :])
            P.dma(SP, yf_, yf_[:], yfs, yfs[:, n * 512:(n + 1) * 512])
            yield
            for j in range(4):
                P.tr(ptb, ptb[:, j * 128:(j + 1) * 128], yf_, yf_[:, j * 128:(j + 1) * 128], identb)
                P.tr(ptb, ptb[:, (4 + j) * 128:(5 + j) * 128], ynsa, ynsa[:, n, j * 128:(j + 1) * 128], identb)
            P.cp(ACT, yT, yT.t.rearrange("p k t -> p (k t)"), ptb, ptb[:])
            yield
            for hf in range(2):
                for j in range(4):
                    P.mm(pa, pa[:], yT, yT[:, j, :], wfb, wfb[:, j, hf * 512:(hf + 1) * 512], start=(j == 0), stop=(j == 3))
                for j in range(4):
                    P.mm(pb, pb[:], yT, yT[:, 4 + j, :], wnb, wnb[:, j, hf * 512:(hf + 1) * 512], start=(j == 0), stop=(j == 3))
                for k in range(8):
                    P.mm(pq, pq[:], hl, hl[:, k, :], wgb, wgb[:, k, hf * 512:(hf + 1) * 512], start=(k == 0), stop=(k == 7))
                P.act(s0, s0[:], pq, pq[:], AF.Sigmoid)
                yield
                for k in range(8):
                    P.mm(pq, pq[:], hl, hl[:, k, :], wgb, wgb[:, k, 1024 + hf * 512:1024 + (hf + 1) * 512], start=(k == 0), stop=(k == 7))
                P.act(s1, s1[:], pq, pq[:], AF.Sigmoid)
                P.tt(DVE, tmp, tmp[:], s0, s0[:], pa, pa[:], ALU.mult)
                yield
                P.tt(DVE, s1, s1[:], s1, s1[:], pb, pb[:], ALU.mult)
                P.tt(DVE, mtok, mtok[:, hf * 512:(hf + 1) * 512], s1, s1[:], tmp, tmp[:], ALU.add)
                yield
            for k in range(8):
                P.tr(ptb, ptb[:, k * 128:(k + 1) * 128], mtok, mtok[:, k * 128:(k + 1) * 128], identb)
            P.cp(ACT, mT, mT.t.rearrange("p k t -> p (k t)"), ptb, ptb[:])
            yield
            for hf, p_ in ((0, pa), (1, pb)):
                for k in range(8):
                    P.mm(p_, p_[:], mT, mT[:, k, :], wmb, wmb[:, k, hf * 512:(hf + 1) * 512], start=(k == 0), stop=(k == 7))
            yield
            for hf, p_ in ((0, pa), (1, pb)):
                P.act(xn2, xn2[:, hf * 512:(hf + 1) * 512], p_, p_[:], AF.Square)
            P.call(DVE, "reduce_sum", [xn2], [ss], out=ss[:], in_=xn2[:], axis=AX.X)
            P.ts(DVE, ss, ss[:], ss, ss[:], 1.0 / D, RMS_EPS, ALU.mult, ALU.add)
            yield
            P.act(ss, ss[:], ss, ss[:], AF.Sqrt)
            P.call(DVE, "reciprocal", [ss], [rstd], out=rstd[:], in_=ss[:])
            yield
            for hf, p_ in ((0, pa), (1, pb)):
                sl = slice(hf * 512, (hf + 1) * 512)
                P.stt(x1_, x1_[:, sl], p_, p_[:], rstd[:, 0:1], G1, G1[:, sl], ALU.mult, ALU.mult, extra=[rstd])
            P.tt(DVE, x1_, x1_[:], x1_, x1_[:], xi, xi[:], ALU.add)
            P.dma(POOL, x1s, x1s[n * 128:(n + 1) * 128, :], x1_, x1_[:], sem_buf=x1_)
            yield
            P.act(xn2, xn2[:], x1_, x1_[:], AF.Square)
            P.call(DVE, "reduce_sum", [xn2], [ss], out=ss[:], in_=xn2[:], axis=AX.X)
            P.ts(DVE, ss, ss[:], ss, ss[:], 1.0 / D, RMS_EPS, ALU.mult, ALU.add)
            yield
            P.act(ss, ss[:], ss, ss[:], AF.Sqrt)
            P.call(DVE, "reciprocal", [ss], [rstd], out=rstd[:], in_=ss[:])
            P.ts(DVE, xn2, xn2[:], x1_, x1_[:], rstd[:, 0:1], None, ALU.mult, extra=[rstd])
            yield
            for hf, p_ in ((0, pa), (1, pb)):
                for k in range(4):
                    kk = hf * 4 + k
                    P.tr(p_, p_[:, k * 128:(k + 1) * 128], xn2, xn2[:, kk * 128:(kk + 1) * 128], identf)
            yield
            for kk in range(8):
                p_ = pa if kk < 4 else pb
                P.act(h2f, h2f[:, kk, :], p_, p_[:, (kk % 4) * 128:(kk % 4 + 1) * 128], AF.Identity,
                      bias=B2[:, kk:kk + 1], scale=A2[:, kk:kk + 1], extra=[A2, B2])
            P.cp(DVE, hb, hb[:], h2f, h2f[:])
            P.dma(POOL, h2Ts, h2Ts[:, :, n * 128:(n + 1) * 128], hb, hb[:], sem_buf=hb)
            yield
            for k in range(8):
                P.mm(pq, pq[:, 0:32], h2f, h2f[:, k, :], wrt, wrt[:, k, :], start=(k == 0), stop=(k == 7))
            P.tt(DVE, lg, lg[:], pq, pq[:, 0:32], brt, brt[:], ALU.add)
            P.call(DVE, "max", [lg], [m8], out=m8[:], in_=lg[:])
            yield
            P.ts(DVE, msk, msk[:], lg, lg[:], m8[:, 3:4], None, ALU.is_ge, extra=[m8])
            P.ts(DVE, nmx, nmx[:], m8, m8[:, 0:1], -1.0, None, ALU.mult)
            P.act(ex_, ex_[:], lg, lg[:], AF.Exp, bias=nmx[:, 0:1], extra=[nmx])
            yield
            P.tt(DVE, ex_, ex_[:], ex_, ex_[:], msk, msk[:], ALU.mult)
            P.call(DVE, "reduce_sum", [ex_], [sm], out=sm[:], in_=ex_[:], axis=AX.X)
            P.call(DVE, "reciprocal", [sm], [sm], out=sm[:], in_=sm[:])
            P.ts(DVE, rw, rw[:, n, :], ex_, ex_[:], sm[:, 0:1], None, ALU.mult, extra=[sm])

        for n0 in range(0, NT, 2):
            gens = [tile_d(n0, chains[0]), tile_d(n0 + 1, chains[1])]
            live = [True, True]
            while any(live):
                for gi_ in range(2):
                    if live[gi_]:
                        try:
                            next(gens[gi_])
                        except StopIteration:
                            live[gi_] = False
        if dbg:
            P.dma(SP, drw, drw[:, :], rw, rw.t.rearrange("p n e -> p (n e)"), sem_buf=rw)
        fin += [x1s, h2Ts, rw]
    if phases >= 3:
        yscope.__exit__(None, None, None)

    if phases >= 5:
      with P.scope():
        TS = min(S, 1024); NTS = TS // 128; NCS = TS // 512
        acc = P.sbuf([128, NTS, D], F32, "acc")
        h2 = P.sbuf([128, 8, TS], BF16, "h2")
        wgt = [P.sbuf([128, 8, D], BF16, f"wE{i}") for i in range(2)]
        wgf = [[Buf(wgt[m].t, f"wE{m}_{f}", P.base) for f in range(8)] for m in range(2)]
        wdb = [P.sbuf([128, 8, D], BF16, f"wD{i}") for i in range(2)]
        wdk = [[Buf(wdb[b_].t, f"wD{b_}_{k}", P.base) for k in range(8)] for b_ in range(2)]
        stg = [P.sbuf([128, D], F32, f"estg{i}") for i in range(4)]
        hid = [P.sbuf([128, 8, 512], BF16, f"hid{i}") for i in range(2)]
        bgs = P.sbuf([128, E, 8], F32, "bgs"); bus = P.sbuf([128, E, 8], F32, "bus")
        bdall = P.sbuf([32, D], F32, "bdall"); rwT = P.sbuf([32, 128], F32, "rwT")
        P.dma(SP, bdall, bdall[0:E, :], bdn, bdn.t.rearrange("o (e d) -> (o e) d", d=D))
        P.dma(SP, bgs, bgs[:], bgT, bgT.t.rearrange("p (e k) -> p e k", k=8))
        P.dma(SP, bus, bus[:], buT, buT.t.rearrange("p (e k) -> p e k", k=8))
        bg17 = P.sbuf([128, E, 8], F32, "bg17")
        P.ts(DVE, bg17, bg17[:], bgs, bgs[:], 1.702, None, ALU.mult)
        SIGCAP = float(1.0 / (1.0 + math.exp(-1.702 * 7.0)))
        pG = [P.psum([128, 512], F32, f"eG{i}") for i in range(2)]
        pU = [P.psum([128, 512], F32, f"eU{i}") for i in range(2)]
        pY = [P.psum([128, 512], F32, f"eY{i}") for i in range(4)]
        gs = [P.sbuf([128, 512], F32, f"gs{i}") for i in range(2)]
        sg = [P.sbuf([128, 512], F32, f"esg{i}") for i in range(2)]
        us = [P.sbuf([128, 512], F32, f"us{i}") for i in range(2)]
        junk = P.sbuf([128, D], F32, "ejunk"); ss = P.sbuf([128, 1], F32, "ess"); rstd = P.sbuf([128, 1], F32, "erstd")
        x1l = [P.sbuf([128, D], F32, f"x1l{i}") for i in range(1)]
        ot = [P.sbuf([128, D], F32, f"ot{i}") for i in range(1)]
        wsrc = (w_gate, w_up, w_down)
        sti = 0; fi = 0; yi = 0

        def pf_gu(e, f):
            nonlocal sti
            for m in (0, 1):
                st = stg[sti % 4]
                stv = st.t.rearrange("p (k n) -> p k n", k=8)
                P.dma(SP, st, st[:], wsrc[m], wsrc[m].t[e, f, :, :])
                P.cp(ACT, wgf[m][f], wgt[m][:, :, f * 128:(f + 1) * 128], st, stv)
                sti += 1

        def pf_d(e, k, b_):
            nonlocal sti
            st = stg[sti % 4]
            P.dma(SP, st, st[:], w_down, w_down.t[e][k * 128:(k + 1) * 128, :])
            P.cp(ACT, wdk[b_][k], wdb[b_][:, k, :], st, st[:])
            sti += 1

        items = [(sp_, e) for sp_ in range(S // TS) for e in range(E)]
        pdown = []

        def emit_down(hd, wb, bd_b, c, sp_, e):
            nonlocal yi
            for tt in range(4):
                n = c * 4 + tt
                for hf in range(2):
                    Y_ = pY[yi % 4]; yi += 1
                    for f in range(8):
                        P.mm(Y_, Y_[:], hd, hd[:, f, tt * 128:(tt + 1) * 128], wdk[wb][f], wdb[wb][:, f, hf * 512:(hf + 1) * 512], start=(f == 0), stop=(f == 7))
                    sl = slice(hf * 512, (hf + 1) * 512)
                    wcol = rw[:, sp_ * NTS + n, e:e + 1]
                    P.stt(acc, acc[:, n, sl], Y_, Y_[:], wcol, acc, acc[:, n, sl], ALU.mult, ALU.add, extra=[rw])

        def flush_down():
            for f_ in pdown:
                f_()
            pdown.clear()
        for f in range(8):
            pf_gu(0, f)
        for k in range(8):
            pf_d(0, k, 0)
        for it, (sp_, e) in enumerate(items):
            nxt = items[it + 1][1] if it + 1 < len(items) else None
            wb = it % 2
            if True:
                if e == 0:
                    t0 = sp_ * TS
                    P.dma(SP, h2, h2[:], h2Ts, h2Ts[:, :, t0:t0 + TS])
                bd_b = None
                if e == 0:
                    for n in range(NTS):
                        Yt = pY[yi % 4]; yi += 1
                        P.tr(Yt, Yt[0:32, 0:128], rw, rw[:, sp_ * NTS + n, :], identf)
                        P.cp(ACT, rwT, rwT[:], Yt, Yt[0:32, 0:128])
                        for hf in range(2):
                            Yb = pY[yi % 4]; yi += 1
                            P.mm(Yb, Yb[:], rwT, rwT[0:E, :], bdall, bdall[0:E, hf * 512:(hf + 1) * 512])
                            P.cp(ACT, acc, acc[:, n, hf * 512:(hf + 1) * 512], Yb, Yb[:])
                for c in range(NCS):
                    hd = hid[(e * NCS + c) % 2]
                    for f in range(8):
                        G_ = pG[fi % 2]; U_ = pU[fi % 2]; g_ = gs[fi % 2]; s_ = sg[fi % 2]; u_ = us[fi % 2]; fi += 1
                        for k in range(8):
                            P.mm(G_, G_[:], wgf[0][f], wgt[0][:, k, f * 128:(f + 1) * 128], h2, h2[:, k, c * 512:(c + 1) * 512], start=(k == 0), stop=(k == 7))
                        for k in range(8):
                            P.mm(U_, U_[:], wgf[1][f], wgt[1][:, k, f * 128:(f + 1) * 128], h2, h2[:, k, c * 512:(c + 1) * 512], start=(k == 0), stop=(k == 7))
                        P.act(s_, s_[:], G_, G_[:], AF.Sigmoid, bias=bg17[:, e, f:f + 1], scale=1.702, extra=[bg17])
                        P.act(u_, u_[:], U_, U_[:], AF.Identity, bias=bus[:, e, f:f + 1], extra=[bus])
                        P.ts(DVE, g_, g_[:], G_, G_[:], bgs[:, e, f:f + 1], 7.0, ALU.add, ALU.min, extra=[bgs])
                        P.stt(g_, g_[:], s_, s_[:], SIGCAP, g_, g_[:], ALU.min, ALU.mult)
                        P.ts(DVE, u_, u_[:], u_, u_[:], -7.0, 7.0, ALU.max, ALU.min)
                        P.stt(hd, hd[:, f, :], u_, u_[:], 1.0, g_, g_[:], ALU.add, ALU.mult)
                        if f == 1:
                            flush_down()
                        if nxt is not None and c == 0 and f >= 2:
                            pf_d(nxt, f - 2, (it + 1) % 2)
                        if nxt is not None and c == NCS - 1:
                            pf_gu(nxt, f)
                    if nxt is not None and c == 0:
                        pf_d(nxt, 6, (it + 1) % 2); pf_d(nxt, 7, (it + 1) % 2)
                    pdown.append(lambda hd=hd, wb=wb, bd_b=bd_b, c=c, sp_=sp_, e=e: emit_down(hd, wb, bd_b, c, sp_, e))
            if e != E - 1:
                continue
            flush_down()
            for n in range(NTS):
                gn = sp_ * NTS + n
                xl = x1l[0]; o_ = ot[0]
                P.dma(SP, xl, xl[:], x1s, x1s[gn * 128:(gn + 1) * 128, :])
                rstd_of(acc, acc[:, n, :], junk, ss, rstd)
                P.stt(o_, o_[:], acc, acc[:, n, :], rstd[:, 0:1], G2, G2[:], ALU.mult, ALU.mult, extra=[rstd])
                P.tt(POOL, o_, o_[:], o_, o_[:], xl, xl[:], ALU.add)
                P.dma(POOL, out, out[gn * 128:(gn + 1) * 128, :], o_, o_[:], sem_buf=o_)
                fin.append(o_)
    P.wait_all(SP, fin)
    P.wait_all(POOL, fin)
    P.emit()
    return nc


def _bucket(n):
    n = np.maximum(n, 0)
    nf = np.maximum(n, 1).astype(np.float32)
    large = 16 + (np.log(nf / np.float32(16)) / np.float32(math.log(128 / 16)) * np.float32(16)).astype(np.int32)
    large = np.minimum(large, 31)
    return np.where(n < 16, n, large).astype(np.int64)


def host_consts(S):
    NT, NSEL = S // 128, S // 64
    NCMP = (S - 32) // 16 + 1
    NCT = (NCMP + 127) // 128
    p = np.arange(128)[:, None]; j = np.arange(512)[None, :]
    winM = np.zeros((9, 128, 512), np.float32); winI = np.zeros((9, 128, 512), np.int64)
    for idx in range(9):
        d = -1 if idx == 8 else idx - 4
        dist = j - (128 * d + p)
        ok = (dist >= 0) if idx == 8 else ((dist >= 0) & (dist < 512))
        winM[idx] = np.where(ok, 0.0, NEGM); winI[idx] = _bucket(dist)
    cmpM = np.zeros((5, 128, 512), np.float32); cmpI = np.zeros((5, 128, 512), np.int64)
    for r in range(5):
        nn = 512 * r + j - 16 * p - 31
        cmpM[r] = np.where(nn >= 0, 0.0, NEGM); cmpI[r] = _bucket(nn)
    caus = np.zeros((4, 128, 512), np.float32)
    for d in range(4):
        caus[d] = np.where(128 * d + p <= j, 0.0, NEGM)
    cs = np.arange(NCT * 128)[:, None] * 16; ss = np.arange(NSEL)[None, :] * 64
    ov = ((cs < ss + 64) & (cs + 32 > ss) & (np.arange(NCT * 128)[:, None] < NCMP)).astype(np.float32)
    ovl = np.concatenate([ov, np.ones((NCT * 128, 1), np.float32)], axis=1)
    t = np.arange(S)[:, None]; jb = np.arange(NSEL)[None, :]
    cur = t // 64
    forced = (jb == 0) | (jb == cur) | (jb == cur - 1)
    svalid = jb * 64 <= t
    A = np.where(forced, BIG, np.where(svalid, 0.0, -BIG)).astype(np.float32)
    Vm = (svalid & ~forced).astype(np.float32)
    tom = lambda a: np.ascontiguousarray(a.reshape(NT, 128, NSEL).transpose(1, 0, 2).reshape(128, NT * NSEL))
    Xp = np.zeros((NSEL, NT, 128), np.float32)
    for k in range(NT):
        Xp[2 * k, k, :64] = 1.0; Xp[2 * k + 1, k, 64:] = 1.0
    tri = (np.arange(128)[:, None] <= np.arange(128)[None, :]).astype(np.float32)
    return dict(winM=winM, cmpM=cmpM, caus=caus, ovl=ovl, selA=tom(A), selV=tom(Vm), Xp=Xp.reshape(NSEL, NT * 128),
                tri=tri), winI, cmpI


def _fblk(w):
    E_ = w.shape[0]
    return np.ascontiguousarray(w.reshape(E_, 8, 128, 8, 128).transpose(0, 3, 2, 1, 4).reshape(E_, 8, 128, 1024), dtype=np.float32)


def host_inputs(inp, b, S, E, consts, winI, cmpI):
    f = lambda a: np.ascontiguousarray(a, dtype=np.float32)
    NT = S // 128
    colT = lambda v: f(v.reshape(8, 128).T)
    rep = lambda v: f(np.broadcast_to(v[None, :], (128, v.shape[0])))
    rb = inp["rel_bias"]
    m = dict(consts)
    m.update(
        x=f(inp["x"][b, :S]), cT=colT(inp["c"][b]), w_ada=f(inp["w_ada"][0]), b_ada=f(inp["b_ada"][0][None, :]),
        gpre1=colT(inp["g_mix_pre"][0]), gpre2=colT(inp["g_ffn_pre"][0]),
        gpost1=rep(inp["g_mix_post"][0]), gpost2=rep(inp["g_ffn_post"][0]),
        w_in=f(inp["w_in"][0][:, w_in_perm()]), bfg=f(np.tile(rep(inp["b_forget"][0]), (1, NT))),
        pekT=f(inp["pe_k"][0].T), pevT=f(inp["pe_v"][0].T), wck=f(inp["w_cmp_k"][0]), wcv=f(inp["w_cmp_v"][0]),
        wfp=f(inp["w_fox_proj"][0]), wnp=f(inp["w_nsa_proj"][0]), wmo=f(inp["w_mix_out"][0]),
        winG=f(rb[winI].transpose(3, 0, 1, 2)), cmpG=f(rb[cmpI].transpose(3, 0, 1, 2)), farc=rep(rb[31]),
        w_router=f(inp["w_router"][0]), b_router=rep(inp["b_router"][0]),
        w_gate=_fblk(inp["w_gate"][0][:E]), w_up=_fblk(inp["w_up"][0][:E]), w_down=f(inp["w_down"][0][:E]),
        bgT=f(inp["b_gate"][0][:E].reshape(E, 8, 128).transpose(2, 0, 1).reshape(128, E * 8)),
        buT=f(inp["b_up"][0][:E].reshape(E, 8, 128).transpose(2, 0, 1).reshape(128, E * 8)),
        bdn=f(inp["b_down"][0][:E].reshape(1, E * 1024)),
    )
    return m


_NC_CACHE = {}


def kernel(**inputs):
    S, E, B = 4096, 32, 8
    if "nc" not in _NC_CACHE:
        _NC_CACHE["nc"] = build(S, E)
    nc = _NC_CACHE["nc"]
    inp = {k: np.asarray(v) for k, v in inputs.items()}
    consts, winI, cmpI = host_consts(S)
    shared = host_inputs(inp, 0, S, E, consts, winI, cmpI)
    in_maps = []
    for b in range(B):
        m = dict(shared)
        m["x"] = np.ascontiguousarray(inp["x"][b], dtype=np.float32)
        m["cT"] = np.ascontiguousarray(inp["c"][b].reshape(8, 128).T, dtype=np.float32)
        in_maps.append(m)
    res = run_bass_kernel_spmd(nc, in_maps, core_ids=list(range(B)))
    return np.stack([np.asarray(r["out"], dtype=np.float32) for r in res.results], axis=0)
```
